# Optimizing a Trainium2 kernel written in Bass

```python
import jax, jax.numpy as jnp
from jax import lax
import numpy as np

D_MODEL = 1024
BATCH = 8
SEQ = 2048
DEPTH = 2

N_HEADS_ATT = 8
HEAD_DIM_ATT = 64
ATT_WIDTH = N_HEADS_ATT * HEAD_DIM_ATT
MOBA_BLOCK = 256
MOBA_TOPK = 3
MOBA_QCHUNK = 16
N_HEADS_RW = 8
HEAD_DIM_RW = 64
RW_WIDTH = N_HEADS_RW * HEAD_DIM_RW
DECAY_LORA = 64
ICLR_LORA = 64
VMIX_LORA = 32
RW_SHIFT_WIDTH = 3 * RW_WIDTH + DECAY_LORA + ICLR_LORA
IN_WIDTHS = (ATT_WIDTH, ATT_WIDTH, ATT_WIDTH, ATT_WIDTH, RW_SHIFT_WIDTH, RW_WIDTH, D_MODEL, D_MODEL)
D_IN = 4 * ATT_WIDTH + RW_SHIFT_WIDTH + RW_WIDTH + 2 * D_MODEL
RMS_EPS = 1e-6
GN_EPS = 64e-5
L2_EPS = 1e-12
NEG_INF = -1e30

kernel_name = "hybrid_moba_rwkv7_gated_block"


def split_cols(p, widths):
    idx = np.cumsum(np.array(widths))[:-1].tolist()
    return jnp.split(p, idx, axis=-1)


def rmsnorm(x, g):
    xf = x.astype(jnp.float32)
    y = xf * lax.rsqrt(jnp.mean(xf * xf, axis=-1, keepdims=True) + RMS_EPS)
    return (y * g.astype(jnp.float32)).astype(x.dtype)


def token_shift_mix(y, mu):
    prev = jnp.pad(y[:, :-1], ((0, 0), (1, 0), (0, 0)))
    return y + (prev - y) * mu


def alibi_slopes(n_heads):
    return 2.0 ** (-8.0 * jnp.arange(1, n_heads + 1, dtype=jnp.float32) / n_heads)


def moba_attention(q, k, v):
    B, S, H, Dh = q.shape
    nb = -(-S // MOBA_BLOCK)
    Sp = nb * MOBA_BLOCK
    nc = Sp // MOBA_QCHUNK
    kk = min(MOBA_TOPK, nb)

    def prep(t):
        return jnp.pad(jnp.transpose(t, (0, 2, 1, 3)), ((0, 0), (0, 0), (0, Sp - S), (0, 0)))

    q = prep(q) * (Dh ** -0.5)
    k = prep(k)
    v = prep(v)
    kb = k.reshape(B, H, nb, MOBA_BLOCK, Dh)
    vb = v.reshape(B, H, nb, MOBA_BLOCK, Dh)

    kmean = jnp.mean(kb.astype(jnp.float32), axis=3)
    gate = jnp.einsum('bhsd,bhnd->bhsn', q.astype(jnp.float32), kmean)
    qblk = jnp.arange(Sp) // MOBA_BLOCK
    past = jnp.arange(nb)[None, :] < qblk[:, None]
    gate = jnp.where(past, gate, NEG_INF)
    _, sel = lax.top_k(gate, kk)

    slopes = alibi_slopes(H)
    bi = jnp.arange(B)[:, None, None, None]
    hi = jnp.arange(H)[None, :, None, None]
    kpos_in = jnp.arange(MOBA_BLOCK)

    def chunk(args):
        qc, selc, start = args
        blk = start // MOBA_BLOCK
        qpos = (start + jnp.arange(MOBA_QCHUNK)).astype(jnp.float32)
        ksel = kb[bi, hi, selc]
        vsel = vb[bi, hi, selc]
        spos = (selc[..., None] * MOBA_BLOCK + kpos_in).astype(jnp.float32)
        ls = jnp.einsum('bhqd,bhqjkd->bhqjk', qc, ksel).astype(jnp.float32)
        ls = ls - slopes[None, :, None, None, None] * (qpos[None, None, :, None, None] - spos)
        ls = jnp.where((selc < blk)[..., None], ls, NEG_INF)
        kown = lax.dynamic_index_in_dim(kb, blk, axis=2, keepdims=False)
        vown = lax.dynamic_index_in_dim(vb, blk, axis=2, keepdims=False)
        opos = (blk * MOBA_BLOCK + kpos_in).astype(jnp.float32)
        dist = qpos[:, None] - opos[None, :]
        lo = jnp.einsum('bhqd,bhkd->bhqk', qc, kown).astype(jnp.float32)
        lo = lo - slopes[None, :, None, None] * dist
        lo = jnp.where(dist >= 0, lo, NEG_INF)
        logits = jnp.concatenate([ls.reshape(B, H, MOBA_QCHUNK, kk * MOBA_BLOCK), lo], axis=-1)
        p = jax.nn.softmax(logits, axis=-1).astype(vb.dtype)
        psel = p[..., :kk * MOBA_BLOCK].reshape(B, H, MOBA_QCHUNK, kk, MOBA_BLOCK)
        pown = p[..., kk * MOBA_BLOCK:]
        return (jnp.einsum('bhqjk,bhqjkd->bhqd', psel, vsel)
                + jnp.einsum('bhqk,bhkd->bhqd', pown, vown))

    qcs = jnp.moveaxis(q.reshape(B, H, nc, MOBA_QCHUNK, Dh), 2, 0)
    sels = jnp.moveaxis(sel.reshape(B, H, nc, MOBA_QCHUNK, kk), 2, 0)
    starts = jnp.arange(nc) * MOBA_QCHUNK
    out = lax.map(chunk, (qcs, sels, starts))
    out = jnp.moveaxis(out, 0, 2).reshape(B, H, Sp, Dh)[:, :, :S]
    return jnp.transpose(out, (0, 2, 1, 3)).reshape(B, S, H * Dh)


def rwkv7_scan(r, w, k, v, kk, a):
    B, S, H, N = r.shape

    def step(state, inp):
        r_t, w_t, k_t, v_t, kk_t, a_t = inp
        sa = jnp.einsum('bhvk,bhk->bhv', state, -kk_t)
        state = (state * w_t[:, :, None, :]
                 + sa[..., None] * (kk_t * a_t)[:, :, None, :]
                 + v_t[..., None] * k_t[:, :, None, :])
        return state, jnp.einsum('bhvk,bhk->bhv', state, r_t)

    xs = tuple(jnp.moveaxis(t.astype(jnp.float32), 1, 0) for t in (r, w, k, v, kk, a))
    state0 = jnp.zeros((B, H, N, N), jnp.float32)
    _, out = lax.scan(step, state0, xs)
    return jnp.moveaxis(out, 0, 1)


def head_groupnorm(o, g, b):
    B, S, H, N = o.shape
    mu = jnp.mean(o, axis=-1, keepdims=True)
    var = jnp.mean(jnp.square(o - mu), axis=-1, keepdims=True)
    on = ((o - mu) * lax.rsqrt(var + GN_EPS)).reshape(B, S, H * N)
    return on * g.astype(jnp.float32) + b.astype(jnp.float32)


def setup_inputs(seed: int = 0) -> dict:
    key = jax.random.key(seed)
    ks = jax.random.split(key, 24)
    f32 = jnp.float32
    L, D, Lv = DEPTH, D_MODEL, DEPTH - 1

    def nrm(k, shape, scale):
        return jax.random.normal(k, shape, f32) * scale

    return {
        "x": nrm(ks[0], (BATCH, SEQ, D), 1.0),
        "norm_pre": 1.0 + nrm(ks[1], (L, D), 0.05),
        "norm_post": 1.0 + nrm(ks[2], (L, D), 0.05),
        "w_in": nrm(ks[3], (L, D, D_IN), D ** -0.5),
        "rw_mu": jax.random.uniform(ks[4], (L, RW_SHIFT_WIDTH), f32, 0.0, 1.0),
        "rw_w0": jax.random.uniform(ks[5], (L, RW_WIDTH), f32, -6.0, 0.5),
        "rw_w_up": nrm(ks[6], (L, DECAY_LORA, RW_WIDTH), 0.1 * DECAY_LORA ** -0.5),
        "rw_a0": nrm(ks[7], (L, RW_WIDTH), 0.5),
        "rw_a_up": nrm(ks[8], (L, ICLR_LORA, RW_WIDTH), 0.5 * ICLR_LORA ** -0.5),
        "rw_k_k": 0.85 + nrm(ks[9], (L, RW_WIDTH), 0.05),
        "rw_k_a": 1.0 + nrm(ks[10], (L, RW_WIDTH), 0.05),
        "rw_r_k": nrm(ks[11], (L, RW_WIDTH), 0.1),
        "rw_ln_g": 1.0 + nrm(ks[12], (L, RW_WIDTH), 0.05),
        "rw_ln_b": nrm(ks[13], (L, RW_WIDTH), 0.02),
        "rw_vmix_down": nrm(ks[14], (Lv, D, VMIX_LORA), D ** -0.5),
        "rw_vmix_mu": jax.random.uniform(ks[15], (Lv, VMIX_LORA), f32, 0.0, 1.0),
        "rw_vmix_up": nrm(ks[16], (Lv, VMIX_LORA, RW_WIDTH), 0.5 * VMIX_LORA ** -0.5),
        "rw_vmix0": nrm(ks[17], (Lv, RW_WIDTH), 0.5),
        "w_up_att": nrm(ks[18], (L, ATT_WIDTH, D), ATT_WIDTH ** -0.5),
        "w_up_rw": nrm(ks[19], (L, RW_WIDTH, D), RW_WIDTH ** -0.5),
        "w_out": nrm(ks[20], (L, D, D), D ** -0.5),
    }


def reference(x, norm_pre, norm_post, w_in, rw_mu, rw_w0, rw_w_up, rw_a0, rw_a_up, rw_k_k, rw_k_a,
              rw_r_k, rw_ln_g, rw_ln_b, rw_vmix_down, rw_vmix_mu, rw_vmix_up, rw_vmix0,
              w_up_att, w_up_rw, w_out):
    B, S, _ = x.shape
    v_first = None
    for l in range(DEPTH):
        h = rmsnorm(x, norm_pre[l])
        proj = jnp.einsum('bsd,de->bse', h, w_in[l])
        aq, ak, av, az, rw_stream, rz, g_att, g_rw = split_cols(proj, IN_WIDTHS)

        heads_a = lambda t: t.reshape(B, S, N_HEADS_ATT, HEAD_DIM_ATT)
        ya = moba_attention(heads_a(aq), heads_a(ak), heads_a(av)).astype(x.dtype)
        ya = ya * jax.nn.silu(az)

        rs, ksr, vs, wd, ad = split_cols(token_shift_mix(rw_stream, rw_mu[l]),
                                         (RW_WIDTH, RW_WIDTH, RW_WIDTH, DECAY_LORA, ICLR_LORA))
        w_log = -jax.nn.softplus(-(rw_w0[l] + jnp.tanh(wd) @ rw_w_up[l])) - 0.5
        decay = jnp.exp(-jnp.exp(w_log.astype(jnp.float32)))
        a = jax.nn.sigmoid(rw_a0[l] + ad @ rw_a_up[l])
        if l == 0:
            v_first = vs
            vr = vs
        else:
            vd = token_shift_mix(h @ rw_vmix_down[l - 1], rw_vmix_mu[l - 1])
            vr = vs + (v_first - vs) * jax.nn.sigmoid(rw_vmix0[l - 1] + vd @ rw_vmix_up[l - 1])
        heads_b = lambda t: t.reshape(B, S, N_HEADS_RW, HEAD_DIM_RW)
        kkf = heads_b(ksr * rw_k_k[l]).astype(jnp.float32)
        kkf = kkf / jnp.maximum(jnp.sqrt(jnp.sum(kkf * kkf, axis=-1, keepdims=True)), L2_EPS)
        kmod = ksr * (1.0 + (a - 1.0) * rw_k_a[l])
        o = rwkv7_scan(heads_b(rs), heads_b(decay), heads_b(kmod), heads_b(vr), kkf, heads_b(a))
        o = head_groupnorm(o, rw_ln_g[l], rw_ln_b[l])
        bonus = (jnp.sum(heads_b((rs * kmod * rw_r_k[l]).astype(jnp.float32)), axis=-1, keepdims=True)
                 * heads_b(vr.astype(jnp.float32))).reshape(B, S, RW_WIDTH)
        yb = (o + bonus).astype(x.dtype) * jax.nn.silu(rz)

        u = (jax.nn.sigmoid(g_att) * (ya @ w_up_att[l])
             + jax.nn.sigmoid(g_rw) * (yb @ w_up_rw[l]))
        y = u @ w_out[l]
        x = x + rmsnorm(y, norm_post[l])
    return x
```

```python
import math
from contextlib import ExitStack
import numpy as np
import ml_dtypes
import concourse.bass as bass
import concourse.mybir as mybir
from concourse.bass_utils import run_bass_kernel_spmd

F32 = mybir.dt.float32
F32R = mybir.dt.float32r
BF16 = mybir.dt.bfloat16
ALU = mybir.AluOpType
AF = mybir.ActivationFunctionType
AX = mybir.AxisListType

D = 1024
T = 2048
DIN = 6272
NL = 2
NCORES = 8
RMS_EPS = 1e-6
GN_EPS = 64e-5
C_ATT_Q, C_ATT_K, C_ATT_V, C_ATT_Z = 0, 512, 1024, 1536
C_RW_R, C_RW_K, C_RW_V, C_RW_WD, C_RW_AD, C_RW_Z = 2048, 2560, 3072, 3584, 3648, 3712
C_G_ATT, C_G_RW = 4224, 5248
PROJ_ROWS = DIN + 32
NEGM = -30000.0
CH = 64
PC_MU, PC_W0, PC_A0, PC_KK, PC_KA, PC_RK, PC_LNG, PC_LNB, PC_VM0, PC_VMU = 0, 13, 17, 21, 25, 29, 33, 37, 41, 45
NPC = 46


class Buf:
    __slots__ = ("name", "w", "rs")

    def __init__(self, name=""):
        self.name = name
        self.w = None
        self.rs = []


class Sched:
    def __init__(self, nc, ndma=10, same_engine_waits=True):
        self.nc = nc
        self.eng = {"pe": nc.tensor, "act": nc.scalar, "dve": nc.vector,
                    "pool": nc.gpsimd, "sp": nc.sync}
        self.stack = []
        self.sem = {}
        self.cnt = {}
        for e in self.eng:
            cm = nc.semaphore("s_" + e)
            self.sem[e] = cm.__enter__()
            self.stack.append(cm)
            self.cnt[e] = 0
        self.dma_sems = {}
        self.dma_rr = {}
        for q in ("sp", "pool", "act"):
            lst = []
            for i in range(ndma):
                key = "d_%s%d" % (q, i)
                cm = nc.semaphore(key)
                self.sem[key] = cm.__enter__()
                self.stack.append(cm)
                self.cnt[key] = 0
                lst.append(key)
            self.dma_sems[q] = lst
            self.dma_rr[q] = 0
        self.seen = {e: {} for e in self.eng}
        self.same = same_engine_waits
        self.n_wait = 0
        self.n_ins = 0

    def _need(self, e, needs, ev):
        if ev is None:
            return
        k, v = ev
        if k == e and (e == "pe" or not self.same):
            return
        if self.seen[e].get(k, 0) >= v:
            return
        if needs.get(k, 0) < v:
            needs[k] = v

    def _collect(self, e, reads, writes):
        needs = {}
        for b in reads:
            self._need(e, needs, b.w)
        for b in writes:
            self._need(e, needs, b.w)
            for r in b.rs:
                self._need(e, needs, r)
        return needs

    def _emit_waits(self, e, needs):
        eng = self.eng[e]
        for k, v in needs.items():
            eng.wait_ge(self.sem[k], v)
            self.seen[e][k] = v
            self.n_wait += 1

    def _commit(self, ev, reads, writes):
        for b in reads:
            b.rs.append(ev)
        for b in writes:
            b.w = ev
            b.rs = []

    def op(self, e, fn, reads=(), writes=()):
        needs = self._collect(e, reads, writes)
        self._emit_waits(e, needs)
        ins = fn(self.eng[e])
        self.cnt[e] += 1
        ins.then_inc(self.sem[e], 1)
        ev = (e, self.cnt[e])
        if e != "pe" and self.same:
            pass
        self._commit(ev, reads, writes)
        self.n_ins += 1
        return ev

    def dma(self, q, out, in_, reads=(), writes=(), **kw):
        lst = self.dma_sems[q]
        key = lst[self.dma_rr[q] % len(lst)]
        self.dma_rr[q] += 1
        needs = self._collect(q, reads, writes)
        if self.cnt[key] > 0:
            self._need(q, needs, (key, self.cnt[key]))
        self._emit_waits(q, needs)
        ins = self.eng[q].dma_start(out=out, in_=in_, **kw)
        self.cnt[key] += 16
        ins.then_inc(self.sem[key], 16)
        ev = (key, self.cnt[key])
        self._commit(ev, reads, writes)
        self.n_ins += 1
        return ev

    def wait_all(self, e, bufs):
        needs = {}
        for b in bufs:
            self._need(e, needs, b.w)
        self._emit_waits(e, needs)

    def barrier(self):
        snap = {k: v for k, v in self.cnt.items() if v > 0}
        for e in self.eng:
            needs = {}
            for k, v in snap.items():
                if k == e:
                    continue
                if self.seen[e].get(k, 0) < v:
                    needs[k] = v
            self._emit_waits(e, needs)

    def close(self):
        for cm in reversed(self.stack):
            cm.__exit__(None, None, None)


_UID = [0]


def _uname(name):
    _UID[0] += 1
    return "%s_%d" % (name, _UID[0])


class Ring:
    def __init__(self, es, nc, name, n, shape, dtype, psum=False):
        self.items = []
        name = _uname(name)
        for i in range(n):
            if psum:
                t = es.enter_context(nc.psum_tensor("%s%d" % (name, i), shape, dtype))
            else:
                t = es.enter_context(nc.sbuf_tensor("%s%d" % (name, i), shape, dtype))
            self.items.append((t, Buf(name + str(i))))
        self.i = 0

    def next(self):
        it = self.items[self.i % len(self.items)]
        self.i += 1
        return it


class PsumRing:
    def __init__(self, es, nc, name, n, width):
        per = 512 // width
        nb = (n + per - 1) // per
        name = _uname(name)
        self.items = []
        for b in range(nb):
            t = es.enter_context(nc.psum_tensor("%s_%d" % (name, b), [128, 512], F32))
            for k in range(per):
                if len(self.items) < n:
                    self.items.append((t[:, k * width:(k + 1) * width], Buf("%s_%d_%d" % (name, b, k))))
        self.i = 0

    def next(self):
        it = self.items[self.i % len(self.items)]
        self.i += 1
        return it


def sb(es, nc, name, shape, dtype):
    name = _uname(name)
    return es.enter_context(nc.sbuf_tensor(name, shape, dtype)), Buf(name)


def host_consts():
    bf = ml_dtypes.bfloat16
    c = {}
    c["ident_f"] = np.eye(128, dtype=np.float32)
    c["ident_b"] = np.eye(128).astype(bf)
    kk, qq = np.meshgrid(np.arange(128), np.arange(128), indexing="ij")
    c["trimask"] = np.where(kk > qq, NEGM, 0.0).astype(bf)
    half = np.arange(128) // 64
    same = (half[:, None] == half[None, :])
    c["blk64"] = same.astype(np.float32)
    loc = np.arange(128) % 64
    su = same & (loc[:, None] < loc[None, :])
    iu = same & (loc[:, None] <= loc[None, :])
    c["mask2"] = np.concatenate([su, iu], axis=1).astype(np.float32)
    c["maskl"] = (same & (loc[:, None] > loc[None, :])).astype(np.float32)
    gs = np.zeros((64, 8, 72), np.float32)
    for n in range(8):
        for m in range(8):
            for qb in range(8):
                if m < qb:
                    gs[n * 8 + m, qb, 64 + n] = 1.0
    c["gsum"] = gs.astype(bf)
    thr = np.zeros((128, 8), np.float32)
    for n in range(8):
        for qb in range(8):
            thr[64 + n, qb] = 2.5 if n < qb else (1e9 if n == qb else -1.0)
    c["thr"] = thr
    pos = np.arange(T)
    hi, lo = pos // 128, pos % 128
    c["qconst"] = np.stack([-128.0 * hi, -1.0 * lo, np.ones(T), np.ones(T)]).astype(bf)
    kc = np.zeros((8, 12, T), np.float32)
    for h in range(8):
        sl = 2.0 ** (-(h + 1))
        for n in range(8):
            kc[h, n] = np.where(pos // 256 == n, NEGM, 0.0)
        kc[h, 8] = sl
        kc[h, 9] = sl
        kc[h, 10] = sl * 128.0 * hi
        kc[h, 11] = sl * lo
    c["kconst"] = kc.astype(bf)
    return c


def pack_params(inp):
    out = np.zeros((NL, 128, NPC), np.float32)

    def put(l, col, vec):
        n = vec.shape[0]
        if n % 128 == 0:
            out[l, :, col:col + n // 128] = vec.reshape(n // 128, 128).T
        else:
            out[l, :n, col] = vec

    for l in range(NL):
        put(l, PC_MU, inp["rw_mu"][l])
        put(l, PC_W0, inp["rw_w0"][l])
        put(l, PC_A0, inp["rw_a0"][l])
        put(l, PC_KK, inp["rw_k_k"][l])
        put(l, PC_KA, inp["rw_k_a"][l])
        put(l, PC_RK, inp["rw_r_k"][l])
        put(l, PC_LNG, inp["rw_ln_g"][l])
        put(l, PC_LNB, inp["rw_ln_b"][l])
        if l >= 1:
            put(l, PC_VM0, inp["rw_vmix0"][l - 1])
            put(l, PC_VMU, inp["rw_vmix_mu"][l - 1])
    return out


class Prog:
    def __init__(self, dbg=(), nlayers=NL, stop_after=None):
        self.dbg = set(dbg)
        self.nlayers = nlayers
        self.stop_after = stop_after
        nc = bass.Bass("TRN2", target_bir_lowering=False)
        self.nc = nc
        self.S = Sched(nc)
        self.inp = {}
        self.scr = {}
        self.dbuf = {}

    def din(self, name, shape, dtype=F32):
        ap = self.nc.dram_tensor(name, list(shape), dtype, kind="ExternalInput").ap()
        self.inp[name] = ap
        self.dbuf[name] = Buf(name)
        return ap

    def dscr(self, name, shape, dtype=F32):
        kind = "ExternalOutput" if name in self.dbg else "Internal"
        ap = self.nc.dram_tensor(name, list(shape), dtype, kind=kind).ap()
        self.scr[name] = ap
        self.dbuf[name] = Buf(name)
        return ap

    def build(self):
        nc, S = self.nc, self.S
        x = self.din("x", [T, D])
        self.din("norm_pre", [NL, D])
        self.din("norm_post", [NL, D])
        self.din("w_in", [NL, D, DIN])
        self.din("rw_w_up", [NL, 64, 512])
        self.din("rw_a_up", [NL, 64, 512])
        self.din("rw_vmix_down", [1, D, 32])
        self.din("rw_vmix_up", [1, 32, 512])
        self.din("w_up_att", [NL, 512, D])
        self.din("w_up_rw", [NL, 512, D])
        self.din("w_out", [NL, D, D])
        self.din("pc", [NL, 128, NPC])
        hc = host_consts()
        for k, v in hc.items():
            self.din("c_" + k, v.shape, BF16 if v.dtype == ml_dtypes.bfloat16 else F32)
        out = self.nc.dram_tensor("out", [T, D], F32, kind="ExternalOutput").ap()
        self.dbuf["out"] = Buf("out")
        self.dscr("projT", [PROJ_ROWS, T])
        self.dscr("vtm", [T, 520], BF16)
        self.dscr("yagT", [512, T], BF16)
        self.dscr("ybT", [512, T], BF16)
        self.dscr("vfirst", [512, T])
        self.dscr("x1", [T, D])

        with ExitStack() as es:
            self.load_consts(es)
            xin, xin_b = x, self.dbuf["x"]
            for l in range(self.nlayers):
                last = (l == self.nlayers - 1)
                xo, xo_b = (out, self.dbuf["out"]) if last else (self.scr["x1"], self.dbuf["x1"])
                self.phase1(l, xin, xin_b)
                if self.stop_after == (l, 1):
                    break
                S.barrier()
                self.phase2(l)
                if self.stop_after == (l, 2):
                    break
                S.barrier()
                self.phase3(l)
                if self.stop_after == (l, 3):
                    break
                S.barrier()
                self.phase4(l, xin, xin_b, xo, xo_b)
                S.barrier()
                xin, xin_b = xo, xo_b
            S.wait_all("sp", list(self.dbuf.values()))
            S.barrier()
        S.close()
        return nc

    def load_consts(self, es):
        nc, S = self.nc, self.S
        self.K = {}
        for name, shape, dt in (("ident_f", [128, 128], F32), ("ident_b", [128, 128], BF16),
                                ("trimask", [128, 128], BF16), ("blk64", [128, 128], F32),
                                ("mask2", [128, 256], F32), ("maskl", [128, 128], F32),
                                ("thr", [128, 8], F32)):
            t, b = sb(es, nc, "k_" + name, shape, dt)
            S.dma("sp", t[:], self.inp["c_" + name][:, :], writes=[b])
            self.K[name] = (t, b)
        t, b = sb(es, nc, "k_gsum", [64, 8, 72], BF16)
        S.dma("sp", t[:], self.inp["c_gsum"][:, :, :], writes=[b])
        self.K["gsum"] = (t, b)
        t, b = sb(es, nc, "k_ones", [128, 64], F32)
        S.op("pool", lambda e: e.memset(t[:], 1.0), writes=[b])
        self.K["ones"] = (t, b)
        t2, b2 = sb(es, nc, "k_pc", [128, NL, NPC], F32)
        S.dma("sp", t2[:], self.inp["pc"].rearrange("l p c -> p l c"), writes=[b2])
        self.K["pc"] = (t2, b2)
        t3, b3 = sb(es, nc, "k_pc1m", [128, NL, NPC], F32)
        S.op("dve", lambda e: e.tensor_scalar(out=t3[:], in0=t2[:], scalar1=-1.0, scalar2=1.0,
                                              op0=ALU.mult, op1=ALU.add), reads=[b2], writes=[b3])
        self.K["pc1m"] = (t3, b3)
        t4, b4 = sb(es, nc, "k_pch", [128, NL, NPC], F32)
        S.op("dve", lambda e: e.tensor_scalar(out=t4[:], in0=t2[:], scalar1=0.5, scalar2=None, op0=ALU.mult),
             reads=[b2], writes=[b4])
        self.K["pch"] = (t4, b4)
        t5, b5 = sb(es, nc, "k_half", [128, 2], F32)
        S.op("pool", lambda e: e.memset(t5[:], 0.5), writes=[b5])
        self.K["half"] = (t5, b5)
        t6, b6 = sb(es, nc, "k_mhalf", [128, 512], F32)
        S.op("pool", lambda e: e.memset(t6[:], -0.5), writes=[b6])
        self.K["mhalf"] = (t6, b6)

    def phase1(self, l, xin, xin_b):
        nc, S = self.nc, self.S
        projT, projT_b = self.scr["projT"], self.dbuf["projT"]
        vtm, vtm_b = self.scr["vtm"], self.dbuf["vtm"]
        identf, identf_b = self.K["ident_f"]
        with ExitStack() as es:
            gpre, gpre_b = sb(es, nc, "p1_gpre", [128, D], F32)
            S.dma("sp", gpre[:], self.inp["norm_pre"][l].partition_broadcast(128), writes=[gpre_b])
            hT, _ = sb(es, nc, "p1_hT", [128, 8, T], BF16)
            hT_b = [Buf("hT%d" % i) for i in range(16)]
            xr = Ring(es, nc, "p1_x", 2, [128, D], F32)
            hr = Ring(es, nc, "p1_h", 2, [128, D], F32)
            junk, junk_b = sb(es, nc, "p1_junk", [128, D], F32)
            ssr = Ring(es, nc, "p1_ss", 4, [128, 2], F32)
            pst = Ring(es, nc, "p1_pst", 2, [128, 512], F32, psum=True)
            psm = Ring(es, nc, "p1_psm", 4, [128, 512], F32, psum=True)
            for tt in range(16):
                xt, xt_b = xr.next()
                S.dma("sp" if tt % 2 == 0 else "pool", xt[:], xin[tt * 128:(tt + 1) * 128, :],
                      reads=[xin_b], writes=[xt_b])
                ss, ss_b = ssr.next()
                S.op("act", lambda e: e.activation(out=junk[:], in_=xt[:], func=AF.Square,
                                                   accum_out=ss[:, 0:1]),
                     reads=[xt_b], writes=[junk_b, ss_b])
                S.op("dve", lambda e: e.tensor_scalar(out=ss[:, 1:2], in0=ss[:, 0:1], scalar1=1.0 / D,
                                                      scalar2=RMS_EPS, op0=ALU.mult, op1=ALU.add),
                     reads=[ss_b], writes=[ss_b])
                S.op("pool", lambda e: e.tensor_tensor(out=ss[:, 0:1], in0=ss[:, 1:2], in1=self.K["mhalf"][0][:, 0:1],
                                                       op=ALU.pow), reads=[ss_b, self.K["mhalf"][1]], writes=[ss_b])
                hf, hf_b = hr.next()
                S.op("dve", lambda e: e.scalar_tensor_tensor(out=hf[:], in0=xt[:], scalar=ss[:, 0:1],
                                                             in1=gpre[:], op0=ALU.mult, op1=ALU.mult),
                     reads=[xt_b, ss_b, gpre_b], writes=[hf_b])
                for half in range(2):
                    ps, ps_b = pst.next()
                    for j in range(4):
                        dc = half * 4 + j
                        S.op("pe", lambda e: e.transpose(ps[:, j * 128:(j + 1) * 128],
                                                         hf[:, dc * 128:(dc + 1) * 128], identf[:]),
                             reads=[hf_b, identf_b], writes=[ps_b])
                    eng = "act" if half == 0 else "dve"
                    dst = hT[:, half * 4:half * 4 + 4, tt * 128:(tt + 1) * 128]
                    src = ps[:, :].rearrange("p (a b) -> p a b", a=4)
                    if eng == "act":
                        S.op("act", lambda e: e.activation(out=dst, in_=src, func=AF.Copy),
                             reads=[ps_b], writes=[hT_b[tt]])
                    else:
                        S.op("dve", lambda e: e.tensor_copy(out=dst, in_=src),
                             reads=[ps_b], writes=[hT_b[tt]])
            wst = Ring(es, nc, "p1_wst", 2, [128, 8, 512], F32)
            wbf = Ring(es, nc, "p1_wbf", 2, [128, 8, 512], BF16)
            stg = Ring(es, nc, "p1_stg", 4, [128, 512], F32)
            vst = Ring(es, nc, "p1_vst", 2, [128, 8, 65], BF16)
            for (vt, vb) in vst.items:
                S.op("pool", lambda e: e.memset(vt[:], 1.0), writes=[vb])
            w_in = self.inp["w_in"][l].rearrange("(dc p) e -> p dc e", p=128)
            w_in_b = self.dbuf["w_in"]
            blocks = [(c0, min(512, DIN - c0)) for c0 in range(0, DIN, 512)]
            nev = 0
            def load_block(bi):
                c0, ncol = blocks[bi]
                ws, ws_b = wst.next()
                S.dma("sp", ws[:, 0:4, 0:ncol], w_in[:, 0:4, c0:c0 + ncol], reads=[w_in_b], writes=[ws_b])
                S.dma("sp", ws[:, 4:8, 0:ncol], w_in[:, 4:8, c0:c0 + ncol], reads=[w_in_b], writes=[ws_b])
                wb, wb_b = wbf.next()
                S.op("pool", lambda e: e.tensor_copy(out=wb[:, 0:4, 0:ncol], in_=ws[:, 0:4, 0:ncol]),
                     reads=[ws_b], writes=[wb_b])
                S.op("pool", lambda e: e.tensor_copy(out=wb[:, 4:8, 0:ncol], in_=ws[:, 4:8, 0:ncol]),
                     reads=[ws_b], writes=[wb_b])
                return wb, wb_b

            pending = load_block(0)
            for bi, (c0, ncol) in enumerate(blocks):
                wb, wb_b = pending
                if bi + 1 < len(blocks):
                    pending = load_block(bi + 1)
                if c0 == C_ATT_V:
                    for tt in range(16):
                        ps, ps_b = psm.next()
                        for dc in range(8):
                            S.op("pe", lambda e: e.matmul(ps[:, :], lhsT=hT[:, dc, tt * 128:(tt + 1) * 128],
                                                          rhs=wb[:, dc, :], start=(dc == 0), stop=(dc == 7)),
                                 reads=[hT_b[tt], wb_b], writes=[ps_b])
                        vt, vt_b = vst.next()
                        src = ps[:, :].rearrange("p (h d) -> p h d", h=8)
                        S.op("dve", lambda e: e.tensor_copy(out=vt[:, :, 0:64], in_=src),
                             reads=[ps_b], writes=[vt_b])
                        S.dma("pool", vtm[tt * 128:(tt + 1) * 128, :], vt[:].rearrange("p h d -> p (h d)"),
                              reads=[vt_b], writes=[vtm_b])
                    continue
                for g in range(ncol // 128):
                    for tc in range(4):
                        ps, ps_b = psm.next()
                        for dc in range(8):
                            S.op("pe", lambda e: e.matmul(ps[:, :], lhsT=wb[:, dc, g * 128:(g + 1) * 128],
                                                          rhs=hT[:, dc, tc * 512:(tc + 1) * 512],
                                                          start=(dc == 0), stop=(dc == 7)),
                                 reads=[wb_b] + hT_b[tc * 4:tc * 4 + 4], writes=[ps_b])
                        st, st_b = stg.next()
                        if nev % 2 == 0:
                            S.op("act", lambda e: e.activation(out=st[:], in_=ps[:, :], func=AF.Copy),
                                 reads=[ps_b], writes=[st_b])
                        else:
                            S.op("dve", lambda e: e.tensor_copy(out=st[:], in_=ps[:, :]),
                                 reads=[ps_b], writes=[st_b])
                        nev += 1
                        r0 = c0 + g * 128
                        S.dma("pool" if nev % 2 == 0 else "act",
                              projT[r0:r0 + 128, tc * 512:(tc + 1) * 512], st[:],
                              reads=[st_b], writes=[projT_b])
            if l >= 1:
                wv, wv_b = sb(es, nc, "p1_wv", [128, 8, 32], F32)
                wvb, wvb_b = sb(es, nc, "p1_wvb", [128, 8, 32], BF16)
                S.dma("sp", wv[:], self.inp["rw_vmix_down"][l - 1].rearrange("(dc p) e -> p dc e", p=128),
                      writes=[wv_b])
                S.op("dve", lambda e: e.tensor_copy(out=wvb[:], in_=wv[:]), reads=[wv_b], writes=[wvb_b])
                for tc in range(4):
                    ps, ps_b = psm.next()
                    for dc in range(8):
                        S.op("pe", lambda e: e.matmul(ps[0:32, :], lhsT=wvb[:, dc, :],
                                                      rhs=hT[:, dc, tc * 512:(tc + 1) * 512],
                                                      start=(dc == 0), stop=(dc == 7)),
                             reads=[wvb_b] + hT_b[tc * 4:tc * 4 + 4], writes=[ps_b])
                    st, st_b = stg.next()
                    S.op("dve", lambda e: e.tensor_copy(out=st[0:32, :], in_=ps[0:32, :]),
                         reads=[ps_b], writes=[st_b])
                    S.dma("sp", projT[DIN:DIN + 32, tc * 512:(tc + 1) * 512], st[0:32, :],
                          reads=[st_b], writes=[projT_b])

    def phase2(self, l):
        nc, S = self.nc, self.S
        projT, projT_b = self.scr["projT"], self.dbuf["projT"]
        vtm, vtm_b = self.scr["vtm"], self.dbuf["vtm"]
        yagT, yagT_b = self.scr["yagT"], self.dbuf["yagT"]
        identb, identb_b = self.K["ident_b"]
        trim, trim_b = self.K["trimask"]
        gsum, gsum_b = self.K["gsum"]
        thr, thr_b = self.K["thr"]
        ones, ones_b = self.K["ones"]
        with ExitStack() as es:
            vext, vext_b = sb(es, nc, "p2_vext", [128, 16, 520], BF16)
            S.dma("pool", vext[:], vtm.rearrange("(t p) c -> p t c", p=128), reads=[vtm_b], writes=[vext_b])
            qaug, _ = sb(es, nc, "p2_qaug", [128, T], BF16)
            kaug, _ = sb(es, nc, "p2_kaug", [128, T], BF16)
            qa_q, qa_n, qa_c = Buf("qa_q"), [Buf("qa_n%d" % i) for i in range(4)], Buf("qa_c")
            ka_k, ka_c = Buf("ka_k"), Buf("ka_c")
            S.dma("sp", qaug[72:76, :], self.inp["c_qconst"][:, :], writes=[qa_c])
            qfr = Ring(es, nc, "p2_qf", 2, [64, T], F32)
            kfr = Ring(es, nc, "p2_kf", 2, [64, T], F32)
            azr = Ring(es, nc, "p2_az", 2, [64, T], F32)
            szr = Ring(es, nc, "p2_sz", 2, [64, T], F32)
            kmean, kmean_b = sb(es, nc, "p2_kmean", [64, 8], F32)
            kdiff, kdiff_b = sb(es, nc, "p2_kdiff", [64, 8, 8], F32)
            indr = Ring(es, nc, "p2_ind", 2, [64, 512], BF16)
            ptr = Ring(es, nc, "p2_pt", 3, [128, 512], BF16)
            rden, rden_b = sb(es, nc, "p2_rden", [128, 512], F32)
            bcs, bcs_b = sb(es, nc, "p2_bcs", [64, 512], F32)
            yac, yac_b = sb(es, nc, "p2_yac", [64, 512], F32)
            yagr = Ring(es, nc, "p2_yag", 2, [64, T], BF16)
            ps_s = Ring(es, nc, "p2_pss", 3, [128, 512], F32, psum=True)
            ps_o = Ring(es, nc, "p2_pso", 2, [128, 512], F32, psum=True)
            ps_m = Ring(es, nc, "p2_psm", 2, [128, 512], F32, psum=True)
            deferred = []
            for h in range(8):
                qf, qf_b = qfr.next()
                kf, kf_b = kfr.next()
                azf, azf_b = azr.next()
                S.dma("sp", qf[:], projT[C_ATT_Q + h * 64:C_ATT_Q + (h + 1) * 64, :], reads=[projT_b], writes=[qf_b])
                S.dma("pool", kf[:], projT[C_ATT_K + h * 64:C_ATT_K + (h + 1) * 64, :], reads=[projT_b], writes=[kf_b])
                S.dma("sp", azf[:], projT[C_ATT_Z + h * 64:C_ATT_Z + (h + 1) * 64, :], reads=[projT_b], writes=[azf_b])
                S.dma("pool", kaug[64:76, :], self.inp["c_kconst"][h], writes=[ka_c])
                S.op("pool", lambda e: e.tensor_copy(out=kaug[0:64, :], in_=kf[:]), reads=[kf_b], writes=[ka_k])
                S.op("act", lambda e: e.activation(out=qaug[0:64, :], in_=qf[:], func=AF.Copy, scale=0.125),
                     reads=[qf_b], writes=[qa_q])
                sz, sz_b = szr.next()
                S.op("act", lambda e: e.activation(out=sz[:], in_=azf[:], func=AF.Tanh, scale=0.5), reads=[azf_b], writes=[sz_b])
                S.op("dve", lambda e: e.scalar_tensor_tensor(out=sz[:], in0=sz[:], scalar=1.0, in1=azf[:],
                                                             op0=ALU.add, op1=ALU.mult),
                     reads=[sz_b, azf_b], writes=[sz_b])
                S.op("dve", lambda e: e.reduce_sum(out=kmean[:], in_=kf[:].rearrange("p (n k) -> p n k", k=256),
                                                   axis=AX.X), reads=[kf_b], writes=[kmean_b])
                S.op("dve", lambda e: e.tensor_tensor(out=kdiff[:], in0=kmean[:, :].unsqueeze(1).to_broadcast([64, 8, 8]),
                                                      in1=kmean[:, :].unsqueeze(2).to_broadcast([64, 8, 8]),
                                                      op=ALU.subtract), reads=[kmean_b], writes=[kdiff_b])
                yag, yag_b = yagr.next()
                for c in range(4):
                    pg, pg_b = ps_m.next()
                    S.op("pe", lambda e: e.matmul(pg[0:64, :], lhsT=kdiff[:].rearrange("p n m -> p (n m)"),
                                                  rhs=qf[:, c * 512:(c + 1) * 512], start=True, stop=True),
                         reads=[kdiff_b, qf_b], writes=[pg_b])
                    ind, ind_b = indr.next()
                    S.op("dve", lambda e: e.tensor_single_scalar(out=ind[:], in_=pg[0:64, :], scalar=0.0, op=ALU.is_gt),
                         reads=[pg_b], writes=[ind_b])
                    pr, pr_b = ps_m.next()
                    for j in range(2):
                        qb = 2 * c + j
                        S.op("pe", lambda e: e.matmul(pr[0:72, j * 256:(j + 1) * 256], lhsT=gsum[:, qb, :],
                                                      rhs=ind[:, j * 256:(j + 1) * 256], start=True, stop=True),
                             reads=[gsum_b, ind_b], writes=[pr_b])
                    for j in range(2):
                        qb = 2 * c + j
                        S.op("dve", lambda e: e.tensor_scalar(out=qaug[64:72, qb * 256:(qb + 1) * 256],
                                                              in0=pr[64:72, j * 256:(j + 1) * 256],
                                                              scalar1=thr[64:72, qb:qb + 1], scalar2=None,
                                                              op0=ALU.is_ge),
                             reads=[pr_b, thr_b], writes=[qa_n[c]])
                    po, po_b = ps_o.next()
                    nkt = 4 * c + 4

                    def qk(kt, c=c, h=h):
                        j = kt - 4 * c
                        off = 0 if j < 0 else j * 128
                        n = 512 - off
                        q0 = c * 512 + off
                        pss, pss_b = ps_s.next()
                        S.op("pe", lambda e: e.matmul(pss[:, 0:n], lhsT=kaug[0:76, kt * 128:(kt + 1) * 128],
                                                      rhs=qaug[0:76, q0:q0 + n], start=True, stop=(j < 0)),
                             reads=[ka_k, ka_c, qa_q, qa_n[c], qa_c], writes=[pss_b])
                        if j >= 0:
                            S.op("pe", lambda e: e.matmul(pss[:, 0:128], lhsT=identb[:], rhs=trim[:],
                                                          start=False, stop=True),
                                 reads=[identb_b, trim_b], writes=[pss_b])
                        return pss, pss_b, off, n

                    def finalize(po=po, po_b=po_b, c=c, h=h, yag=yag, yag_b=yag_b, sz=sz, sz_b=sz_b, last=(c == 3)):
                        S.op("dve", lambda e: e.reciprocal(out=rden[64:65, :], in_=po[64:65, :]),
                             reads=[po_b], writes=[rden_b])
                        pb, pb_b = ps_m.next()
                        S.op("pe", lambda e: e.matmul(pb[0:64, :], lhsT=ones[64:65, 0:64], rhs=rden[64:65, :],
                                                      start=True, stop=True), reads=[ones_b, rden_b], writes=[pb_b])
                        S.op("act", lambda e: e.activation(out=bcs[:], in_=pb[0:64, :], func=AF.Copy),
                             reads=[pb_b], writes=[bcs_b])
                        S.op("dve", lambda e: e.scalar_tensor_tensor(out=yac[:], in0=po[0:64, :], scalar=0.5, in1=bcs[:],
                                                                     op0=ALU.mult, op1=ALU.mult),
                             reads=[po_b, bcs_b], writes=[yac_b])
                        S.op("pool", lambda e: e.tensor_tensor(out=yag[:, c * 512:(c + 1) * 512], in0=yac[:],
                                                               in1=sz[:, c * 512:(c + 1) * 512], op=ALU.mult),
                             reads=[yac_b, sz_b], writes=[yag_b])
                        if last:
                            S.dma("sp", yagT[h * 64:(h + 1) * 64, :], yag[:], reads=[yag_b], writes=[yagT_b])

                    cur = qk(0)
                    for kt in range(nkt):
                        nxt = qk(kt + 1) if kt + 1 < nkt else None
                        if kt == 1 and deferred:
                            deferred.pop()()
                        pss, pss_b, off, n = cur
                        pt, pt_b = ptr.next()
                        S.op("act", lambda e: e.activation(out=pt[:, 0:n], in_=pss[:, 0:n], func=AF.Exp),
                             reads=[pss_b], writes=[pt_b])
                        S.op("pe", lambda e: e.matmul(po[0:65, off:512], lhsT=vext[:, kt, h * 65:(h + 1) * 65],
                                                      rhs=pt[:, 0:n], start=(kt == 0), stop=(kt == nkt - 1)),
                             reads=[vext_b, pt_b], writes=[po_b])
                        cur = nxt
                    deferred.append(finalize)
            while deferred:
                deferred.pop()()

    def phase3_gen(self, l, es, TH=256):
        nc, S = self.nc, self.S
        projT, projT_b = self.scr["projT"], self.dbuf["projT"]
        ybT, ybT_b = self.scr["ybT"], self.dbuf["ybT"]
        vfirst, vfirst_b = self.scr["vfirst"], self.dbuf["vfirst"]
        identf, identf_b = self.K["ident_f"]
        blk64, blk64_b = self.K["blk64"]
        mask2, mask2_b = self.K["mask2"]
        maskl, maskl_b = self.K["maskl"]
        pc, pc_b = self.K["pc"]
        pc1m, pc1m_b = self.K["pc1m"]
        NCH = TH // CH
        C0 = math.exp(-0.5)
        PE_PER_B = 6
        R32 = F32R
        CARVE = False

        def col(t, c):
            return t[:, l, c:c + 1]

        wau, wau_b = sb(es, nc, "p3_wau", [128, 512], F32)
        S.dma("sp", wau[0:64, :], self.inp["rw_w_up"][l], writes=[wau_b])
        S.dma("sp", wau[64:128, :], self.inp["rw_a_up"][l], writes=[wau_b])
        if l >= 1:
            vmu, vmu_b = sb(es, nc, "p3_vmu", [32, 512], F32)
            S.dma("sp", vmu[:], self.inp["rw_vmix_up"][l - 1], writes=[vmu_b])
        idr, idr_b = sb(es, nc, "p3_idr", [128, 128], R32)
        S.op("dve", lambda e: e.tensor_copy(out=idr[:], in_=identf[:]), reads=[identf_b], writes=[idr_b])
        rings = {}

        def R(name, n=1, shape=None, dt=F32):
            if name not in rings:
                rings[name] = Ring(es, nc, "p3_" + name, n, list(shape or (128, TH)), dt)
            return rings[name].next()

        zf, zf_b = sb(es, nc, "p3_zf", [128, 2], F32)
        S.op("pool", lambda e: e.memset(zf[:], 0.0), writes=[zf_b])
        ARr = Ring(es, nc, "p3_AR", 4, [128, NCH * 256], R32)
        BKr = Ring(es, nc, "p3_BK", 3, [128, NCH * 256], R32)
        BVr = Ring(es, nc, "p3_BV", 3, [128, NCH * 384], R32)
        Wn2 = [Ring(es, nc, "p3_W%d" % i, 2 * NCH, [128, 384], R32) for i in range(2)]
        for rg, pat, kw in ((ARr, "p (c a q t) -> p c a q t", dict(c=NCH, a=2, q=2)),
                            (BKr, "p (c a q t) -> p c a q t", dict(c=NCH, a=2, q=2)),
                            (BVr, "p (c a q t) -> p c a q t", dict(c=NCH, a=3, q=2)),
                            (Wn2[0], "p (a b) -> p a b", dict(a=3)),
                            (Wn2[1], "p (a b) -> p a b", dict(a=3))):
            new_items = []
            for (t_, b_) in rg.items:
                n_ = t_[:].shape[1]
                S.op("dve", lambda e: e.tensor_copy(out=t_[:, :], in_=zf[:, 0:1].to_broadcast([128, n_])),
                     reads=[zf_b], writes=[b_])
                new_items.append((t_[:, :].rearrange(pat, **kw), b_))
            rg.items = new_items
        PTr2 = [Ring(es, nc, "p3_PT%d" % i, 2 * NCH, [128, 128], R32) for i in range(2)]
        NM1r = Ring(es, nc, "p3_NM1", 3 * NCH, [128, 256], R32)
        NM2r = Ring(es, nc, "p3_NM2", 3 * NCH, [128, 256], R32)
        NbTr2 = [Ring(es, nc, "p3_NbT%d" % i, NCH, [128, 128], R32) for i in range(2)]
        Tfr = Ring(es, nc, "p3_Tf", 3 * NCH, [128, 128], R32)
        TM3r = Ring(es, nc, "p3_TM3", 3 * NCH, [128, 3, 128], R32)
        W1r = Ring(es, nc, "p3_W1", 2, [128, 128], R32)
        UTr = Ring(es, nc, "p3_UT", 2, [128, 128], R32)
        Sr = Ring(es, nc, "p3_S", 2, [128, 128], R32)
        if CARVE:
            psA = PsumRing(es, nc, "p3_psA", 4, 256)
            psB = PsumRing(es, nc, "p3_psB", 8, 128)
        else:
            psA = PsumRing(es, nc, "p3_psA", 2, 512)
            psB = PsumRing(es, nc, "p3_psB", 4, 512)
            psA.items = [(a[:, 0:256], b) for a, b in psA.items]
            psB.items = [(a[:, 0:128], b) for a, b in psB.items]
        psC = PsumRing(es, nc, "p3_psC", 2, 512)

        pch, pch_b = self.K["pch"]
        half, half_b = self.K["half"]
        mhalf, mhalf_b = self.K["mhalf"]

        def shift(dst, dst_b, X, X_b, mucol, npart=128, eng="pool"):
            tmp, tmp_b = R("shtmp", 2)
            S.op("act", lambda e: e.activation(out=dst[0:npart, :], in_=X[0:npart, 1:TH + 1], func=AF.Copy,
                                               scale=col(pc1m, mucol)[0:npart]),
                 reads=[X_b, pc1m_b], writes=[dst_b])
            S.op("act", lambda e: e.activation(out=tmp[0:npart, :], in_=X[0:npart, 0:TH], func=AF.Copy,
                                               scale=col(pc, mucol)[0:npart]),
                 reads=[X_b, pc_b], writes=[tmp_b])
            S.op("pool", lambda e: e.tensor_tensor(out=dst[0:npart, :], in0=dst[0:npart, :], in1=tmp[0:npart, :],
                                                   op=ALU.add), reads=[dst_b, tmp_b], writes=[dst_b])

        def sigm(dst, dst_b, src, src_b, bcol):
            S.op("act", lambda e: e.activation(out=dst[:], in_=src, func=AF.Tanh, scale=0.5, bias=col(pch, bcol)),
                 reads=[src_b, pch_b], writes=[dst_b])
            S.op("act", lambda e: e.activation(out=dst[:], in_=dst[:], func=AF.Identity, scale=0.5, bias=half[:, 0:1]),
                 reads=[dst_b, half_b], writes=[dst_b])

        def load_shifted(name, row0, nrows, t0, q):
            X, X_b = R("X" + name, 2, (128, TH + 1))
            if t0 == 0:
                S.op("pool", lambda e: e.memset(X[0:nrows, 0:1], 0.0), writes=[X_b])
                S.dma(q, X[0:nrows, 1:TH + 1], projT[row0:row0 + nrows, 0:TH], reads=[projT_b], writes=[X_b])
            else:
                S.dma(q, X[0:nrows, :], projT[row0:row0 + nrows, t0 - 1:t0 + TH], reads=[projT_b], writes=[X_b])
            return X, X_b

        def prepA(j, tb, par):
            t0 = tb * TH
            ctx = {}
            Wn, PTr, NbTr = Wn2[par], PTr2[par], NbTr2[par]
            Xr, Xr_b = load_shifted("r", C_RW_R + j * 128, 128, t0, "sp")
            Xk, Xk_b = load_shifted("k", C_RW_K + j * 128, 128, t0, "pool")
            Xv, Xv_b = load_shifted("v", C_RW_V + j * 128, 128, t0, "sp")
            Xw, Xw_b = load_shifted("w", C_RW_WD, 128, t0, "pool")
            Xz, Xz_b = R("Xz", 4)
            S.dma("sp", Xz[:], projT[C_RW_Z + j * 128:C_RW_Z + (j + 1) * 128, t0:t0 + TH],
                  reads=[projT_b], writes=[Xz_b])
            yield
            rs, rs_b = R("rs")
            ks, ks_b = R("ks")
            vs, vs_b = R("vs", 2)
            was, was_b = R("was")
            shift(rs, rs_b, Xr, Xr_b, PC_MU + j)
            shift(ks, ks_b, Xk, Xk_b, PC_MU + 4 + j)
            yield
            shift(vs, vs_b, Xv, Xv_b, PC_MU + 8 + j, eng="pool")
            shift(was, was_b, Xw, Xw_b, PC_MU + 12, eng="pool")
            yield
            S.op("act", lambda e: e.activation(out=was[0:64, :], in_=was[0:64, :], func=AF.Tanh),
                 reads=[was_b], writes=[was_b])
            pz, pz_b = psC.next()
            S.op("pe", lambda e: e.matmul(pz[:, 0:TH], lhsT=wau[0:64, j * 128:(j + 1) * 128], rhs=was[0:64, :],
                                          start=True, stop=True), reads=[wau_b, was_b], writes=[pz_b])
            sg, sg_b = R("sg")
            sigm(sg, sg_b, pz[:, 0:TH], pz_b, PC_W0 + j)
            pa, pa_b = psC.next()
            S.op("pe", lambda e: e.matmul(pa[:, 0:TH], lhsT=wau[64:128, j * 128:(j + 1) * 128],
                                          rhs=was[64:128, :], start=True, stop=True),
                 reads=[wau_b, was_b], writes=[pa_b])
            aic, aic_b = R("aic")
            sigm(aic, aic_b, pa[:, 0:TH], pa_b, PC_A0 + j)
            yield
            if l == 0:
                vr, vr_b = vs, vs_b
                S.dma("pool", vfirst[j * 128:(j + 1) * 128, t0:t0 + TH], vs[:], reads=[vs_b], writes=[vfirst_b])
            else:
                Xm, Xm_b = load_shifted("m", DIN, 32, t0, "sp")
                vms, vms_b = R("vms")
                shift(vms, vms_b, Xm, Xm_b, PC_VMU, npart=32, eng="pool")
                pv, pv_b = psC.next()
                S.op("pe", lambda e: e.matmul(pv[:, 0:TH], lhsT=vmu[0:32, j * 128:(j + 1) * 128],
                                              rhs=vms[0:32, :], start=True, stop=True),
                     reads=[vmu_b, vms_b], writes=[pv_b])
                gt, gt_b = R("gt")
                sigm(gt, gt_b, pv[:, 0:TH], pv_b, PC_VM0 + j)
                vf, vf_b = R("vf")
                S.dma("pool", vf[:], vfirst[j * 128:(j + 1) * 128, t0:t0 + TH], reads=[vfirst_b], writes=[vf_b])
                S.op("pool", lambda e: e.tensor_tensor(out=vf[:], in0=vf[:], in1=vs[:], op=ALU.subtract),
                     reads=[vf_b, vs_b], writes=[vf_b])
                S.op("pool", lambda e: e.tensor_tensor(out=vf[:], in0=vf[:], in1=gt[:], op=ALU.mult),
                     reads=[vf_b, gt_b], writes=[vf_b])
                vr, vr_b = R("vr", 2)
                S.op("pool", lambda e: e.tensor_tensor(out=vr[:], in0=vf[:], in1=vs[:], op=ALU.add),
                     reads=[vf_b, vs_b], writes=[vr_b])
            yield
            kk, kk_b = R("kk")
            S.op("act", lambda e: e.activation(out=kk[:], in_=ks[:], func=AF.Copy, scale=col(pc, PC_KK + j)),
                 reads=[ks_b, pc_b], writes=[kk_b])
            sq, sq_b = R("sq")
            S.op("pool", lambda e: e.tensor_tensor(out=sq[:], in0=kk[:], in1=kk[:], op=ALU.mult),
                 reads=[kk_b], writes=[sq_b])
            pq, pq_b = psC.next()
            S.op("pe", lambda e: e.matmul(pq[:, 0:TH], lhsT=blk64[:], rhs=sq[:], start=True, stop=True),
                 reads=[blk64_b, sq_b], writes=[pq_b])
            rn, rn_b = R("rn")
            S.op("dve", lambda e: e.tensor_scalar_max(out=rn[:], in0=pq[:, 0:TH], scalar1=1e-24),
                 reads=[pq_b], writes=[rn_b])
            S.op("dve", lambda e: e.reciprocal(out=rn[:], in_=rn[:]), reads=[rn_b], writes=[rn_b])
            bv, bv_b = R("bv")
            S.op("pool", lambda e: e.tensor_tensor(out=bv[:], in0=kk[:], in1=aic[:], op=ALU.mult),
                 reads=[kk_b, aic_b], writes=[bv_b])
            S.op("pool", lambda e: e.tensor_tensor(out=kk[:], in0=kk[:], in1=rn[:], op=ALU.mult),
                 reads=[kk_b, rn_b], writes=[kk_b])
            yield
            km, km_b = R("km")
            S.op("dve", lambda e: e.tensor_scalar(out=km[:], in0=aic[:], scalar1=col(pc, PC_KA + j),
                                                  scalar2=col(pc1m, PC_KA + j), op0=ALU.mult, op1=ALU.add),
                 reads=[aic_b, pc_b, pc1m_b], writes=[km_b])
            S.op("dve", lambda e: e.tensor_tensor(out=km[:], in0=km[:], in1=ks[:], op=ALU.mult),
                 reads=[km_b, ks_b], writes=[km_b])
            S.op("dve", lambda e: e.scalar_tensor_tensor(out=sq[:], in0=rs[:], scalar=col(pc, PC_RK + j),
                                                         in1=km[:], op0=ALU.mult, op1=ALU.mult),
                 reads=[rs_b, pc_b, km_b, sq_b], writes=[sq_b])
            pb, pb_b = psC.next()
            S.op("pe", lambda e: e.matmul(pb[:, 0:TH], lhsT=blk64[:], rhs=sq[:], start=True, stop=True),
                 reads=[blk64_b, sq_b], writes=[pb_b])
            bon, bon_b = R("bon", 4)
            S.op("dve", lambda e: e.tensor_tensor(out=bon[:], in0=pb[:, 0:TH], in1=vr[:], op=ALU.mult),
                 reads=[pb_b, vr_b], writes=[bon_b])
            yield
            G, G_b = R("G")
            S.op("dve", lambda e: e.tensor_tensor_scan(out=G[:], data0=sg[:], data1=sg[:], initial=0.0,
                                                       op0=ALU.add, op1=ALU.bypass), reads=[sg_b], writes=[G_b])
            Gs, Gs_b = R("Gs", 1, (128, NCH))
            S.op("pool", lambda e: e.memset(Gs[:, 0:1], 0.0), writes=[Gs_b])
            G3 = G[:, :].rearrange("p (c t) -> p c t", t=CH)
            S.op("dve", lambda e: e.tensor_copy(out=Gs[:, 1:NCH], in_=G3[:, 0:NCH - 1, CH - 1]),
                 reads=[G_b], writes=[Gs_b])
            csp, csp_b = R("csp")
            csp3 = csp[:, :].rearrange("p (c t) -> p c t", t=CH)
            S.op("dve", lambda e: e.tensor_tensor(out=csp3, in0=G3,
                                                  in1=Gs[:, :].unsqueeze(2).to_broadcast([128, NCH, CH]),
                                                  op=ALU.subtract), reads=[G_b, Gs_b], writes=[csp_b])
            Ep, Ep_b = R("Ep", 4)
            Em, Em_b = R("Em")
            Eq, Eq_b = R("Eq")
            S.op("act", lambda e: e.activation(out=Ep[:], in_=csp[:], func=AF.Exp, scale=-C0),
                 reads=[csp_b], writes=[Ep_b])
            S.op("act", lambda e: e.activation(out=Em[:], in_=csp[:], func=AF.Exp, scale=C0),
                 reads=[csp_b], writes=[Em_b])
            S.op("pool", lambda e: e.tensor_tensor(out=Eq[:], in0=csp[:], in1=sg[:], op=ALU.subtract),
                 reads=[csp_b, sg_b], writes=[Eq_b])
            S.op("act", lambda e: e.activation(out=Eq[:], in_=Eq[:], func=AF.Exp, scale=-C0),
                 reads=[Eq_b], writes=[Eq_b])
            yield
            Ep3 = Ep[:, :].rearrange("p (c t) -> p c t", t=CH)
            AR, AR_b = ARr.next()
            BK, BK_b = BKr.next()
            BV, BV_b = BVr.next()

            def v3(t_, p):
                return t_[p * 64:(p + 1) * 64, :].rearrange("p (c t) -> p c t", t=CH)

            for p in range(2):
                hs = slice(p * 64, (p + 1) * 64)
                S.op("dve", lambda e: e.scalar_tensor_tensor(out=AR[hs, :, 0, p, :], in0=v3(kk, p), scalar=-1.0,
                                                             in1=v3(Eq, p), op0=ALU.mult, op1=ALU.mult),
                     reads=[kk_b, Eq_b], writes=[AR_b])
                S.op("dve", lambda e: e.tensor_tensor(out=AR[hs, :, 1, p, :], in0=v3(rs, p), in1=v3(Ep, p),
                                                      op=ALU.mult), reads=[rs_b, Ep_b], writes=[AR_b])
                S.op("dve", lambda e: e.tensor_tensor(out=BK[hs, :, 0, p, :], in0=v3(bv, p), in1=v3(Em, p),
                                                      op=ALU.mult), reads=[bv_b, Em_b], writes=[BK_b])
                S.op("dve", lambda e: e.tensor_tensor(out=BK[hs, :, 1, p, :], in0=v3(km, p), in1=v3(Em, p),
                                                      op=ALU.mult), reads=[km_b, Em_b], writes=[BK_b])
                gcb = Ep3[hs, :, CH - 1:CH].to_broadcast([64, NCH, CH])
                S.op("dve", lambda e: e.tensor_tensor(out=BV[hs, :, 0, p, :], in0=BK[hs, :, 0, p, :], in1=gcb,
                                                      op=ALU.mult), reads=[BK_b, Ep_b], writes=[BV_b])
                S.op("dve", lambda e: e.tensor_tensor(out=BV[hs, :, 1, p, :], in0=BK[hs, :, 1, p, :], in1=gcb,
                                                      op=ALU.mult), reads=[BK_b, Ep_b], writes=[BV_b])
                S.op("dve", lambda e: e.tensor_copy(out=BV[hs, :, 2, p, :], in_=v3(vr, p)),
                     reads=[vr_b], writes=[BV_b])
                yield
            yield "A"
            ch = []
            for c in range(NCH):
                ARc = AR[:, c].rearrange("p a q t -> p (a q t)")
                d = {"ARc": ARc, "Abd": ARc[:, 0:128], "Rbd": ARc[:, 128:256],
                     "Bbd": BK[:, c, 0].rearrange("p q t -> p (q t)"),
                     "Kbd": BK[:, c, 1].rearrange("p q t -> p (q t)")}
                ch.append(d)
            for d in ch:
                p1, p1_b = psA.next()
                S.op("pe", lambda e: e.matmul(p1[:, 0:256], lhsT=d["Bbd"], rhs=d["ARc"], start=True, stop=True),
                     reads=[BK_b, AR_b], writes=[p1_b])
                d["NM1"], d["NM1_b"] = NM1r.next()
                S.op("dve", lambda e: e.tensor_tensor(out=d["NM1"][:], in0=p1[:, 0:256], in1=mask2[:], op=ALU.mult),
                     reads=[p1_b, mask2_b], writes=[d["NM1_b"]])
            yield
            for d in ch:
                p2, p2_b = psA.next()
                S.op("pe", lambda e: e.matmul(p2[:, 0:256], lhsT=d["Kbd"], rhs=d["ARc"], start=True, stop=True),
                     reads=[BK_b, AR_b], writes=[p2_b])
                d["NM2"], d["NM2_b"] = NM2r.next()
                S.op("dve", lambda e: e.tensor_tensor(out=d["NM2"][:], in0=p2[:, 0:256], in1=mask2[:], op=ALU.mult),
                     reads=[p2_b, mask2_b], writes=[d["NM2_b"]])
            yield
            for d in ch:
                p3, p3_b = psB.next()
                S.op("pe", lambda e: e.matmul(p3[:, :], lhsT=d["Abd"], rhs=d["Bbd"], start=True, stop=True),
                     reads=[AR_b, BK_b], writes=[p3_b])
                d["NbT"], d["NbT_b"] = NbTr.next()
                S.op("dve", lambda e: e.tensor_tensor(out=d["NbT"][:], in0=p3[:, :], in1=maskl[:], op=ALU.mult),
                     reads=[p3_b, maskl_b], writes=[d["NbT_b"]])
            yield
            for d in ch:
                Nba = d["NM1"][:, 0:128]
                d["W"], d["W_b"] = Wn.next()
                W = d["W"]
                S.op("pool", lambda e: e.tensor_tensor(out=W[:, 2, :], in0=Nba, in1=idr[:], op=ALU.add),
                     reads=[d["NM1_b"], idr_b], writes=[d["W_b"]])
                p4, p4_b = psB.next()
                S.op("pe", lambda e: e.matmul(p4[:, :], lhsT=d["NbT"][:], rhs=Nba, start=True, stop=True),
                     reads=[d["NbT_b"], d["NM1_b"]], writes=[p4_b])
                S.op("act", lambda e: e.activation(out=W[:, 0, :], in_=p4[:, :], func=AF.Copy),
                     reads=[p4_b], writes=[d["W_b"]])
                p5, p5_b = psB.next()
                S.op("pe", lambda e: e.matmul(p5[:, :], lhsT=Nba, rhs=d["NbT"][:], start=True, stop=True),
                     reads=[d["NbT_b"], d["NM1_b"]], writes=[p5_b])
                d["PT"], d["PT_b"] = PTr.next()
                PT = d["PT"]
                S.op("act", lambda e: e.activation(out=PT[:], in_=p5[:, :], func=AF.Copy),
                     reads=[p5_b], writes=[d["PT_b"]])
            yield
            for m in (2, 4, 8, 16, 32):
                for d in ch:
                    W, W_b, PT, PT_b = d["W"], d["W_b"], d["PT"], d["PT_b"]
                    pw, pw_b = psA.next()
                    S.op("pe", lambda e: e.matmul(pw[:, 0:256].rearrange("p (a b) -> p a b", a=2), lhsT=PT[:],
                                                  rhs=W[:, 0:3:2, :], start=True, stop=True),
                         reads=[PT_b, W_b], writes=[pw_b])
                    if m < 32:
                        W2, W2_b = Wn.next()
                        S.op("dve", lambda e: e.tensor_tensor(out=W2[:, 0:3:2, :],
                                                              in0=pw[:, 0:256].rearrange("p (a b) -> p a b", a=2),
                                                              in1=W[:, 1:3, :], op=ALU.add),
                             reads=[pw_b, W_b], writes=[W2_b])
                        pq2, pq2_b = psB.next()
                        S.op("pe", lambda e: e.matmul(pq2[:, :], lhsT=W[:, 0, :], rhs=PT[:], start=True, stop=True),
                             reads=[W_b, PT_b], writes=[pq2_b])
                        PT2, PT2_b = PTr.next()
                        S.op("act", lambda e: e.activation(out=PT2[:], in_=pq2[:, :], func=AF.Copy),
                             reads=[pq2_b], writes=[PT2_b])
                        d["W"], d["W_b"], d["PT"], d["PT_b"] = W2, W2_b, PT2, PT2_b
                    else:
                        d["Tf"], d["Tf_b"] = Tfr.next()
                        Tf = d["Tf"]
                        S.op("dve", lambda e: e.tensor_tensor(out=Tf[:], in0=pw[:, 128:256], in1=W[:, 2, :], op=ALU.add),
                             reads=[pw_b, W_b], writes=[d["Tf_b"]])
                yield
            for c, d in enumerate(ch):
                pt3, pt3_b = psC.next()
                for i in range(3):
                    S.op("pe", lambda e: e.matmul(pt3[:, i * 128:(i + 1) * 128],
                                                  lhsT=BV[:, c, i].rearrange("p q t -> p (q t)"), rhs=idr[:],
                                                  start=True, stop=True),
                         reads=[BV_b, idr_b], writes=[pt3_b])
                d["TM3"], d["TM3_b"] = TM3r.next()
                TM3 = d["TM3"]
                S.op("act", lambda e: e.activation(out=TM3[:].rearrange("p a b -> p (a b)"), in_=pt3[:, 0:384],
                                                   func=AF.Copy), reads=[pt3_b], writes=[d["TM3_b"]])
            yield
            ctx.update(ch=ch, AR_b=AR_b, Ep=Ep, Ep_b=Ep_b, bon=bon, bon_b=bon_b, Xz=Xz, Xz_b=Xz_b, j=j, t0=t0)
            return ctx

        def stageB(ctx, state):
            ch, AR_b, Ep, Ep_b = ctx["ch"], ctx["AR_b"], ctx["Ep"], ctx["Ep_b"]
            j, t0 = ctx["j"], ctx["t0"]
            ob, ob_b = R("ob", 2)
            for c, d in enumerate(ch):
                Scur, Scur_b = state["S"], state["S_b"]
                Nka, Mkr = d["NM2"][:, 0:128], d["NM2"][:, 128:256]
                Mbr = d["NM1"][:, 128:256]
                TM3, TM3_b, Tf, Tf_b = d["TM3"], d["TM3_b"], d["Tf"], d["Tf_b"]
                BpT, KpT, VT = TM3[:, 0, :], TM3[:, 1, :], TM3[:, 2, :]
                pw1, pw1_b = psB.next()
                S.op("pe", lambda e: e.matmul(pw1[:, :], lhsT=d["Abd"], rhs=Scur[:], start=True, stop=False),
                     reads=[AR_b, Scur_b], writes=[pw1_b])
                S.op("pe", lambda e: e.matmul(pw1[:, :], lhsT=Nka, rhs=VT, start=False, stop=True),
                     reads=[d["NM2_b"], TM3_b], writes=[pw1_b])
                W1, W1_b = W1r.next()
                S.op("act", lambda e: e.activation(out=W1[:], in_=pw1[:, :], func=AF.Copy),
                     reads=[pw1_b], writes=[W1_b])
                yield
                pu, pu_b = psB.next()
                S.op("pe", lambda e: e.matmul(pu[:, :], lhsT=Tf[:], rhs=W1[:], start=True, stop=True),
                     reads=[Tf_b, W1_b], writes=[pu_b])
                UT, UT_b = UTr.next()
                S.op("dve", lambda e: e.tensor_copy(out=UT[:], in_=pu[:, :]), reads=[pu_b], writes=[UT_b])
                yield
                ps2, ps2_b = psB.next()
                S.op("pe", lambda e: e.matmul(ps2[:, :], lhsT=BpT, rhs=UT[:], start=True, stop=False),
                     reads=[TM3_b, UT_b], writes=[ps2_b])
                S.op("pe", lambda e: e.matmul(ps2[:, :], lhsT=KpT, rhs=VT, start=False, stop=True),
                     reads=[TM3_b], writes=[ps2_b])
                Snx, Snx_b = Sr.next()
                gc = Ep[:, c * CH + CH - 1:c * CH + CH]
                S.op("dve", lambda e: e.scalar_tensor_tensor(out=Snx[:], in0=Scur[:], scalar=gc, in1=ps2[:, :],
                                                             op0=ALU.mult, op1=ALU.add),
                     reads=[Scur_b, Ep_b, ps2_b], writes=[Snx_b])
                po, po_b = psB.next()
                S.op("pe", lambda e: e.matmul(po[:, :], lhsT=Scur[:], rhs=d["Rbd"], start=True, stop=False),
                     reads=[Scur_b, AR_b], writes=[po_b])
                S.op("pe", lambda e: e.matmul(po[:, :], lhsT=UT[:], rhs=Mbr, start=False, stop=False),
                     reads=[UT_b, d["NM1_b"]], writes=[po_b])
                S.op("pe", lambda e: e.matmul(po[:, :], lhsT=VT, rhs=Mkr, start=False, stop=True),
                     reads=[TM3_b, d["NM2_b"]], writes=[po_b])
                for p in range(2):
                    hs = slice(p * 64, (p + 1) * 64)
                    S.op("act", lambda e: e.activation(out=ob[hs, c * CH:(c + 1) * CH],
                                                       in_=po[hs, p * 64:(p + 1) * 64], func=AF.Copy),
                         reads=[po_b], writes=[ob_b])
                state["S"], state["S_b"] = Snx, Snx_b
                yield
            bon, bon_b, Xz, Xz_b = ctx["bon"], ctx["bon_b"], ctx["Xz"], ctx["Xz_b"]
            pm, pm_b = psC.next()
            S.op("pe", lambda e: e.matmul(pm[:, 0:TH], lhsT=blk64[:], rhs=ob[:], start=True, stop=True),
                 reads=[blk64_b, ob_b], writes=[pm_b])
            dd, dd_b = R("dd")
            S.op("dve", lambda e: e.scalar_tensor_tensor(out=dd[:], in0=pm[:, 0:TH], scalar=-1.0 / 64, in1=ob[:],
                                                         op0=ALU.mult, op1=ALU.add),
                 reads=[pm_b, ob_b], writes=[dd_b])
            sq2, sq2_b = R("sq2")
            S.op("pool", lambda e: e.tensor_tensor(out=sq2[:], in0=dd[:], in1=dd[:], op=ALU.mult),
                 reads=[dd_b], writes=[sq2_b])
            pvv, pvv_b = psC.next()
            S.op("pe", lambda e: e.matmul(pvv[:, 0:TH], lhsT=blk64[:], rhs=sq2[:], start=True, stop=True),
                 reads=[blk64_b, sq2_b], writes=[pvv_b])
            rn2, rn2_b = R("rn2")
            S.op("dve", lambda e: e.tensor_scalar(out=rn2[:], in0=pvv[:, 0:TH], scalar1=1.0 / 64, scalar2=GN_EPS,
                                                  op0=ALU.mult, op1=ALU.add), reads=[pvv_b], writes=[rn2_b])
            S.op("act", lambda e: e.activation(out=rn2[:], in_=rn2[:], func=AF.Sqrt), reads=[rn2_b], writes=[rn2_b])
            S.op("dve", lambda e: e.reciprocal(out=rn2[:], in_=rn2[:]), reads=[rn2_b], writes=[rn2_b])
            yield
            S.op("pool", lambda e: e.tensor_tensor(out=dd[:], in0=dd[:], in1=rn2[:], op=ALU.mult),
                 reads=[dd_b, rn2_b], writes=[dd_b])
            S.op("dve", lambda e: e.tensor_scalar(out=dd[:], in0=dd[:], scalar1=col(pc, PC_LNG + j),
                                                  scalar2=col(pc, PC_LNB + j), op0=ALU.mult, op1=ALU.add),
                 reads=[dd_b, pc_b], writes=[dd_b])
            S.op("pool", lambda e: e.tensor_tensor(out=dd[:], in0=dd[:], in1=bon[:], op=ALU.add),
                 reads=[dd_b, bon_b], writes=[dd_b])
            th, th_b = R("th")
            S.op("act", lambda e: e.activation(out=th[:], in_=Xz[:], func=AF.Tanh, scale=0.5), reads=[Xz_b], writes=[th_b])
            S.op("dve", lambda e: e.scalar_tensor_tensor(out=th[:], in0=th[:], scalar=1.0, in1=Xz[:],
                                                         op0=ALU.add, op1=ALU.mult),
                 reads=[th_b, Xz_b], writes=[th_b])
            yb, yb_b = R("yb", 2, (128, TH), BF16)
            S.op("dve", lambda e: e.scalar_tensor_tensor(out=yb[:], in0=dd[:], scalar=0.5, in1=th[:],
                                                         op0=ALU.mult, op1=ALU.mult),
                 reads=[dd_b, th_b], writes=[yb_b])
            S.dma("sp", ybT[j * 128:(j + 1) * 128, t0:t0 + TH], yb[:], reads=[yb_b], writes=[ybT_b])
            yield

        blocks = [(j, tb) for j in range(4) for tb in range(T // TH)]
        state = {}
        nxt = 0
        gP = gB = None
        gAs = []
        gP_waiting = False
        bq = []
        while nxt < len(blocks) or gP is not None or gAs or gB is not None or bq:
            if gP is None and nxt < len(blocks):
                gP = prepA(blocks[nxt][0], blocks[nxt][1], nxt % 2)
                nxt += 1
                gP_waiting = False
            if gB is None and bq:
                ctx = bq.pop(0)
                if ctx["t0"] == 0:
                    S0, S0_b = Sr.next()
                    S.op("dve", lambda e: e.tensor_copy(out=S0[:], in_=zf[:, 0:1].to_broadcast([128, 128])),
                         reads=[zf_b], writes=[S0_b])
                    state["S"], state["S_b"] = S0, S0_b
                gB = stageB(ctx, state)
            if gB is not None:
                try:
                    next(gB)
                except StopIteration:
                    gB = None
            for g in list(gAs):
                try:
                    next(g)
                except StopIteration as st:
                    assert g is gAs[0]
                    bq.append(st.value)
                    gAs.remove(g)
            if gP is not None:
                if not gP_waiting:
                    if next(gP) == "A":
                        gP_waiting = True
                if gP_waiting and len(gAs) < 2 and len(gAs) + len(bq) + (1 if gB is not None else 0) <= 2:
                    gAs.append(gP)
                    gP, gP_waiting = None, False
            yield

    def phase3(self, l):
        with ExitStack() as es:
            for _ in self.phase3_gen(l, es):
                pass

    def phase4(self, l, xin, xin_b, xo, xo_b):
        nc, S = self.nc, self.S
        projT, projT_b = self.scr["projT"], self.dbuf["projT"]
        yagT, yagT_b = self.scr["yagT"], self.dbuf["yagT"]
        ybT, ybT_b = self.scr["ybT"], self.dbuf["ybT"]
        with ExitStack() as es:
            wst = Ring(es, nc, "p4_wst", 2, [128, 4, D], F32)
            wua, wua_b = sb(es, nc, "p4_wua", [128, 4, D], BF16)
            wur, wur_b = sb(es, nc, "p4_wur", [128, 4, D], BF16)
            wo, wo_b = sb(es, nc, "p4_wo", [128, 8, D], BF16)
            srcs = [(self.inp["w_up_att"][l].rearrange("(c p) e -> p c e", p=128), wua[:, :, :], wua_b),
                    (self.inp["w_up_rw"][l].rearrange("(c p) e -> p c e", p=128), wur[:, :, :], wur_b),
                    (self.inp["w_out"][l].rearrange("(c p) e -> p c e", p=128)[:, 0:4, :], wo[:, 0:4, :], wo_b),
                    (self.inp["w_out"][l].rearrange("(c p) e -> p c e", p=128)[:, 4:8, :], wo[:, 4:8, :], wo_b)]
            for i, (src, dst, dst_b) in enumerate(srcs):
                ws, ws_b = wst.next()
                S.dma("sp" if i % 2 == 0 else "pool", ws[:], src, writes=[ws_b])
                S.op("pool" if i % 2 == 0 else "dve", lambda e: e.tensor_copy(out=dst, in_=ws[:]),
                     reads=[ws_b], writes=[dst_b])
            gpost, gpost_b = sb(es, nc, "p4_gpost", [128, D], F32)
            S.dma("sp", gpost[:], self.inp["norm_post"][l].partition_broadcast(128), writes=[gpost_b])
            ya, ya_b = sb(es, nc, "p4_ya", [128, 4, T], BF16)
            yb, yb_b = sb(es, nc, "p4_yb", [128, 4, T], BF16)
            S.dma("sp", ya[:], yagT.rearrange("(c p) t -> p c t", p=128), reads=[yagT_b], writes=[ya_b])
            S.dma("pool", yb[:], ybT.rearrange("(c p) t -> p c t", p=128), reads=[ybT_b], writes=[yb_b])
            uTr = Ring(es, nc, "p4_uT", 2, [128, 8, 512], BF16)
            gAr = Ring(es, nc, "p4_gA", 2, [128, 512], F32)
            gRr = Ring(es, nc, "p4_gR", 2, [128, 512], F32)
            t1r = Ring(es, nc, "p4_t1", 2, [128, 512], F32)
            t2r = Ring(es, nc, "p4_t2", 2, [128, 512], F32)
            junk, junk_b = sb(es, nc, "p4_junk", [128, 512], F32)
            ssr = Ring(es, nc, "p4_ss", 4, [128, 4], F32)
            xtr = Ring(es, nc, "p4_xt", 2, [128, D], F32)
            otr = Ring(es, nc, "p4_ot", 2, [128, D], F32)
            psa = Ring(es, nc, "p4_psa", 2, [128, 512], F32, psum=True)
            psr = Ring(es, nc, "p4_psr", 2, [128, 512], F32, psum=True)
            psy = Ring(es, nc, "p4_psy", 4, [128, 512], F32, psum=True)
            for tc in range(4):
                ts_ = slice(tc * 512, (tc + 1) * 512)
                uT, uT_b = uTr.next()
                for eg in range(8):
                    gA, gA_b = gAr.next()
                    gR, gR_b = gRr.next()
                    S.dma("sp", gA[:], projT[C_G_ATT + eg * 128:C_G_ATT + (eg + 1) * 128, ts_], reads=[projT_b], writes=[gA_b])
                    S.dma("pool", gR[:], projT[C_G_RW + eg * 128:C_G_RW + (eg + 1) * 128, ts_], reads=[projT_b], writes=[gR_b])
                    for (g_, g_b) in ((gA, gA_b), (gR, gR_b)):
                        S.op("act", lambda e: e.activation(out=g_[:], in_=g_[:], func=AF.Tanh, scale=0.5),
                             reads=[g_b], writes=[g_b])
                        S.op("act", lambda e: e.activation(out=g_[:], in_=g_[:], func=AF.Identity, scale=0.5,
                                                           bias=self.K["half"][0][:, 0:1]),
                             reads=[g_b, self.K["half"][1]], writes=[g_b])
                    pa, pa_b = psa.next()
                    pr, pr_b = psr.next()
                    for c in range(4):
                        S.op("pe", lambda e: e.matmul(pa[:, :], lhsT=wua[:, c, eg * 128:(eg + 1) * 128], rhs=ya[:, c, ts_],
                                                      start=(c == 0), stop=(c == 3)), reads=[wua_b, ya_b], writes=[pa_b])
                    for c in range(4):
                        S.op("pe", lambda e: e.matmul(pr[:, :], lhsT=wur[:, c, eg * 128:(eg + 1) * 128], rhs=yb[:, c, ts_],
                                                      start=(c == 0), stop=(c == 3)), reads=[wur_b, yb_b], writes=[pr_b])
                    t1, t1_b = t1r.next()
                    t2, t2_b = t2r.next()
                    S.op("dve", lambda e: e.tensor_tensor(out=t1[:], in0=pa[:, :], in1=gA[:], op=ALU.mult),
                         reads=[pa_b, gA_b], writes=[t1_b])
                    S.op("dve", lambda e: e.tensor_tensor(out=t2[:], in0=pr[:, :], in1=gR[:], op=ALU.mult),
                         reads=[pr_b, gR_b], writes=[t2_b])
                    S.op("pool", lambda e: e.tensor_tensor(out=uT[:, eg, :], in0=t1[:], in1=t2[:], op=ALU.add),
                         reads=[t1_b, t2_b], writes=[uT_b])
                for tt in range(4):
                    tok0 = tc * 512 + tt * 128
                    xt, xt_b = xtr.next()
                    S.dma("sp", xt[:], xin[tok0:tok0 + 128, :], reads=[xin_b], writes=[xt_b])
                    ss, ss_b = ssr.next()
                    pys = []
                    for hf in range(2):
                        py, py_b = psy.next()
                        for eg in range(8):
                            S.op("pe", lambda e: e.matmul(py[:, :], lhsT=uT[:, eg, tt * 128:(tt + 1) * 128],
                                                          rhs=wo[:, eg, hf * 512:(hf + 1) * 512],
                                                          start=(eg == 0), stop=(eg == 7)), reads=[uT_b, wo_b], writes=[py_b])
                        S.op("act", lambda e: e.activation(out=junk[:], in_=py[:, :], func=AF.Square,
                                                           accum_out=ss[:, hf:hf + 1]),
                             reads=[py_b], writes=[junk_b, ss_b])
                        pys.append((py, py_b))
                    S.op("dve", lambda e: e.tensor_tensor(out=ss[:, 2:3], in0=ss[:, 0:1], in1=ss[:, 1:2], op=ALU.add),
                         reads=[ss_b], writes=[ss_b])
                    S.op("dve", lambda e: e.tensor_scalar(out=ss[:, 3:4], in0=ss[:, 2:3], scalar1=1.0 / D, scalar2=RMS_EPS,
                                                          op0=ALU.mult, op1=ALU.add), reads=[ss_b], writes=[ss_b])
                    S.op("pool", lambda e: e.tensor_tensor(out=ss[:, 2:3], in0=ss[:, 3:4], in1=self.K["mhalf"][0][:, 0:1],
                                                           op=ALU.pow), reads=[ss_b, self.K["mhalf"][1]], writes=[ss_b])
                    ot, ot_b = otr.next()
                    for hf in range(2):
                        py, py_b = pys[hf]
                        S.op("dve", lambda e: e.scalar_tensor_tensor(out=ot[:, hf * 512:(hf + 1) * 512], in0=py[:, :],
                                                                     scalar=ss[:, 2:3], in1=gpost[:, hf * 512:(hf + 1) * 512],
                                                                     op0=ALU.mult, op1=ALU.mult),
                             reads=[py_b, ss_b, gpost_b], writes=[ot_b])
                    S.op("pool", lambda e: e.tensor_tensor(out=ot[:], in0=ot[:], in1=xt[:], op=ALU.add),
                         reads=[ot_b, xt_b], writes=[ot_b])
                    S.dma("sp", xo[tok0:tok0 + 128, :], ot[:], reads=[ot_b], writes=[xo_b])


def make_in_maps(inputs, hc=None):
    hc = hc or host_consts()
    pc = pack_params(inputs)
    shared = {}
    for k in ("norm_pre", "norm_post", "w_in", "rw_w_up", "rw_a_up", "rw_vmix_down", "rw_vmix_up",
              "w_up_att", "w_up_rw", "w_out"):
        shared[k] = np.ascontiguousarray(np.asarray(inputs[k], dtype=np.float32))
    shared["pc"] = pc
    for k, v in hc.items():
        shared["c_" + k] = v
    x = np.asarray(inputs["x"], dtype=np.float32)
    maps = []
    for c in range(NCORES):
        m = dict(shared)
        m["x"] = np.ascontiguousarray(x[c])
        maps.append(m)
    return maps


def kernel(**inputs):
    prog = Prog()
    nc = prog.build()
    maps = make_in_maps(inputs)
    res = run_bass_kernel_spmd(nc, maps, core_ids=list(range(NCORES)))
    return np.stack([np.asarray(r["out"], dtype=np.float32) for r in res.results], axis=0)
```

```python
import math
from contextlib import ExitStack
import numpy as np
import ml_dtypes
import concourse.bass as bass
import concourse.mybir as mybir
from concourse.bass_utils import run_bass_kernel_spmd

F32 = mybir.dt.float32
F32R = mybir.dt.float32r
BF16 = mybir.dt.bfloat16
ALU = mybir.AluOpType
AF = mybir.ActivationFunctionType
AX = mybir.AxisListType

D = 1024
T = 2048
DIN = 6272
NL = 2
NCORES = 8
RMS_EPS = 1e-6
GN_EPS = 64e-5
C_ATT_Q, C_ATT_K, C_ATT_V, C_ATT_Z = 0, 512, 1024, 1536
C_RW_R, C_RW_K, C_RW_V, C_RW_WD, C_RW_AD, C_RW_Z = 2048, 2560, 3072, 3584, 3648, 3712
C_G_ATT, C_G_RW = 4224, 5248
PROJ_ROWS = DIN + 32
NEGM = -30000.0
CH = 64
PC_MU, PC_W0, PC_A0, PC_KK, PC_KA, PC_RK, PC_LNG, PC_LNB, PC_VM0, PC_VMU = 0, 13, 17, 21, 25, 29, 33, 37, 41, 45
NPC = 46


class Buf:
    __slots__ = ("name", "w", "rs")

    def __init__(self, name=""):
        self.name = name
        self.w = None
        self.rs = []


class Sched:
    def __init__(self, nc, ndma=10, same_engine_waits=True):
        self.nc = nc
        self.eng = {"pe": nc.tensor, "act": nc.scalar, "dve": nc.vector,
                    "pool": nc.gpsimd, "sp": nc.sync}
        self.stack = []
        self.sem = {}
        self.cnt = {}
        for e in self.eng:
            cm = nc.semaphore("s_" + e)
            self.sem[e] = cm.__enter__()
            self.stack.append(cm)
            self.cnt[e] = 0
        self.dma_sems = {}
        self.dma_rr = {}
        for q in ("sp", "pool", "act"):
            lst = []
            for i in range(ndma):
                key = "d_%s%d" % (q, i)
                cm = nc.semaphore(key)
                self.sem[key] = cm.__enter__()
                self.stack.append(cm)
                self.cnt[key] = 0
                lst.append(key)
            self.dma_sems[q] = lst
            self.dma_rr[q] = 0
        self.seen = {e: {} for e in self.eng}
        self.same = same_engine_waits
        self.n_wait = 0
        self.n_ins = 0

    def _need(self, e, needs, ev):
        if ev is None:
            return
        k, v = ev
        if k == e and (e == "pe" or not self.same):
            return
        if self.seen[e].get(k, 0) >= v:
            return
        if needs.get(k, 0) < v:
            needs[k] = v

    def _collect(self, e, reads, writes):
        needs = {}
        for b in reads:
            self._need(e, needs, b.w)
        for b in writes:
            self._need(e, needs, b.w)
            for r in b.rs:
                self._need(e, needs, r)
        return needs

    def _emit_waits(self, e, needs):
        eng = self.eng[e]
        for k, v in needs.items():
            eng.wait_ge(self.sem[k], v)
            self.seen[e][k] = v
            self.n_wait += 1

    def _commit(self, ev, reads, writes):
        for b in reads:
            b.rs.append(ev)
        for b in writes:
            b.w = ev
            b.rs = []

    def op(self, e, fn, reads=(), writes=()):
        needs = self._collect(e, reads, writes)
        self._emit_waits(e, needs)
        ins = fn(self.eng[e])
        self.cnt[e] += 1
        ins.then_inc(self.sem[e], 1)
        ev = (e, self.cnt[e])
        if e != "pe" and self.same:
            pass
        self._commit(ev, reads, writes)
        self.n_ins += 1
        return ev

    def dma(self, q, out, in_, reads=(), writes=(), **kw):
        lst = self.dma_sems[q]
        key = lst[self.dma_rr[q] % len(lst)]
        self.dma_rr[q] += 1
        needs = self._collect(q, reads, writes)
        if self.cnt[key] > 0:
            self._need(q, needs, (key, self.cnt[key]))
        self._emit_waits(q, needs)
        ins = self.eng[q].dma_start(out=out, in_=in_, **kw)
        self.cnt[key] += 16
        ins.then_inc(self.sem[key], 16)
        ev = (key, self.cnt[key])
        self._commit(ev, reads, writes)
        self.n_ins += 1
        return ev

    def wait_all(self, e, bufs):
        needs = {}
        for b in bufs:
            self._need(e, needs, b.w)
        self._emit_waits(e, needs)

    def barrier(self):
        snap = {k: v for k, v in self.cnt.items() if v > 0}
        for e in self.eng:
            needs = {}
            for k, v in snap.items():
                if k == e:
                    continue
                if self.seen[e].get(k, 0) < v:
                    needs[k] = v
            self._emit_waits(e, needs)

    def close(self):
        for cm in reversed(self.stack):
            cm.__exit__(None, None, None)


_UID = [0]


def _uname(name):
    _UID[0] += 1
    return "%s_%d" % (name, _UID[0])


class Ring:
    def __init__(self, es, nc, name, n, shape, dtype, psum=False):
        self.items = []
        name = _uname(name)
        for i in range(n):
            if psum:
                t = es.enter_context(nc.psum_tensor("%s%d" % (name, i), shape, dtype))
            else:
                t = es.enter_context(nc.sbuf_tensor("%s%d" % (name, i), shape, dtype))
            self.items.append((t, Buf(name + str(i))))
        self.i = 0

    def next(self):
        it = self.items[self.i % len(self.items)]
        self.i += 1
        return it


class PsumRing:
    def __init__(self, es, nc, name, n, width):
        per = 512 // width
        nb = (n + per - 1) // per
        name = _uname(name)
        self.items = []
        for b in range(nb):
            t = es.enter_context(nc.psum_tensor("%s_%d" % (name, b), [128, 512], F32))
            for k in range(per):
                if len(self.items) < n:
                    self.items.append((t[:, k * width:(k + 1) * width], Buf("%s_%d_%d" % (name, b, k))))
        self.i = 0

    def next(self):
        it = self.items[self.i % len(self.items)]
        self.i += 1
        return it


def sb(es, nc, name, shape, dtype):
    name = _uname(name)
    return es.enter_context(nc.sbuf_tensor(name, shape, dtype)), Buf(name)


def host_consts():
    bf = ml_dtypes.bfloat16
    c = {}
    c["ident_f"] = np.eye(128, dtype=np.float32)
    c["ident_b"] = np.eye(128).astype(bf)
    kk, qq = np.meshgrid(np.arange(128), np.arange(128), indexing="ij")
    c["trimask"] = np.where(kk > qq, NEGM, 0.0).astype(bf)
    half = np.arange(128) // 64
    same = (half[:, None] == half[None, :])
    c["blk64"] = same.astype(np.float32)
    loc = np.arange(128) % 64
    su = same & (loc[:, None] < loc[None, :])
    iu = same & (loc[:, None] <= loc[None, :])
    c["mask2"] = np.concatenate([su, iu], axis=1).astype(np.float32)
    c["maskl"] = (same & (loc[:, None] > loc[None, :])).astype(np.float32)
    gs = np.zeros((64, 8, 72), np.float32)
    for n in range(8):
        for m in range(8):
            for qb in range(8):
                if m < qb:
                    gs[n * 8 + m, qb, 64 + n] = 1.0
    c["gsum"] = gs.astype(bf)
    thr = np.zeros((128, 8), np.float32)
    for n in range(8):
        for qb in range(8):
            thr[64 + n, qb] = 2.5 if n < qb else (1e9 if n == qb else -1.0)
    c["thr"] = thr
    pos = np.arange(T)
    hi, lo = pos // 128, pos % 128
    c["qconst"] = np.stack([-128.0 * hi, -1.0 * lo, np.ones(T), np.ones(T)]).astype(bf)
    kc = np.zeros((8, 12, T), np.float32)
    for h in range(8):
        sl = 2.0 ** (-(h + 1))
        for n in range(8):
            kc[h, n] = np.where(pos // 256 == n, NEGM, 0.0)
        kc[h, 8] = sl
        kc[h, 9] = sl
        kc[h, 10] = sl * 128.0 * hi
        kc[h, 11] = sl * lo
    c["kconst"] = kc.astype(bf)
    return c


def pack_params(inp):
    out = np.zeros((NL, 128, NPC), np.float32)

    def put(l, col, vec):
        n = vec.shape[0]
        if n % 128 == 0:
            out[l, :, col:col + n // 128] = vec.reshape(n // 128, 128).T
        else:
            out[l, :n, col] = vec

    for l in range(NL):
        put(l, PC_MU, inp["rw_mu"][l])
        put(l, PC_W0, inp["rw_w0"][l])
        put(l, PC_A0, inp["rw_a0"][l])
        put(l, PC_KK, inp["rw_k_k"][l])
        put(l, PC_KA, inp["rw_k_a"][l])
        put(l, PC_RK, inp["rw_r_k"][l])
        put(l, PC_LNG, inp["rw_ln_g"][l])
        put(l, PC_LNB, inp["rw_ln_b"][l])
        if l >= 1:
            put(l, PC_VM0, inp["rw_vmix0"][l - 1])
            put(l, PC_VMU, inp["rw_vmix_mu"][l - 1])
    return out


class Prog:
    def __init__(self, dbg=(), nlayers=NL, stop_after=None):
        self.dbg = set(dbg)
        self.nlayers = nlayers
        self.stop_after = stop_after
        nc = bass.Bass("TRN2", target_bir_lowering=False)
        self.nc = nc
        self.S = Sched(nc)
        self.inp = {}
        self.scr = {}
        self.dbuf = {}

    def din(self, name, shape, dtype=F32):
        ap = self.nc.dram_tensor(name, list(shape), dtype, kind="ExternalInput").ap()
        self.inp[name] = ap
        self.dbuf[name] = Buf(name)
        return ap

    def dscr(self, name, shape, dtype=F32):
        kind = "ExternalOutput" if name in self.dbg else "Internal"
        ap = self.nc.dram_tensor(name, list(shape), dtype, kind=kind).ap()
        self.scr[name] = ap
        self.dbuf[name] = Buf(name)
        return ap

    def build(self):
        nc, S = self.nc, self.S
        x = self.din("x", [T, D])
        self.din("norm_pre", [NL, D])
        self.din("norm_post", [NL, D])
        self.din("w_in", [NL, D, DIN])
        self.din("rw_w_up", [NL, 64, 512])
        self.din("rw_a_up", [NL, 64, 512])
        self.din("rw_vmix_down", [1, D, 32])
        self.din("rw_vmix_up", [1, 32, 512])
        self.din("w_up_att", [NL, 512, D])
        self.din("w_up_rw", [NL, 512, D])
        self.din("w_out", [NL, D, D])
        self.din("pc", [NL, 128, NPC])
        hc = host_consts()
        for k, v in hc.items():
            self.din("c_" + k, v.shape, BF16 if v.dtype == ml_dtypes.bfloat16 else F32)
        out = self.nc.dram_tensor("out", [T, D], F32, kind="ExternalOutput").ap()
        self.dbuf["out"] = Buf("out")
        self.dscr("projT", [PROJ_ROWS, T])
        self.dscr("vtm", [T, 520], BF16)
        self.dscr("yagT", [512, T], BF16)
        self.dscr("ybT", [512, T], BF16)
        self.dscr("vfirst", [512, T])
        self.dscr("x1", [T, D])

        with ExitStack() as es:
            self.load_consts(es)
            xin, xin_b = x, self.dbuf["x"]
            for l in range(self.nlayers):
                last = (l == self.nlayers - 1)
                xo, xo_b = (out, self.dbuf["out"]) if last else (self.scr["x1"], self.dbuf["x1"])
                self.phase1(l, xin, xin_b)
                if self.stop_after == (l, 1):
                    break
                S.barrier()
                self.phase2(l)
                if self.stop_after == (l, 2):
                    break
                S.barrier()
                self.phase3(l)
                if self.stop_after == (l, 3):
                    break
                S.barrier()
                self.phase4(l, xin, xin_b, xo, xo_b)
                S.barrier()
                xin, xin_b = xo, xo_b
            S.wait_all("sp", list(self.dbuf.values()))
            S.barrier()
        S.close()
        return nc

    def load_consts(self, es):
        nc, S = self.nc, self.S
        self.K = {}
        for name, shape, dt in (("ident_f", [128, 128], F32), ("ident_b", [128, 128], BF16),
                                ("trimask", [128, 128], BF16), ("blk64", [128, 128], F32),
                                ("mask2", [128, 256], F32), ("maskl", [128, 128], F32),
                                ("thr", [128, 8], F32)):
            t, b = sb(es, nc, "k_" + name, shape, dt)
            S.dma("sp", t[:], self.inp["c_" + name][:, :], writes=[b])
            self.K[name] = (t, b)
        t, b = sb(es, nc, "k_gsum", [64, 8, 72], BF16)
        S.dma("sp", t[:], self.inp["c_gsum"][:, :, :], writes=[b])
        self.K["gsum"] = (t, b)
        t, b = sb(es, nc, "k_ones", [128, 64], F32)
        S.op("pool", lambda e: e.memset(t[:], 1.0), writes=[b])
        self.K["ones"] = (t, b)
        t2, b2 = sb(es, nc, "k_pc", [128, NL, NPC], F32)
        S.dma("sp", t2[:], self.inp["pc"].rearrange("l p c -> p l c"), writes=[b2])
        self.K["pc"] = (t2, b2)
        t3, b3 = sb(es, nc, "k_pc1m", [128, NL, NPC], F32)
        S.op("dve", lambda e: e.tensor_scalar(out=t3[:], in0=t2[:], scalar1=-1.0, scalar2=1.0,
                                              op0=ALU.mult, op1=ALU.add), reads=[b2], writes=[b3])
        self.K["pc1m"] = (t3, b3)
        t4, b4 = sb(es, nc, "k_pch", [128, NL, NPC], F32)
        S.op("dve", lambda e: e.tensor_scalar(out=t4[:], in0=t2[:], scalar1=0.5, scalar2=None, op0=ALU.mult),
             reads=[b2], writes=[b4])
        self.K["pch"] = (t4, b4)
        t5, b5 = sb(es, nc, "k_half", [128, 2], F32)
        S.op("pool", lambda e: e.memset(t5[:], 0.5), writes=[b5])
        self.K["half"] = (t5, b5)
        t6, b6 = sb(es, nc, "k_mhalf", [128, 512], F32)
        S.op("pool", lambda e: e.memset(t6[:], -0.5), writes=[b6])
        self.K["mhalf"] = (t6, b6)

    def phase1(self, l, xin, xin_b):
        nc, S = self.nc, self.S
        projT, projT_b = self.scr["projT"], self.dbuf["projT"]
        vtm, vtm_b = self.scr["vtm"], self.dbuf["vtm"]
        identf, identf_b = self.K["ident_f"]
        with ExitStack() as es:
            gpre, gpre_b = sb(es, nc, "p1_gpre", [128, D], F32)
            S.dma("sp", gpre[:], self.inp["norm_pre"][l].partition_broadcast(128), writes=[gpre_b])
            hT, _ = sb(es, nc, "p1_hT", [128, 8, T], BF16)
            hT_b = [Buf("hT%d" % i) for i in range(16)]
            xr = Ring(es, nc, "p1_x", 2, [128, D], F32)
            hr = Ring(es, nc, "p1_h", 2, [128, D], F32)
            junk, junk_b = sb(es, nc, "p1_junk", [128, D], F32)
            ssr = Ring(es, nc, "p1_ss", 4, [128, 2], F32)
            pst = Ring(es, nc, "p1_pst", 2, [128, 512], F32, psum=True)
            psm = Ring(es, nc, "p1_psm", 4, [128, 512], F32, psum=True)
            wst = Ring(es, nc, "p1_wst", 3, [128, 8, 512], F32)
            wbf = Ring(es, nc, "p1_wbf", 3, [128, 8, 512], BF16)
            w_in = self.inp["w_in"][l].rearrange("(dc p) e -> p dc e", p=128)
            w_in_b = self.dbuf["w_in"]
            blocks = [(c0, min(512, DIN - c0)) for c0 in range(0, DIN, 512)]

            def load_block(bi):
                c0, ncol = blocks[bi]
                ws, ws_b = wst.next()
                S.dma("sp", ws[:, 0:4, 0:ncol], w_in[:, 0:4, c0:c0 + ncol], reads=[w_in_b], writes=[ws_b])
                S.dma("sp", ws[:, 4:8, 0:ncol], w_in[:, 4:8, c0:c0 + ncol], reads=[w_in_b], writes=[ws_b])
                wb, wb_b = wbf.next()
                S.op("dve", lambda e: e.tensor_copy(out=wb[:, 0:4, 0:ncol], in_=ws[:, 0:4, 0:ncol]),
                     reads=[ws_b], writes=[wb_b])
                S.op("act", lambda e: e.activation(out=wb[:, 4:8, 0:ncol], in_=ws[:, 4:8, 0:ncol], func=AF.Copy),
                     reads=[ws_b], writes=[wb_b])
                return wb, wb_b

            pending = [load_block(0), load_block(1)]
            for tt in range(16):
                xt, xt_b = xr.next()
                S.dma("pool", xt[:], xin[tt * 128:(tt + 1) * 128, :],
                      reads=[xin_b], writes=[xt_b])
                ss, ss_b = ssr.next()
                S.op("act", lambda e: e.activation(out=junk[:], in_=xt[:], func=AF.Square,
                                                   accum_out=ss[:, 0:1]),
                     reads=[xt_b], writes=[junk_b, ss_b])
                S.op("dve", lambda e: e.tensor_scalar(out=ss[:, 1:2], in0=ss[:, 0:1], scalar1=1.0 / D,
                                                      scalar2=RMS_EPS, op0=ALU.mult, op1=ALU.add),
                     reads=[ss_b], writes=[ss_b])
                S.op("pool", lambda e: e.tensor_tensor(out=ss[:, 0:1], in0=ss[:, 1:2], in1=self.K["mhalf"][0][:, 0:1],
                                                       op=ALU.pow), reads=[ss_b, self.K["mhalf"][1]], writes=[ss_b])
                hf, hf_b = hr.next()
                S.op("dve", lambda e: e.scalar_tensor_tensor(out=hf[:], in0=xt[:], scalar=ss[:, 0:1],
                                                             in1=gpre[:], op0=ALU.mult, op1=ALU.mult),
                     reads=[xt_b, ss_b, gpre_b], writes=[hf_b])
                for half in range(2):
                    ps, ps_b = pst.next()
                    for j in range(4):
                        dc = half * 4 + j
                        S.op("pe", lambda e: e.transpose(ps[:, j * 128:(j + 1) * 128],
                                                         hf[:, dc * 128:(dc + 1) * 128], identf[:]),
                             reads=[hf_b, identf_b], writes=[ps_b])
                    eng = "act" if half == 0 else "dve"
                    dst = hT[:, half * 4:half * 4 + 4, tt * 128:(tt + 1) * 128]
                    src = ps[:, :].rearrange("p (a b) -> p a b", a=4)
                    if eng == "act":
                        S.op("act", lambda e: e.activation(out=dst, in_=src, func=AF.Copy),
                             reads=[ps_b], writes=[hT_b[tt]])
                    else:
                        S.op("dve", lambda e: e.tensor_copy(out=dst, in_=src),
                             reads=[ps_b], writes=[hT_b[tt]])
            stg = Ring(es, nc, "p1_stg", 12, [128, 512], F32)
            vst = Ring(es, nc, "p1_vst", 2, [128, 8, 65], BF16)
            for (vt, vb) in vst.items:
                S.op("pool", lambda e: e.memset(vt[:], 1.0), writes=[vb])
            nev = 0
            for bi, (c0, ncol) in enumerate(blocks):
                wb, wb_b = pending.pop(0)
                if bi + 2 < len(blocks):
                    pending.append(load_block(bi + 2))
                if c0 == C_ATT_V:
                    for tt in range(16):
                        ps, ps_b = psm.next()
                        for dc in range(8):
                            S.op("pe", lambda e: e.matmul(ps[:, :], lhsT=hT[:, dc, tt * 128:(tt + 1) * 128],
                                                          rhs=wb[:, dc, :], start=(dc == 0), stop=(dc == 7)),
                                 reads=[hT_b[tt], wb_b], writes=[ps_b])
                        vt, vt_b = vst.next()
                        src = ps[:, :].rearrange("p (h d) -> p h d", h=8)
                        S.op("dve", lambda e: e.tensor_copy(out=vt[:, :, 0:64], in_=src),
                             reads=[ps_b], writes=[vt_b])
                        S.dma("pool", vtm[tt * 128:(tt + 1) * 128, :], vt[:].rearrange("p h d -> p (h d)"),
                              reads=[vt_b], writes=[vtm_b])
                    continue
                for g in range(ncol // 128):
                    for tc in range(4):
                        ps, ps_b = psm.next()
                        for dc in range(8):
                            S.op("pe", lambda e: e.matmul(ps[:, :], lhsT=wb[:, dc, g * 128:(g + 1) * 128],
                                                          rhs=hT[:, dc, tc * 512:(tc + 1) * 512],
                                                          start=(dc == 0), stop=(dc == 7)),
                                 reads=[wb_b] + hT_b[tc * 4:tc * 4 + 4], writes=[ps_b])
                        st, st_b = stg.next()
                        if nev % 2 == 0:
                            S.op("act", lambda e: e.activation(out=st[:], in_=ps[:, :], func=AF.Copy),
                                 reads=[ps_b], writes=[st_b])
                        else:
                            S.op("dve", lambda e: e.tensor_copy(out=st[:], in_=ps[:, :]),
                                 reads=[ps_b], writes=[st_b])
                        nev += 1
                        r0 = c0 + g * 128
                        S.dma(("pool", "act", "sp")[nev % 3],
                              projT[r0:r0 + 128, tc * 512:(tc + 1) * 512], st[:],
                              reads=[st_b], writes=[projT_b])
            if l >= 1:
                wv, wv_b = sb(es, nc, "p1_wv", [128, 8, 32], F32)
                wvb, wvb_b = sb(es, nc, "p1_wvb", [128, 8, 32], BF16)
                S.dma("sp", wv[:], self.inp["rw_vmix_down"][l - 1].rearrange("(dc p) e -> p dc e", p=128),
                      writes=[wv_b])
                S.op("dve", lambda e: e.tensor_copy(out=wvb[:], in_=wv[:]), reads=[wv_b], writes=[wvb_b])
                for tc in range(4):
                    ps, ps_b = psm.next()
                    for dc in range(8):
                        S.op("pe", lambda e: e.matmul(ps[0:32, :], lhsT=wvb[:, dc, :],
                                                      rhs=hT[:, dc, tc * 512:(tc + 1) * 512],
                                                      start=(dc == 0), stop=(dc == 7)),
                             reads=[wvb_b] + hT_b[tc * 4:tc * 4 + 4], writes=[ps_b])
                    st, st_b = stg.next()
                    S.op("dve", lambda e: e.tensor_copy(out=st[0:32, :], in_=ps[0:32, :]),
                         reads=[ps_b], writes=[st_b])
                    S.dma("sp", projT[DIN:DIN + 32, tc * 512:(tc + 1) * 512], st[0:32, :],
                          reads=[st_b], writes=[projT_b])

    def phase2(self, l):
        nc, S = self.nc, self.S
        projT, projT_b = self.scr["projT"], self.dbuf["projT"]
        vtm, vtm_b = self.scr["vtm"], self.dbuf["vtm"]
        yagT, yagT_b = self.scr["yagT"], self.dbuf["yagT"]
        identb, identb_b = self.K["ident_b"]
        trim, trim_b = self.K["trimask"]
        gsum, gsum_b = self.K["gsum"]
        thr, thr_b = self.K["thr"]
        ones, ones_b = self.K["ones"]
        with ExitStack() as es:
            vext, vext_b = sb(es, nc, "p2_vext", [128, 16, 520], BF16)
            S.dma("pool", vext[:], vtm.rearrange("(t p) c -> p t c", p=128), reads=[vtm_b], writes=[vext_b])
            slots = []
            for i in range(2):
                qaug_, _ = sb(es, nc, "p2_qaug", [128, T], BF16)
                kaug_, _ = sb(es, nc, "p2_kaug", [128, T], BF16)
                sl = dict(qaug=qaug_, kaug=kaug_, qa_q=Buf("qa_q"), qa_n=[Buf("qa_n%d" % k) for k in range(4)],
                          qa_c=Buf("qa_c"), ka_k=Buf("ka_k"), ka_c=Buf("ka_c"))
                S.dma("sp", qaug_[72:76, :], self.inp["c_qconst"][:, :], writes=[sl["qa_c"]])
                slots.append(sl)
            qfr = Ring(es, nc, "p2_qf", 2, [64, T], F32)
            kfr = Ring(es, nc, "p2_kf", 2, [64, T], F32)
            azr = Ring(es, nc, "p2_az", 2, [64, T], F32)
            szr = Ring(es, nc, "p2_sz", 2, [64, T], F32)
            kmr = Ring(es, nc, "p2_kmean", 2, [64, 8], F32)
            kdr = Ring(es, nc, "p2_kdiff", 2, [64, 8, 8], F32)
            indr = Ring(es, nc, "p2_ind", 2, [64, 512], BF16)
            ptr = Ring(es, nc, "p2_pt", 3, [128, 512], BF16)
            rden, rden_b = sb(es, nc, "p2_rden", [128, 512], F32)
            bcs, bcs_b = sb(es, nc, "p2_bcs", [64, 512], F32)
            yac, yac_b = sb(es, nc, "p2_yac", [64, 512], F32)
            yagr = Ring(es, nc, "p2_yag", 2, [64, T], BF16)
            ps_s = Ring(es, nc, "p2_pss", 4, [128, 512], F32, psum=True)
            ps_o = Ring(es, nc, "p2_pso", 2, [128, 512], F32, psum=True)
            ps_m = Ring(es, nc, "p2_psm", 2, [128, 512], F32, psum=True)
            deferred = []

            def setup(h):
                sl = slots[h % 2]
                qaug, kaug = sl["qaug"], sl["kaug"]
                qf, qf_b = qfr.next()
                kf, kf_b = kfr.next()
                azf, azf_b = azr.next()
                S.dma("sp", qf[:], projT[C_ATT_Q + h * 64:C_ATT_Q + (h + 1) * 64, :], reads=[projT_b], writes=[qf_b])
                S.dma("pool", kf[:], projT[C_ATT_K + h * 64:C_ATT_K + (h + 1) * 64, :], reads=[projT_b], writes=[kf_b])
                S.dma("sp", azf[:], projT[C_ATT_Z + h * 64:C_ATT_Z + (h + 1) * 64, :], reads=[projT_b], writes=[azf_b])
                S.dma("pool", kaug[64:76, :], self.inp["c_kconst"][h], writes=[sl["ka_c"]])
                S.op("pool", lambda e: e.tensor_copy(out=kaug[0:64, :], in_=kf[:]), reads=[kf_b], writes=[sl["ka_k"]])
                S.op("act", lambda e: e.activation(out=qaug[0:64, :], in_=qf[:], func=AF.Copy, scale=0.125),
                     reads=[qf_b], writes=[sl["qa_q"]])
                sz, sz_b = szr.next()
                S.op("act", lambda e: e.activation(out=sz[:], in_=azf[:], func=AF.Tanh, scale=0.5), reads=[azf_b], writes=[sz_b])
                S.op("dve", lambda e: e.scalar_tensor_tensor(out=sz[:], in0=sz[:], scalar=1.0, in1=azf[:],
                                                             op0=ALU.add, op1=ALU.mult),
                     reads=[sz_b, azf_b], writes=[sz_b])
                kmean, kmean_b = kmr.next()
                kdiff, kdiff_b = kdr.next()
                S.op("dve", lambda e: e.reduce_sum(out=kmean[:], in_=kf[:].rearrange("p (n k) -> p n k", k=256),
                                                   axis=AX.X), reads=[kf_b], writes=[kmean_b])
                S.op("dve", lambda e: e.tensor_tensor(out=kdiff[:], in0=kmean[:, :].unsqueeze(1).to_broadcast([64, 8, 8]),
                                                      in1=kmean[:, :].unsqueeze(2).to_broadcast([64, 8, 8]),
                                                      op=ALU.subtract), reads=[kmean_b], writes=[kdiff_b])
                return dict(sl=sl, qf=qf, qf_b=qf_b, sz=sz, sz_b=sz_b, kdiff=kdiff, kdiff_b=kdiff_b)

            nxt_setup = setup(0)
            for h in range(8):
                st_ = nxt_setup
                sl = st_["sl"]
                qaug, kaug = sl["qaug"], sl["kaug"]
                qa_q, qa_n, qa_c, ka_k, ka_c = sl["qa_q"], sl["qa_n"], sl["qa_c"], sl["ka_k"], sl["ka_c"]
                qf, qf_b, sz, sz_b = st_["qf"], st_["qf_b"], st_["sz"], st_["sz_b"]
                kdiff, kdiff_b = st_["kdiff"], st_["kdiff_b"]
                yag, yag_b = yagr.next()
                for c in range(4):
                    if c == 2 and h + 1 < 8:
                        nxt_setup = setup(h + 1)
                    pg, pg_b = ps_m.next()
                    S.op("pe", lambda e: e.matmul(pg[0:64, :], lhsT=kdiff[:].rearrange("p n m -> p (n m)"),
                                                  rhs=qf[:, c * 512:(c + 1) * 512], start=True, stop=True),
                         reads=[kdiff_b, qf_b], writes=[pg_b])
                    ind, ind_b = indr.next()
                    S.op("dve", lambda e: e.tensor_single_scalar(out=ind[:], in_=pg[0:64, :], scalar=0.0, op=ALU.is_gt),
                         reads=[pg_b], writes=[ind_b])
                    pr, pr_b = ps_m.next()
                    for j in range(2):
                        qb = 2 * c + j
                        S.op("pe", lambda e: e.matmul(pr[0:72, j * 256:(j + 1) * 256], lhsT=gsum[:, qb, :],
                                                      rhs=ind[:, j * 256:(j + 1) * 256], start=True, stop=True),
                             reads=[gsum_b, ind_b], writes=[pr_b])
                    for j in range(2):
                        qb = 2 * c + j
                        S.op("dve", lambda e: e.tensor_scalar(out=qaug[64:72, qb * 256:(qb + 1) * 256],
                                                              in0=pr[64:72, j * 256:(j + 1) * 256],
                                                              scalar1=thr[64:72, qb:qb + 1], scalar2=None,
                                                              op0=ALU.is_ge),
                             reads=[pr_b, thr_b], writes=[qa_n[c]])
                    po, po_b = ps_o.next()
                    nkt = 4 * c + 4

                    def qk(kt, c=c, h=h):
                        j = kt - 4 * c
                        off = 0 if j < 0 else j * 128
                        n = 512 - off
                        q0 = c * 512 + off
                        pss, pss_b = ps_s.next()
                        S.op("pe", lambda e: e.matmul(pss[:, 0:n], lhsT=kaug[0:76, kt * 128:(kt + 1) * 128],
                                                      rhs=qaug[0:76, q0:q0 + n], start=True, stop=(j < 0)),
                             reads=[ka_k, ka_c, qa_q, qa_n[c], qa_c], writes=[pss_b])
                        if j >= 0:
                            S.op("pe", lambda e: e.matmul(pss[:, 0:128], lhsT=identb[:], rhs=trim[:],
                                                          start=False, stop=True),
                                 reads=[identb_b, trim_b], writes=[pss_b])
                        return pss, pss_b, off, n

                    def finalize(po=po, po_b=po_b, c=c, h=h, yag=yag, yag_b=yag_b, sz=sz, sz_b=sz_b, last=(c == 3)):
                        S.op("dve", lambda e: e.reciprocal(out=rden[64:65, :], in_=po[64:65, :]),
                             reads=[po_b], writes=[rden_b])
                        pb, pb_b = ps_m.next()
                        S.op("pe", lambda e: e.matmul(pb[0:64, :], lhsT=ones[64:65, 0:64], rhs=rden[64:65, :],
                                                      start=True, stop=True), reads=[ones_b, rden_b], writes=[pb_b])
                        S.op("act", lambda e: e.activation(out=bcs[:], in_=pb[0:64, :], func=AF.Copy),
                             reads=[pb_b], writes=[bcs_b])
                        S.op("dve", lambda e: e.scalar_tensor_tensor(out=yac[:], in0=po[0:64, :], scalar=0.5, in1=bcs[:],
                                                                     op0=ALU.mult, op1=ALU.mult),
                             reads=[po_b, bcs_b], writes=[yac_b])
                        S.op("pool", lambda e: e.tensor_tensor(out=yag[:, c * 512:(c + 1) * 512], in0=yac[:],
                                                               in1=sz[:, c * 512:(c + 1) * 512], op=ALU.mult),
                             reads=[yac_b, sz_b], writes=[yag_b])
                        if last:
                            S.dma("sp", yagT[h * 64:(h + 1) * 64, :], yag[:], reads=[yag_b], writes=[yagT_b])

                    pend = [qk(0), qk(1)]
                    for kt in range(nkt):
                        if kt + 2 < nkt:
                            pend.append(qk(kt + 2))
                        if kt == 1 and deferred:
                            deferred.pop()()
                        pss, pss_b, off, n = pend.pop(0)
                        pt, pt_b = ptr.next()
                        S.op("act", lambda e: e.activation(out=pt[:, 0:n], in_=pss[:, 0:n], func=AF.Exp),
                             reads=[pss_b], writes=[pt_b])
                        S.op("pe", lambda e: e.matmul(po[0:65, off:512], lhsT=vext[:, kt, h * 65:(h + 1) * 65],
                                                      rhs=pt[:, 0:n], start=(kt == 0), stop=(kt == nkt - 1)),
                             reads=[vext_b, pt_b], writes=[po_b])
                    deferred.append(finalize)
            while deferred:
                deferred.pop()()

    def phase3_gen(self, l, es, TH=256):
        nc, S = self.nc, self.S
        projT, projT_b = self.scr["projT"], self.dbuf["projT"]
        ybT, ybT_b = self.scr["ybT"], self.dbuf["ybT"]
        vfirst, vfirst_b = self.scr["vfirst"], self.dbuf["vfirst"]
        identf, identf_b = self.K["ident_f"]
        blk64, blk64_b = self.K["blk64"]
        mask2, mask2_b = self.K["mask2"]
        maskl, maskl_b = self.K["maskl"]
        pc, pc_b = self.K["pc"]
        pc1m, pc1m_b = self.K["pc1m"]
        NCH = TH // CH
        C0 = math.exp(-0.5)
        PE_PER_B = 6
        R32 = F32R
        CARVE = False

        def col(t, c):
            return t[:, l, c:c + 1]

        wau, wau_b = sb(es, nc, "p3_wau", [128, 512], F32)
        S.dma("sp", wau[0:64, :], self.inp["rw_w_up"][l], writes=[wau_b])
        S.dma("sp", wau[64:128, :], self.inp["rw_a_up"][l], writes=[wau_b])
        if l >= 1:
            vmu, vmu_b = sb(es, nc, "p3_vmu", [32, 512], F32)
            S.dma("sp", vmu[:], self.inp["rw_vmix_up"][l - 1], writes=[vmu_b])
        idr, idr_b = sb(es, nc, "p3_idr", [128, 128], R32)
        S.op("dve", lambda e: e.tensor_copy(out=idr[:], in_=identf[:]), reads=[identf_b], writes=[idr_b])
        rings = {}

        def R(name, n=1, shape=None, dt=F32):
            if name not in rings:
                rings[name] = Ring(es, nc, "p3_" + name, n, list(shape or (128, TH)), dt)
            return rings[name].next()

        zf, zf_b = sb(es, nc, "p3_zf", [128, 2], F32)
        S.op("pool", lambda e: e.memset(zf[:], 0.0), writes=[zf_b])
        ARr = Ring(es, nc, "p3_AR", 4, [128, NCH * 256], R32)
        BKr = Ring(es, nc, "p3_BK", 3, [128, NCH * 256], R32)
        BVr = Ring(es, nc, "p3_BV", 3, [128, NCH * 384], R32)
        Wn2 = [Ring(es, nc, "p3_W%d" % i, 2 * NCH, [128, 384], R32) for i in range(2)]
        for rg, pat, kw in ((ARr, "p (c a q t) -> p c a q t", dict(c=NCH, a=2, q=2)),
                            (BKr, "p (c a q t) -> p c a q t", dict(c=NCH, a=2, q=2)),
                            (BVr, "p (c a q t) -> p c a q t", dict(c=NCH, a=3, q=2)),
                            (Wn2[0], "p (a b) -> p a b", dict(a=3)),
                            (Wn2[1], "p (a b) -> p a b", dict(a=3))):
            new_items = []
            for (t_, b_) in rg.items:
                n_ = t_[:].shape[1]
                S.op("dve", lambda e: e.tensor_copy(out=t_[:, :], in_=zf[:, 0:1].to_broadcast([128, n_])),
                     reads=[zf_b], writes=[b_])
                new_items.append((t_[:, :].rearrange(pat, **kw), b_))
            rg.items = new_items
        PTr2 = [Ring(es, nc, "p3_PT%d" % i, 2 * NCH, [128, 128], R32) for i in range(2)]
        NM1r = Ring(es, nc, "p3_NM1", 3 * NCH, [128, 256], R32)
        NM2r = Ring(es, nc, "p3_NM2", 3 * NCH, [128, 256], R32)
        NbTr2 = [Ring(es, nc, "p3_NbT%d" % i, NCH, [128, 128], R32) for i in range(2)]
        Tfr = Ring(es, nc, "p3_Tf", 3 * NCH, [128, 128], R32)
        TM3r = Ring(es, nc, "p3_TM3", 3 * NCH, [128, 3, 128], R32)
        W1r = Ring(es, nc, "p3_W1", 2, [128, 128], R32)
        UTr = Ring(es, nc, "p3_UT", 2, [128, 128], R32)
        Sr = Ring(es, nc, "p3_S", 2, [128, 128], R32)
        if CARVE:
            psA = PsumRing(es, nc, "p3_psA", 4, 256)
            psB = PsumRing(es, nc, "p3_psB", 8, 128)
        else:
            psA = PsumRing(es, nc, "p3_psA", 2, 512)
            psB = PsumRing(es, nc, "p3_psB", 4, 512)
            psA.items = [(a[:, 0:256], b) for a, b in psA.items]
            psB.items = [(a[:, 0:128], b) for a, b in psB.items]
        psC = PsumRing(es, nc, "p3_psC", 2, 512)

        pch, pch_b = self.K["pch"]
        half, half_b = self.K["half"]
        mhalf, mhalf_b = self.K["mhalf"]

        def shift(dst, dst_b, X, X_b, mucol, npart=128, eng="pool"):
            tmp, tmp_b = R("shtmp", 2)
            S.op("act", lambda e: e.activation(out=dst[0:npart, :], in_=X[0:npart, 1:TH + 1], func=AF.Copy,
                                               scale=col(pc1m, mucol)[0:npart]),
                 reads=[X_b, pc1m_b], writes=[dst_b])
            S.op("act", lambda e: e.activation(out=tmp[0:npart, :], in_=X[0:npart, 0:TH], func=AF.Copy,
                                               scale=col(pc, mucol)[0:npart]),
                 reads=[X_b, pc_b], writes=[tmp_b])
            S.op("pool", lambda e: e.tensor_tensor(out=dst[0:npart, :], in0=dst[0:npart, :], in1=tmp[0:npart, :],
                                                   op=ALU.add), reads=[dst_b, tmp_b], writes=[dst_b])

        def sigm(dst, dst_b, src, src_b, bcol):
            S.op("act", lambda e: e.activation(out=dst[:], in_=src, func=AF.Tanh, scale=0.5, bias=col(pch, bcol)),
                 reads=[src_b, pch_b], writes=[dst_b])
            S.op("act", lambda e: e.activation(out=dst[:], in_=dst[:], func=AF.Identity, scale=0.5, bias=half[:, 0:1]),
                 reads=[dst_b, half_b], writes=[dst_b])

        def load_shifted(name, row0, nrows, t0, q):
            X, X_b = R("X" + name, 2, (128, TH + 1))
            if t0 == 0:
                S.op("pool", lambda e: e.memset(X[0:nrows, 0:1], 0.0), writes=[X_b])
                S.dma(q, X[0:nrows, 1:TH + 1], projT[row0:row0 + nrows, 0:TH], reads=[projT_b], writes=[X_b])
            else:
                S.dma(q, X[0:nrows, :], projT[row0:row0 + nrows, t0 - 1:t0 + TH], reads=[projT_b], writes=[X_b])
            return X, X_b

        def prepA(j, tb, par):
            t0 = tb * TH
            ctx = {}
            Wn, PTr, NbTr = Wn2[par], PTr2[par], NbTr2[par]
            Xr, Xr_b = load_shifted("r", C_RW_R + j * 128, 128, t0, "sp")
            Xk, Xk_b = load_shifted("k", C_RW_K + j * 128, 128, t0, "pool")
            Xv, Xv_b = load_shifted("v", C_RW_V + j * 128, 128, t0, "sp")
            Xw, Xw_b = load_shifted("w", C_RW_WD, 128, t0, "pool")
            Xz, Xz_b = R("Xz", 4)
            S.dma("sp", Xz[:], projT[C_RW_Z + j * 128:C_RW_Z + (j + 1) * 128, t0:t0 + TH],
                  reads=[projT_b], writes=[Xz_b])
            yield
            rs, rs_b = R("rs")
            ks, ks_b = R("ks")
            vs, vs_b = R("vs", 2)
            was, was_b = R("was")
            shift(rs, rs_b, Xr, Xr_b, PC_MU + j)
            shift(ks, ks_b, Xk, Xk_b, PC_MU + 4 + j)
            yield
            shift(vs, vs_b, Xv, Xv_b, PC_MU + 8 + j, eng="pool")
            shift(was, was_b, Xw, Xw_b, PC_MU + 12, eng="pool")
            yield
            S.op("act", lambda e: e.activation(out=was[0:64, :], in_=was[0:64, :], func=AF.Tanh),
                 reads=[was_b], writes=[was_b])
            pz, pz_b = psC.next()
            S.op("pe", lambda e: e.matmul(pz[:, 0:TH], lhsT=wau[0:64, j * 128:(j + 1) * 128], rhs=was[0:64, :],
                                          start=True, stop=True), reads=[wau_b, was_b], writes=[pz_b])
            sg, sg_b = R("sg")
            sigm(sg, sg_b, pz[:, 0:TH], pz_b, PC_W0 + j)
            pa, pa_b = psC.next()
            S.op("pe", lambda e: e.matmul(pa[:, 0:TH], lhsT=wau[64:128, j * 128:(j + 1) * 128],
                                          rhs=was[64:128, :], start=True, stop=True),
                 reads=[wau_b, was_b], writes=[pa_b])
            aic, aic_b = R("aic")
            sigm(aic, aic_b, pa[:, 0:TH], pa_b, PC_A0 + j)
            yield
            if l == 0:
                vr, vr_b = vs, vs_b
                S.dma("pool", vfirst[j * 128:(j + 1) * 128, t0:t0 + TH], vs[:], reads=[vs_b], writes=[vfirst_b])
            else:
                Xm, Xm_b = load_shifted("m", DIN, 32, t0, "sp")
                vms, vms_b = R("vms")
                shift(vms, vms_b, Xm, Xm_b, PC_VMU, npart=32, eng="pool")
                pv, pv_b = psC.next()
                S.op("pe", lambda e: e.matmul(pv[:, 0:TH], lhsT=vmu[0:32, j * 128:(j + 1) * 128],
                                              rhs=vms[0:32, :], start=True, stop=True),
                     reads=[vmu_b, vms_b], writes=[pv_b])
                gt, gt_b = R("gt")
                sigm(gt, gt_b, pv[:, 0:TH], pv_b, PC_VM0 + j)
                vf, vf_b = R("vf")
                S.dma("pool", vf[:], vfirst[j * 128:(j + 1) * 128, t0:t0 + TH], reads=[vfirst_b], writes=[vf_b])
                S.op("pool", lambda e: e.tensor_tensor(out=vf[:], in0=vf[:], in1=vs[:], op=ALU.subtract),
                     reads=[vf_b, vs_b], writes=[vf_b])
                S.op("pool", lambda e: e.tensor_tensor(out=vf[:], in0=vf[:], in1=gt[:], op=ALU.mult),
                     reads=[vf_b, gt_b], writes=[vf_b])
                vr, vr_b = R("vr", 2)
                S.op("pool", lambda e: e.tensor_tensor(out=vr[:], in0=vf[:], in1=vs[:], op=ALU.add),
                     reads=[vf_b, vs_b], writes=[vr_b])
            yield
            kk, kk_b = R("kk")
            S.op("act", lambda e: e.activation(out=kk[:], in_=ks[:], func=AF.Copy, scale=col(pc, PC_KK + j)),
                 reads=[ks_b, pc_b], writes=[kk_b])
            sq, sq_b = R("sq")
            S.op("pool", lambda e: e.tensor_tensor(out=sq[:], in0=kk[:], in1=kk[:], op=ALU.mult),
                 reads=[kk_b], writes=[sq_b])
            pq, pq_b = psC.next()
            S.op("pe", lambda e: e.matmul(pq[:, 0:TH], lhsT=blk64[:], rhs=sq[:], start=True, stop=True),
                 reads=[blk64_b, sq_b], writes=[pq_b])
            rn, rn_b = R("rn")
            S.op("dve", lambda e: e.tensor_scalar_max(out=rn[:], in0=pq[:, 0:TH], scalar1=1e-24),
                 reads=[pq_b], writes=[rn_b])
            S.op("dve", lambda e: e.reciprocal(out=rn[:], in_=rn[:]), reads=[rn_b], writes=[rn_b])
            bv, bv_b = R("bv")
            S.op("pool", lambda e: e.tensor_tensor(out=bv[:], in0=kk[:], in1=aic[:], op=ALU.mult),
                 reads=[kk_b, aic_b], writes=[bv_b])
            S.op("pool", lambda e: e.tensor_tensor(out=kk[:], in0=kk[:], in1=rn[:], op=ALU.mult),
                 reads=[kk_b, rn_b], writes=[kk_b])
            yield
            km, km_b = R("km")
            S.op("dve", lambda e: e.tensor_scalar(out=km[:], in0=aic[:], scalar1=col(pc, PC_KA + j),
                                                  scalar2=col(pc1m, PC_KA + j), op0=ALU.mult, op1=ALU.add),
                 reads=[aic_b, pc_b, pc1m_b], writes=[km_b])
            S.op("dve", lambda e: e.tensor_tensor(out=km[:], in0=km[:], in1=ks[:], op=ALU.mult),
                 reads=[km_b, ks_b], writes=[km_b])
            S.op("dve", lambda e: e.scalar_tensor_tensor(out=sq[:], in0=rs[:], scalar=col(pc, PC_RK + j),
                                                         in1=km[:], op0=ALU.mult, op1=ALU.mult),
                 reads=[rs_b, pc_b, km_b, sq_b], writes=[sq_b])
            pb, pb_b = psC.next()
            S.op("pe", lambda e: e.matmul(pb[:, 0:TH], lhsT=blk64[:], rhs=sq[:], start=True, stop=True),
                 reads=[blk64_b, sq_b], writes=[pb_b])
            bon, bon_b = R("bon", 4)
            S.op("dve", lambda e: e.tensor_tensor(out=bon[:], in0=pb[:, 0:TH], in1=vr[:], op=ALU.mult),
                 reads=[pb_b, vr_b], writes=[bon_b])
            yield
            G, G_b = R("G")
            S.op("dve", lambda e: e.tensor_tensor_scan(out=G[:], data0=sg[:], data1=sg[:], initial=0.0,
                                                       op0=ALU.add, op1=ALU.bypass), reads=[sg_b], writes=[G_b])
            Gs, Gs_b = R("Gs", 1, (128, NCH))
            S.op("pool", lambda e: e.memset(Gs[:, 0:1], 0.0), writes=[Gs_b])
            G3 = G[:, :].rearrange("p (c t) -> p c t", t=CH)
            S.op("dve", lambda e: e.tensor_copy(out=Gs[:, 1:NCH], in_=G3[:, 0:NCH - 1, CH - 1]),
                 reads=[G_b], writes=[Gs_b])
            csp, csp_b = R("csp")
            csp3 = csp[:, :].rearrange("p (c t) -> p c t", t=CH)
            S.op("dve", lambda e: e.tensor_tensor(out=csp3, in0=G3,
                                                  in1=Gs[:, :].unsqueeze(2).to_broadcast([128, NCH, CH]),
                                                  op=ALU.subtract), reads=[G_b, Gs_b], writes=[csp_b])
            Ep, Ep_b = R("Ep", 4)
            Em, Em_b = R("Em")
            Eq, Eq_b = R("Eq")
            S.op("act", lambda e: e.activation(out=Ep[:], in_=csp[:], func=AF.Exp, scale=-C0),
                 reads=[csp_b], writes=[Ep_b])
            S.op("act", lambda e: e.activation(out=Em[:], in_=csp[:], func=AF.Exp, scale=C0),
                 reads=[csp_b], writes=[Em_b])
            S.op("pool", lambda e: e.tensor_tensor(out=Eq[:], in0=csp[:], in1=sg[:], op=ALU.subtract),
                 reads=[csp_b, sg_b], writes=[Eq_b])
            S.op("act", lambda e: e.activation(out=Eq[:], in_=Eq[:], func=AF.Exp, scale=-C0),
                 reads=[Eq_b], writes=[Eq_b])
            yield
            Ep3 = Ep[:, :].rearrange("p (c t) -> p c t", t=CH)
            AR, AR_b = ARr.next()
            BK, BK_b = BKr.next()
            BV, BV_b = BVr.next()

            def v3(t_, p):
                return t_[p * 64:(p + 1) * 64, :].rearrange("p (c t) -> p c t", t=CH)

            for p in range(2):
                hs = slice(p * 64, (p + 1) * 64)
                S.op("dve", lambda e: e.scalar_tensor_tensor(out=AR[hs, :, 0, p, :], in0=v3(kk, p), scalar=-1.0,
                                                             in1=v3(Eq, p), op0=ALU.mult, op1=ALU.mult),
                     reads=[kk_b, Eq_b], writes=[AR_b])
                S.op("dve", lambda e: e.tensor_tensor(out=AR[hs, :, 1, p, :], in0=v3(rs, p), in1=v3(Ep, p),
                                                      op=ALU.mult), reads=[rs_b, Ep_b], writes=[AR_b])
                S.op("dve", lambda e: e.tensor_tensor(out=BK[hs, :, 0, p, :], in0=v3(bv, p), in1=v3(Em, p),
                                                      op=ALU.mult), reads=[bv_b, Em_b], writes=[BK_b])
                S.op("dve", lambda e: e.tensor_tensor(out=BK[hs, :, 1, p, :], in0=v3(km, p), in1=v3(Em, p),
                                                      op=ALU.mult), reads=[km_b, Em_b], writes=[BK_b])
                gcb = Ep3[hs, :, CH - 1:CH].to_broadcast([64, NCH, CH])
                S.op("dve", lambda e: e.tensor_tensor(out=BV[hs, :, 0, p, :], in0=BK[hs, :, 0, p, :], in1=gcb,
                                                      op=ALU.mult), reads=[BK_b, Ep_b], writes=[BV_b])
                S.op("dve", lambda e: e.tensor_tensor(out=BV[hs, :, 1, p, :], in0=BK[hs, :, 1, p, :], in1=gcb,
                                                      op=ALU.mult), reads=[BK_b, Ep_b], writes=[BV_b])
                S.op("dve", lambda e: e.tensor_copy(out=BV[hs, :, 2, p, :], in_=v3(vr, p)),
                     reads=[vr_b], writes=[BV_b])
                yield
            yield "A"
            ch = []
            for c in range(NCH):
                ARc = AR[:, c].rearrange("p a q t -> p (a q t)")
                d = {"ARc": ARc, "Abd": ARc[:, 0:128], "Rbd": ARc[:, 128:256],
                     "Bbd": BK[:, c, 0].rearrange("p q t -> p (q t)"),
                     "Kbd": BK[:, c, 1].rearrange("p q t -> p (q t)")}
                ch.append(d)
            for d in ch:
                p1, p1_b = psA.next()
                S.op("pe", lambda e: e.matmul(p1[:, 0:256], lhsT=d["Bbd"], rhs=d["ARc"], start=True, stop=True),
                     reads=[BK_b, AR_b], writes=[p1_b])
                d["NM1"], d["NM1_b"] = NM1r.next()
                S.op("dve", lambda e: e.tensor_tensor(out=d["NM1"][:], in0=p1[:, 0:256], in1=mask2[:], op=ALU.mult),
                     reads=[p1_b, mask2_b], writes=[d["NM1_b"]])
            yield
            for d in ch:
                p2, p2_b = psA.next()
                S.op("pe", lambda e: e.matmul(p2[:, 0:256], lhsT=d["Kbd"], rhs=d["ARc"], start=True, stop=True),
                     reads=[BK_b, AR_b], writes=[p2_b])
                d["NM2"], d["NM2_b"] = NM2r.next()
                S.op("dve", lambda e: e.tensor_tensor(out=d["NM2"][:], in0=p2[:, 0:256], in1=mask2[:], op=ALU.mult),
                     reads=[p2_b, mask2_b], writes=[d["NM2_b"]])
            yield
            for d in ch:
                p3, p3_b = psB.next()
                S.op("pe", lambda e: e.matmul(p3[:, :], lhsT=d["Abd"], rhs=d["Bbd"], start=True, stop=True),
                     reads=[AR_b, BK_b], writes=[p3_b])
                d["NbT"], d["NbT_b"] = NbTr.next()
                S.op("dve", lambda e: e.tensor_tensor(out=d["NbT"][:], in0=p3[:, :], in1=maskl[:], op=ALU.mult),
                     reads=[p3_b, maskl_b], writes=[d["NbT_b"]])
            yield
            for d in ch:
                Nba = d["NM1"][:, 0:128]
                d["W"], d["W_b"] = Wn.next()
                W = d["W"]
                S.op("pool", lambda e: e.tensor_tensor(out=W[:, 2, :], in0=Nba, in1=idr[:], op=ALU.add),
                     reads=[d["NM1_b"], idr_b], writes=[d["W_b"]])
                p4, p4_b = psB.next()
                S.op("pe", lambda e: e.matmul(p4[:, :], lhsT=d["NbT"][:], rhs=Nba, start=True, stop=True),
                     reads=[d["NbT_b"], d["NM1_b"]], writes=[p4_b])
                S.op("act", lambda e: e.activation(out=W[:, 0, :], in_=p4[:, :], func=AF.Copy),
                     reads=[p4_b], writes=[d["W_b"]])
                p5, p5_b = psB.next()
                S.op("pe", lambda e: e.matmul(p5[:, :], lhsT=Nba, rhs=d["NbT"][:], start=True, stop=True),
                     reads=[d["NbT_b"], d["NM1_b"]], writes=[p5_b])
                d["PT"], d["PT_b"] = PTr.next()
                PT = d["PT"]
                S.op("act", lambda e: e.activation(out=PT[:], in_=p5[:, :], func=AF.Copy),
                     reads=[p5_b], writes=[d["PT_b"]])
            yield
            for m in (2, 4, 8, 16, 32):
                for d in ch:
                    W, W_b, PT, PT_b = d["W"], d["W_b"], d["PT"], d["PT_b"]
                    pw, pw_b = psA.next()
                    S.op("pe", lambda e: e.matmul(pw[:, 0:256].rearrange("p (a b) -> p a b", a=2), lhsT=PT[:],
                                                  rhs=W[:, 0:3:2, :], start=True, stop=True),
                         reads=[PT_b, W_b], writes=[pw_b])
                    if m < 32:
                        W2, W2_b = Wn.next()
                        S.op("dve", lambda e: e.tensor_tensor(out=W2[:, 0:3:2, :],
                                                              in0=pw[:, 0:256].rearrange("p (a b) -> p a b", a=2),
                                                              in1=W[:, 1:3, :], op=ALU.add),
                             reads=[pw_b, W_b], writes=[W2_b])
                        pq2, pq2_b = psB.next()
                        S.op("pe", lambda e: e.matmul(pq2[:, :], lhsT=W[:, 0, :], rhs=PT[:], start=True, stop=True),
                             reads=[W_b, PT_b], writes=[pq2_b])
                        PT2, PT2_b = PTr.next()
                        S.op("act", lambda e: e.activation(out=PT2[:], in_=pq2[:, :], func=AF.Copy),
                             reads=[pq2_b], writes=[PT2_b])
                        d["W"], d["W_b"], d["PT"], d["PT_b"] = W2, W2_b, PT2, PT2_b
                    else:
                        d["Tf"], d["Tf_b"] = Tfr.next()
                        Tf = d["Tf"]
                        S.op("dve", lambda e: e.tensor_tensor(out=Tf[:], in0=pw[:, 128:256], in1=W[:, 2, :], op=ALU.add),
                             reads=[pw_b, W_b], writes=[d["Tf_b"]])
                yield
            for c, d in enumerate(ch):
                pt3, pt3_b = psC.next()
                for i in range(3):
                    S.op("pe", lambda e: e.matmul(pt3[:, i * 128:(i + 1) * 128],
                                                  lhsT=BV[:, c, i].rearrange("p q t -> p (q t)"), rhs=idr[:],
                                                  start=True, stop=True),
                         reads=[BV_b, idr_b], writes=[pt3_b])
                d["TM3"], d["TM3_b"] = TM3r.next()
                TM3 = d["TM3"]
                S.op("act", lambda e: e.activation(out=TM3[:].rearrange("p a b -> p (a b)"), in_=pt3[:, 0:384],
                                                   func=AF.Copy), reads=[pt3_b], writes=[d["TM3_b"]])
            yield
            ctx.update(ch=ch, AR_b=AR_b, Ep=Ep, Ep_b=Ep_b, bon=bon, bon_b=bon_b, Xz=Xz, Xz_b=Xz_b, j=j, t0=t0)
            return ctx

        def stageB(ctx, state):
            ch, AR_b, Ep, Ep_b = ctx["ch"], ctx["AR_b"], ctx["Ep"], ctx["Ep_b"]
            j, t0 = ctx["j"], ctx["t0"]
            ob, ob_b = R("ob", 2)
            for c, d in enumerate(ch):
                Scur, Scur_b = state["S"], state["S_b"]
                Nka, Mkr = d["NM2"][:, 0:128], d["NM2"][:, 128:256]
                Mbr = d["NM1"][:, 128:256]
                TM3, TM3_b, Tf, Tf_b = d["TM3"], d["TM3_b"], d["Tf"], d["Tf_b"]
                BpT, KpT, VT = TM3[:, 0, :], TM3[:, 1, :], TM3[:, 2, :]
                pw1, pw1_b = psB.next()
                S.op("pe", lambda e: e.matmul(pw1[:, :], lhsT=d["Abd"], rhs=Scur[:], start=True, stop=False),
                     reads=[AR_b, Scur_b], writes=[pw1_b])
                S.op("pe", lambda e: e.matmul(pw1[:, :], lhsT=Nka, rhs=VT, start=False, stop=True),
                     reads=[d["NM2_b"], TM3_b], writes=[pw1_b])
                W1, W1_b = W1r.next()
                S.op("act", lambda e: e.activation(out=W1[:], in_=pw1[:, :], func=AF.Copy),
                     reads=[pw1_b], writes=[W1_b])
                yield
                pu, pu_b = psB.next()
                S.op("pe", lambda e: e.matmul(pu[:, :], lhsT=Tf[:], rhs=W1[:], start=True, stop=True),
                     reads=[Tf_b, W1_b], writes=[pu_b])
                UT, UT_b = UTr.next()
                S.op("dve", lambda e: e.tensor_copy(out=UT[:], in_=pu[:, :]), reads=[pu_b], writes=[UT_b])
                yield
                ps2, ps2_b = psB.next()
                S.op("pe", lambda e: e.matmul(ps2[:, :], lhsT=BpT, rhs=UT[:], start=True, stop=False),
                     reads=[TM3_b, UT_b], writes=[ps2_b])
                S.op("pe", lambda e: e.matmul(ps2[:, :], lhsT=KpT, rhs=VT, start=False, stop=True),
                     reads=[TM3_b], writes=[ps2_b])
                Snx, Snx_b = Sr.next()
                gc = Ep[:, c * CH + CH - 1:c * CH + CH]
                S.op("dve", lambda e: e.scalar_tensor_tensor(out=Snx[:], in0=Scur[:], scalar=gc, in1=ps2[:, :],
                                                             op0=ALU.mult, op1=ALU.add),
                     reads=[Scur_b, Ep_b, ps2_b], writes=[Snx_b])
                po, po_b = psB.next()
                S.op("pe", lambda e: e.matmul(po[:, :], lhsT=Scur[:], rhs=d["Rbd"], start=True, stop=False),
                     reads=[Scur_b, AR_b], writes=[po_b])
                S.op("pe", lambda e: e.matmul(po[:, :], lhsT=UT[:], rhs=Mbr, start=False, stop=False),
                     reads=[UT_b, d["NM1_b"]], writes=[po_b])
                S.op("pe", lambda e: e.matmul(po[:, :], lhsT=VT, rhs=Mkr, start=False, stop=True),
                     reads=[TM3_b, d["NM2_b"]], writes=[po_b])
                for p in range(2):
                    hs = slice(p * 64, (p + 1) * 64)
                    S.op("act", lambda e: e.activation(out=ob[hs, c * CH:(c + 1) * CH],
                                                       in_=po[hs, p * 64:(p + 1) * 64], func=AF.Copy),
                         reads=[po_b], writes=[ob_b])
                state["S"], state["S_b"] = Snx, Snx_b
                yield
            bon, bon_b, Xz, Xz_b = ctx["bon"], ctx["bon_b"], ctx["Xz"], ctx["Xz_b"]
            pm, pm_b = psC.next()
            S.op("pe", lambda e: e.matmul(pm[:, 0:TH], lhsT=blk64[:], rhs=ob[:], start=True, stop=True),
                 reads=[blk64_b, ob_b], writes=[pm_b])
            dd, dd_b = R("dd")
            S.op("dve", lambda e: e.scalar_tensor_tensor(out=dd[:], in0=pm[:, 0:TH], scalar=-1.0 / 64, in1=ob[:],
                                                         op0=ALU.mult, op1=ALU.add),
                 reads=[pm_b, ob_b], writes=[dd_b])
            sq2, sq2_b = R("sq2")
            S.op("pool", lambda e: e.tensor_tensor(out=sq2[:], in0=dd[:], in1=dd[:], op=ALU.mult),
                 reads=[dd_b], writes=[sq2_b])
            pvv, pvv_b = psC.next()
            S.op("pe", lambda e: e.matmul(pvv[:, 0:TH], lhsT=blk64[:], rhs=sq2[:], start=True, stop=True),
                 reads=[blk64_b, sq2_b], writes=[pvv_b])
            rn2, rn2_b = R("rn2")
            S.op("dve", lambda e: e.tensor_scalar(out=rn2[:], in0=pvv[:, 0:TH], scalar1=1.0 / 64, scalar2=GN_EPS,
                                                  op0=ALU.mult, op1=ALU.add), reads=[pvv_b], writes=[rn2_b])
            S.op("act", lambda e: e.activation(out=rn2[:], in_=rn2[:], func=AF.Sqrt), reads=[rn2_b], writes=[rn2_b])
            S.op("dve", lambda e: e.reciprocal(out=rn2[:], in_=rn2[:]), reads=[rn2_b], writes=[rn2_b])
            yield
            S.op("pool", lambda e: e.tensor_tensor(out=dd[:], in0=dd[:], in1=rn2[:], op=ALU.mult),
                 reads=[dd_b, rn2_b], writes=[dd_b])
            S.op("dve", lambda e: e.tensor_scalar(out=dd[:], in0=dd[:], scalar1=col(pc, PC_LNG + j),
                                                  scalar2=col(pc, PC_LNB + j), op0=ALU.mult, op1=ALU.add),
                 reads=[dd_b, pc_b], writes=[dd_b])
            S.op("pool", lambda e: e.tensor_tensor(out=dd[:], in0=dd[:], in1=bon[:], op=ALU.add),
                 reads=[dd_b, bon_b], writes=[dd_b])
            th, th_b = R("th")
            S.op("act", lambda e: e.activation(out=th[:], in_=Xz[:], func=AF.Tanh, scale=0.5), reads=[Xz_b], writes=[th_b])
            S.op("dve", lambda e: e.scalar_tensor_tensor(out=th[:], in0=th[:], scalar=1.0, in1=Xz[:],
                                                         op0=ALU.add, op1=ALU.mult),
                 reads=[th_b, Xz_b], writes=[th_b])
            yb, yb_b = R("yb", 2, (128, TH), BF16)
            S.op("dve", lambda e: e.scalar_tensor_tensor(out=yb[:], in0=dd[:], scalar=0.5, in1=th[:],
                                                         op0=ALU.mult, op1=ALU.mult),
                 reads=[dd_b, th_b], writes=[yb_b])
            S.dma("sp", ybT[j * 128:(j + 1) * 128, t0:t0 + TH], yb[:], reads=[yb_b], writes=[ybT_b])
            yield

        blocks = [(j, tb) for j in range(4) for tb in range(T // TH)]
        state = {}
        nxt = 0
        gP = gB = None
        gAs = []
        gP_waiting = False
        bq = []
        while nxt < len(blocks) or gP is not None or gAs or gB is not None or bq:
            if gP is None and nxt < len(blocks):
                gP = prepA(blocks[nxt][0], blocks[nxt][1], nxt % 2)
                nxt += 1
                gP_waiting = False
            if gB is None and bq:
                ctx = bq.pop(0)
                if ctx["t0"] == 0:
                    S0, S0_b = Sr.next()
                    S.op("dve", lambda e: e.tensor_copy(out=S0[:], in_=zf[:, 0:1].to_broadcast([128, 128])),
                         reads=[zf_b], writes=[S0_b])
                    state["S"], state["S_b"] = S0, S0_b
                gB = stageB(ctx, state)
            if gB is not None:
                try:
                    next(gB)
                except StopIteration:
                    gB = None
            for g in list(gAs):
                try:
                    next(g)
                except StopIteration as st:
                    assert g is gAs[0]
                    bq.append(st.value)
                    gAs.remove(g)
            if gP is not None:
                if not gP_waiting:
                    if next(gP) == "A":
                        gP_waiting = True
                if gP_waiting and len(gAs) < 2 and len(gAs) + len(bq) + (1 if gB is not None else 0) <= 2:
                    gAs.append(gP)
                    gP, gP_waiting = None, False
            yield

    def phase3(self, l):
        with ExitStack() as es:
            for _ in self.phase3_gen(l, es):
                pass

    def phase4(self, l, xin, xin_b, xo, xo_b):
        nc, S = self.nc, self.S
        projT, projT_b = self.scr["projT"], self.dbuf["projT"]
        yagT, yagT_b = self.scr["yagT"], self.dbuf["yagT"]
        ybT, ybT_b = self.scr["ybT"], self.dbuf["ybT"]
        with ExitStack() as es:
            gpost, gpost_b = sb(es, nc, "p4_gpost", [128, D], F32)
            S.dma("pool", gpost[:], self.inp["norm_post"][l].partition_broadcast(128), writes=[gpost_b])
            ya, ya_b = sb(es, nc, "p4_ya", [128, 4, T], BF16)
            yb, yb_b = sb(es, nc, "p4_yb", [128, 4, T], BF16)
            S.dma("pool", ya[:], yagT.rearrange("(c p) t -> p c t", p=128), reads=[yagT_b], writes=[ya_b])
            S.dma("pool", yb[:], ybT.rearrange("(c p) t -> p c t", p=128), reads=[ybT_b], writes=[yb_b])
            wst = Ring(es, nc, "p4_wst", 2, [128, 4, D], F32)
            wua, wua_b = sb(es, nc, "p4_wua", [128, 4, D], BF16)
            wur, wur_b = sb(es, nc, "p4_wur", [128, 4, D], BF16)
            wo, wo_b = sb(es, nc, "p4_wo", [128, 8, D], BF16)
            srcs = [(self.inp["w_up_att"][l].rearrange("(c p) e -> p c e", p=128), wua[:, :, :], wua_b),
                    (self.inp["w_up_rw"][l].rearrange("(c p) e -> p c e", p=128), wur[:, :, :], wur_b),
                    (self.inp["w_out"][l].rearrange("(c p) e -> p c e", p=128)[:, 0:4, :], wo[:, 0:4, :], wo_b),
                    (self.inp["w_out"][l].rearrange("(c p) e -> p c e", p=128)[:, 4:8, :], wo[:, 4:8, :], wo_b)]
            for i, (src, dst, dst_b) in enumerate(srcs):
                ws, ws_b = wst.next()
                S.dma("sp", ws[:], src, writes=[ws_b])
                S.op("pool" if i % 2 == 0 else "dve", lambda e: e.tensor_copy(out=dst, in_=ws[:]),
                     reads=[ws_b], writes=[dst_b])
            uTr = Ring(es, nc, "p4_uT", 2, [128, 8, 512], BF16)
            gAr = Ring(es, nc, "p4_gA", 2, [128, 512], F32)
            gRr = Ring(es, nc, "p4_gR", 2, [128, 512], F32)
            t1r = Ring(es, nc, "p4_t1", 2, [128, 512], F32)
            t2r = Ring(es, nc, "p4_t2", 2, [128, 512], F32)
            junk, junk_b = sb(es, nc, "p4_junk", [128, 512], F32)
            ssr = Ring(es, nc, "p4_ss", 4, [128, 4], F32)
            xtr = Ring(es, nc, "p4_xt", 2, [128, D], F32)
            otr = Ring(es, nc, "p4_ot", 2, [128, D], F32)
            psa = Ring(es, nc, "p4_psa", 2, [128, 512], F32, psum=True)
            psr = Ring(es, nc, "p4_psr", 2, [128, 512], F32, psum=True)
            psy = Ring(es, nc, "p4_psy", 4, [128, 512], F32, psum=True)
            for tc in range(4):
                ts_ = slice(tc * 512, (tc + 1) * 512)
                uT, uT_b = uTr.next()
                for eg in range(8):
                    gA, gA_b = gAr.next()
                    gR, gR_b = gRr.next()
                    S.dma("sp", gA[:], projT[C_G_ATT + eg * 128:C_G_ATT + (eg + 1) * 128, ts_], reads=[projT_b], writes=[gA_b])
                    S.dma("pool", gR[:], projT[C_G_RW + eg * 128:C_G_RW + (eg + 1) * 128, ts_], reads=[projT_b], writes=[gR_b])
                    for (g_, g_b) in ((gA, gA_b), (gR, gR_b)):
                        S.op("act", lambda e: e.activation(out=g_[:], in_=g_[:], func=AF.Tanh, scale=0.5),
                             reads=[g_b], writes=[g_b])
                        S.op("act", lambda e: e.activation(out=g_[:], in_=g_[:], func=AF.Identity, scale=0.5,
                                                           bias=self.K["half"][0][:, 0:1]),
                             reads=[g_b, self.K["half"][1]], writes=[g_b])
                    pa, pa_b = psa.next()
                    pr, pr_b = psr.next()
                    for c in range(4):
                        S.op("pe", lambda e: e.matmul(pa[:, :], lhsT=wua[:, c, eg * 128:(eg + 1) * 128], rhs=ya[:, c, ts_],
                                                      start=(c == 0), stop=(c == 3)), reads=[wua_b, ya_b], writes=[pa_b])
                    for c in range(4):
                        S.op("pe", lambda e: e.matmul(pr[:, :], lhsT=wur[:, c, eg * 128:(eg + 1) * 128], rhs=yb[:, c, ts_],
                                                      start=(c == 0), stop=(c == 3)), reads=[wur_b, yb_b], writes=[pr_b])
                    t1, t1_b = t1r.next()
                    t2, t2_b = t2r.next()
                    S.op("dve", lambda e: e.tensor_tensor(out=t1[:], in0=pa[:, :], in1=gA[:], op=ALU.mult),
                         reads=[pa_b, gA_b], writes=[t1_b])
                    S.op("dve", lambda e: e.tensor_tensor(out=t2[:], in0=pr[:, :], in1=gR[:], op=ALU.mult),
                         reads=[pr_b, gR_b], writes=[t2_b])
                    S.op("pool", lambda e: e.tensor_tensor(out=uT[:, eg, :], in0=t1[:], in1=t2[:], op=ALU.add),
                         reads=[t1_b, t2_b], writes=[uT_b])
                for tt in range(4):
                    tok0 = tc * 512 + tt * 128
                    xt, xt_b = xtr.next()
                    S.dma("sp", xt[:], xin[tok0:tok0 + 128, :], reads=[xin_b], writes=[xt_b])
                    ss, ss_b = ssr.next()
                    pys = []
                    for hf in range(2):
                        py, py_b = psy.next()
                        for eg in range(8):
                            S.op("pe", lambda e: e.matmul(py[:, :], lhsT=uT[:, eg, tt * 128:(tt + 1) * 128],
                                                          rhs=wo[:, eg, hf * 512:(hf + 1) * 512],
                                                          start=(eg == 0), stop=(eg == 7)), reads=[uT_b, wo_b], writes=[py_b])
                        S.op("act", lambda e: e.activation(out=junk[:], in_=py[:, :], func=AF.Square,
                                                           accum_out=ss[:, hf:hf + 1]),
                             reads=[py_b], writes=[junk_b, ss_b])
                        pys.append((py, py_b))
                    S.op("dve", lambda e: e.tensor_tensor(out=ss[:, 2:3], in0=ss[:, 0:1], in1=ss[:, 1:2], op=ALU.add),
                         reads=[ss_b], writes=[ss_b])
                    S.op("dve", lambda e: e.tensor_scalar(out=ss[:, 3:4], in0=ss[:, 2:3], scalar1=1.0 / D, scalar2=RMS_EPS,
                                                          op0=ALU.mult, op1=ALU.add), reads=[ss_b], writes=[ss_b])
                    S.op("pool", lambda e: e.tensor_tensor(out=ss[:, 2:3], in0=ss[:, 3:4], in1=self.K["mhalf"][0][:, 0:1],
                                                           op=ALU.pow), reads=[ss_b, self.K["mhalf"][1]], writes=[ss_b])
                    ot, ot_b = otr.next()
                    for hf in range(2):
                        py, py_b = pys[hf]
                        S.op("dve", lambda e: e.scalar_tensor_tensor(out=ot[:, hf * 512:(hf + 1) * 512], in0=py[:, :],
                                                                     scalar=ss[:, 2:3], in1=gpost[:, hf * 512:(hf + 1) * 512],
                                                                     op0=ALU.mult, op1=ALU.mult),
                             reads=[py_b, ss_b, gpost_b], writes=[ot_b])
                    S.op("pool", lambda e: e.tensor_tensor(out=ot[:], in0=ot[:], in1=xt[:], op=ALU.add),
                         reads=[ot_b, xt_b], writes=[ot_b])
                    S.dma("sp", xo[tok0:tok0 + 128, :], ot[:], reads=[ot_b], writes=[xo_b])


def make_in_maps(inputs, hc=None):
    hc = hc or host_consts()
    pc = pack_params(inputs)
    shared = {}
    for k in ("norm_pre", "norm_post", "w_in", "rw_w_up", "rw_a_up", "rw_vmix_down", "rw_vmix_up",
              "w_up_att", "w_up_rw", "w_out"):
        shared[k] = np.ascontiguousarray(np.asarray(inputs[k], dtype=np.float32))
    shared["pc"] = pc
    for k, v in hc.items():
        shared["c_" + k] = v
    x = np.asarray(inputs["x"], dtype=np.float32)
    maps = []
    for c in range(NCORES):
        m = dict(shared)
        m["x"] = np.ascontiguousarray(x[c])
        maps.append(m)
    return maps


def kernel(**inputs):
    prog = Prog()
    nc = prog.build()
    maps = make_in_maps(inputs)
    res = run_bass_kernel_spmd(nc, maps, core_ids=list(range(NCORES)))
    return np.stack([np.asarray(r["out"], dtype=np.float32) for r in res.results], axis=0)
```

```python
import math
from contextlib import ExitStack
import numpy as np
import ml_dtypes
import concourse.bass as bass
import concourse.mybir as mybir
from concourse.bass_utils import run_bass_kernel_spmd

F32 = mybir.dt.float32
F32R = mybir.dt.float32r
BF16 = mybir.dt.bfloat16
ALU = mybir.AluOpType
AF = mybir.ActivationFunctionType
AX = mybir.AxisListType

D = 1024
T = 2048
DIN = 6272
NL = 2
NCORES = 8
RMS_EPS = 1e-6
GN_EPS = 64e-5
C_ATT_Q, C_ATT_K, C_ATT_V, C_ATT_Z = 0, 512, 1024, 1536
C_RW_R, C_RW_K, C_RW_V, C_RW_WD, C_RW_AD, C_RW_Z = 2048, 2560, 3072, 3584, 3648, 3712
C_G_ATT, C_G_RW = 4224, 5248
PROJ_ROWS = DIN + 32
NEGM = -30000.0
CH = 64
PC_MU, PC_W0, PC_A0, PC_KK, PC_KA, PC_RK, PC_LNG, PC_LNB, PC_VM0, PC_VMU = 0, 13, 17, 21, 25, 29, 33, 37, 41, 45
NPC = 46


class Buf:
    __slots__ = ("name", "w", "rs")

    def __init__(self, name=""):
        self.name = name
        self.w = None
        self.rs = []


class Sched:
    def __init__(self, nc, ndma=10, same_engine_waits=True):
        self.nc = nc
        self.eng = {"pe": nc.tensor, "act": nc.scalar, "dve": nc.vector,
                    "pool": nc.gpsimd, "sp": nc.sync}
        self.stack = []
        self.sem = {}
        self.cnt = {}
        for e in self.eng:
            cm = nc.semaphore("s_" + e)
            self.sem[e] = cm.__enter__()
            self.stack.append(cm)
            self.cnt[e] = 0
        self.dma_sems = {}
        self.dma_rr = {}
        for q in ("sp", "pool", "act"):
            lst = []
            for i in range(ndma):
                key = "d_%s%d" % (q, i)
                cm = nc.semaphore(key)
                self.sem[key] = cm.__enter__()
                self.stack.append(cm)
                self.cnt[key] = 0
                lst.append(key)
            self.dma_sems[q] = lst
            self.dma_rr[q] = 0
        self.seen = {e: {} for e in self.eng}
        self.same = same_engine_waits
        self.n_wait = 0
        self.n_ins = 0

    def _need(self, e, needs, ev):
        if ev is None:
            return
        k, v = ev
        if k == e and (e == "pe" or not self.same):
            return
        if self.seen[e].get(k, 0) >= v:
            return
        if needs.get(k, 0) < v:
            needs[k] = v

    def _collect(self, e, reads, writes):
        needs = {}
        for b in reads:
            self._need(e, needs, b.w)
        for b in writes:
            self._need(e, needs, b.w)
            for r in b.rs:
                self._need(e, needs, r)
        return needs

    def _emit_waits(self, e, needs):
        eng = self.eng[e]
        for k, v in needs.items():
            eng.wait_ge(self.sem[k], v)
            self.seen[e][k] = v
            self.n_wait += 1

    def _commit(self, ev, reads, writes):
        for b in reads:
            b.rs.append(ev)
        for b in writes:
            b.w = ev
            b.rs = []

    def op(self, e, fn, reads=(), writes=()):
        needs = self._collect(e, reads, writes)
        self._emit_waits(e, needs)
        ins = fn(self.eng[e])
        self.cnt[e] += 1
        ins.then_inc(self.sem[e], 1)
        ev = (e, self.cnt[e])
        if e != "pe" and self.same:
            pass
        self._commit(ev, reads, writes)
        self.n_ins += 1
        return ev

    def dma(self, q, out, in_, reads=(), writes=(), **kw):
        lst = self.dma_sems[q]
        key = lst[self.dma_rr[q] % len(lst)]
        self.dma_rr[q] += 1
        needs = self._collect(q, reads, writes)
        if self.cnt[key] > 0:
            self._need(q, needs, (key, self.cnt[key]))
        self._emit_waits(q, needs)
        ins = self.eng[q].dma_start(out=out, in_=in_, **kw)
        self.cnt[key] += 16
        ins.then_inc(self.sem[key], 16)
        ev = (key, self.cnt[key])
        self._commit(ev, reads, writes)
        self.n_ins += 1
        return ev

    def wait_all(self, e, bufs):
        needs = {}
        for b in bufs:
            self._need(e, needs, b.w)
        self._emit_waits(e, needs)

    def barrier(self):
        snap = {k: v for k, v in self.cnt.items() if v > 0}
        for e in self.eng:
            needs = {}
            for k, v in snap.items():
                if k == e:
                    continue
                if self.seen[e].get(k, 0) < v:
                    needs[k] = v
            self._emit_waits(e, needs)

    def close(self):
        for cm in reversed(self.stack):
            cm.__exit__(None, None, None)


_UID = [0]


def _uname(name):
    _UID[0] += 1
    return "%s_%d" % (name, _UID[0])


class Ring:
    def __init__(self, es, nc, name, n, shape, dtype, psum=False):
        self.items = []
        name = _uname(name)
        for i in range(n):
            if psum:
                t = es.enter_context(nc.psum_tensor("%s%d" % (name, i), shape, dtype))
            else:
                t = es.enter_context(nc.sbuf_tensor("%s%d" % (name, i), shape, dtype))
            self.items.append((t, Buf(name + str(i))))
        self.i = 0

    def next(self):
        it = self.items[self.i % len(self.items)]
        self.i += 1
        return it


class PsumRing:
    def __init__(self, es, nc, name, n, width):
        per = 512 // width
        nb = (n + per - 1) // per
        name = _uname(name)
        self.items = []
        for b in range(nb):
            t = es.enter_context(nc.psum_tensor("%s_%d" % (name, b), [128, 512], F32))
            for k in range(per):
                if len(self.items) < n:
                    self.items.append((t[:, k * width:(k + 1) * width], Buf("%s_%d_%d" % (name, b, k))))
        self.i = 0

    def next(self):
        it = self.items[self.i % len(self.items)]
        self.i += 1
        return it


def sb(es, nc, name, shape, dtype):
    name = _uname(name)
    return es.enter_context(nc.sbuf_tensor(name, shape, dtype)), Buf(name)


def host_consts():
    bf = ml_dtypes.bfloat16
    c = {}
    c["ident_f"] = np.eye(128, dtype=np.float32)
    c["ident_b"] = np.eye(128).astype(bf)
    kk, qq = np.meshgrid(np.arange(128), np.arange(128), indexing="ij")
    c["trimask"] = np.where(kk > qq, NEGM, 0.0).astype(bf)
    half = np.arange(128) // 64
    same = (half[:, None] == half[None, :])
    c["blk64"] = same.astype(np.float32)
    loc = np.arange(128) % 64
    su = same & (loc[:, None] < loc[None, :])
    iu = same & (loc[:, None] <= loc[None, :])
    c["mask2"] = np.concatenate([su, iu], axis=1).astype(np.float32)
    c["maskl"] = (same & (loc[:, None] > loc[None, :])).astype(np.float32)
    gs = np.zeros((64, 8, 72), np.float32)
    for n in range(8):
        for m in range(8):
            for qb in range(8):
                if m < qb:
                    gs[n * 8 + m, qb, 64 + n] = 1.0
    c["gsum"] = gs.astype(bf)
    thr = np.zeros((128, 8), np.float32)
    for n in range(8):
        for qb in range(8):
            thr[64 + n, qb] = 2.5 if n < qb else (1e9 if n == qb else -1.0)
    c["thr"] = thr
    pos = np.arange(T)
    hi, lo = pos // 128, pos % 128
    c["qconst"] = np.stack([-128.0 * hi, -1.0 * lo, np.ones(T), np.ones(T)]).astype(bf)
    kc = np.zeros((8, 12, T), np.float32)
    for h in range(8):
        sl = 2.0 ** (-(h + 1))
        for n in range(8):
            kc[h, n] = np.where(pos // 256 == n, NEGM, 0.0)
        kc[h, 8] = sl
        kc[h, 9] = sl
        kc[h, 10] = sl * 128.0 * hi
        kc[h, 11] = sl * lo
    c["kconst"] = kc.astype(bf)
    return c


def pack_params(inp):
    out = np.zeros((NL, 128, NPC), np.float32)

    def put(l, col, vec):
        n = vec.shape[0]
        if n % 128 == 0:
            out[l, :, col:col + n // 128] = vec.reshape(n // 128, 128).T
        else:
            out[l, :n, col] = vec

    for l in range(NL):
        put(l, PC_MU, inp["rw_mu"][l])
        put(l, PC_W0, inp["rw_w0"][l])
        put(l, PC_A0, inp["rw_a0"][l])
        put(l, PC_KK, inp["rw_k_k"][l])
        put(l, PC_KA, inp["rw_k_a"][l])
        put(l, PC_RK, inp["rw_r_k"][l])
        put(l, PC_LNG, inp["rw_ln_g"][l])
        put(l, PC_LNB, inp["rw_ln_b"][l])
        if l >= 1:
            put(l, PC_VM0, inp["rw_vmix0"][l - 1])
            put(l, PC_VMU, inp["rw_vmix_mu"][l - 1])
    return out


class Prog:
    def __init__(self, dbg=(), nlayers=NL, stop_after=None):
        self.dbg = set(dbg)
        self.nlayers = nlayers
        self.stop_after = stop_after
        nc = bass.Bass("TRN2", target_bir_lowering=False)
        self.nc = nc
        self.S = Sched(nc)
        self.inp = {}
        self.scr = {}
        self.dbuf = {}

    def din(self, name, shape, dtype=F32):
        ap = self.nc.dram_tensor(name, list(shape), dtype, kind="ExternalInput").ap()
        self.inp[name] = ap
        self.dbuf[name] = Buf(name)
        return ap

    def dscr(self, name, shape, dtype=F32):
        kind = "ExternalOutput" if name in self.dbg else "Internal"
        ap = self.nc.dram_tensor(name, list(shape), dtype, kind=kind).ap()
        self.scr[name] = ap
        self.dbuf[name] = Buf(name)
        return ap

    def build(self):
        nc, S = self.nc, self.S
        x = self.din("x", [T, D])
        self.din("norm_pre", [NL, D])
        self.din("norm_post", [NL, D])
        self.din("w_in", [NL, D, DIN])
        self.din("rw_w_up", [NL, 64, 512])
        self.din("rw_a_up", [NL, 64, 512])
        self.din("rw_vmix_down", [1, D, 32])
        self.din("rw_vmix_up", [1, 32, 512])
        self.din("w_up_att", [NL, 512, D])
        self.din("w_up_rw", [NL, 512, D])
        self.din("w_out", [NL, D, D])
        self.din("pc", [NL, 128, NPC])
        hc = host_consts()
        for k, v in hc.items():
            self.din("c_" + k, v.shape, BF16 if v.dtype == ml_dtypes.bfloat16 else F32)
        out = self.nc.dram_tensor("out", [T, D], F32, kind="ExternalOutput").ap()
        self.dbuf["out"] = Buf("out")
        self.dscr("projT", [PROJ_ROWS, T])
        self.dscr("vtm", [T, 520], BF16)
        self.dscr("yagT", [512, T], BF16)
        self.dscr("ybT", [512, T], BF16)
        self.dscr("vfirst", [512, T])
        self.dscr("x1", [T, D])

        with ExitStack() as es:
            self.load_consts(es)
            xin, xin_b = x, self.dbuf["x"]
            for l in range(self.nlayers):
                last = (l == self.nlayers - 1)
                xo, xo_b = (out, self.dbuf["out"]) if last else (self.scr["x1"], self.dbuf["x1"])
                self.phase1(l, xin, xin_b)
                if self.stop_after == (l, 1):
                    break
                S.barrier()
                self.phase2(l)
                if self.stop_after == (l, 2):
                    break
                S.barrier()
                self.phase3(l)
                if self.stop_after == (l, 3):
                    break
                S.barrier()
                self.phase4(l, xin, xin_b, xo, xo_b)
                S.barrier()
                xin, xin_b = xo, xo_b
            S.wait_all("sp", list(self.dbuf.values()))
            S.barrier()
        S.close()
        return nc

    def load_consts(self, es):
        nc, S = self.nc, self.S
        self.K = {}
        for name, shape, dt in (("ident_f", [128, 128], F32), ("ident_b", [128, 128], BF16),
                                ("trimask", [128, 128], BF16), ("blk64", [128, 128], F32),
                                ("mask2", [128, 256], F32), ("maskl", [128, 128], F32),
                                ("thr", [128, 8], F32)):
            t, b = sb(es, nc, "k_" + name, shape, dt)
            S.dma("sp", t[:], self.inp["c_" + name][:, :], writes=[b])
            self.K[name] = (t, b)
        t, b = sb(es, nc, "k_gsum", [64, 8, 72], BF16)
        S.dma("sp", t[:], self.inp["c_gsum"][:, :, :], writes=[b])
        self.K["gsum"] = (t, b)
        t, b = sb(es, nc, "k_ones", [128, 64], F32)
        S.op("pool", lambda e: e.memset(t[:], 1.0), writes=[b])
        self.K["ones"] = (t, b)
        t2, b2 = sb(es, nc, "k_pc", [128, NL, NPC], F32)
        S.dma("sp", t2[:], self.inp["pc"].rearrange("l p c -> p l c"), writes=[b2])
        self.K["pc"] = (t2, b2)
        t3, b3 = sb(es, nc, "k_pc1m", [128, NL, NPC], F32)
        S.op("dve", lambda e: e.tensor_scalar(out=t3[:], in0=t2[:], scalar1=-1.0, scalar2=1.0,
                                              op0=ALU.mult, op1=ALU.add), reads=[b2], writes=[b3])
        self.K["pc1m"] = (t3, b3)
        t4, b4 = sb(es, nc, "k_pch", [128, NL, NPC], F32)
        S.op("dve", lambda e: e.tensor_scalar(out=t4[:], in0=t2[:], scalar1=0.5, scalar2=None, op0=ALU.mult),
             reads=[b2], writes=[b4])
        self.K["pch"] = (t4, b4)
        t5, b5 = sb(es, nc, "k_half", [128, 2], F32)
        S.op("pool", lambda e: e.memset(t5[:], 0.5), writes=[b5])
        self.K["half"] = (t5, b5)
        t6, b6 = sb(es, nc, "k_mhalf", [128, 512], F32)
        S.op("pool", lambda e: e.memset(t6[:], -0.5), writes=[b6])
        self.K["mhalf"] = (t6, b6)

    def phase1(self, l, xin, xin_b):
        nc, S = self.nc, self.S
        projT, projT_b = self.scr["projT"], self.dbuf["projT"]
        vtm, vtm_b = self.scr["vtm"], self.dbuf["vtm"]
        identf, identf_b = self.K["ident_f"]
        with ExitStack() as es:
            gpre, gpre_b = sb(es, nc, "p1_gpre", [128, D], F32)
            S.dma("sp", gpre[:], self.inp["norm_pre"][l].partition_broadcast(128), writes=[gpre_b])
            hT, _ = sb(es, nc, "p1_hT", [128, 8, T], BF16)
            hT_b = [Buf("hT%d" % i) for i in range(16)]
            xr = Ring(es, nc, "p1_x", 2, [128, D], F32)
            hr = Ring(es, nc, "p1_h", 2, [128, D], F32)
            junk, junk_b = sb(es, nc, "p1_junk", [128, D], F32)
            ssr = Ring(es, nc, "p1_ss", 4, [128, 2], F32)
            pst = Ring(es, nc, "p1_pst", 2, [128, 512], F32, psum=True)
            psm = Ring(es, nc, "p1_psm", 4, [128, 512], F32, psum=True)
            wst = Ring(es, nc, "p1_wst", 3, [128, 8, 512], F32)
            wbf = Ring(es, nc, "p1_wbf", 3, [128, 8, 512], BF16)
            w_in = self.inp["w_in"][l].rearrange("(dc p) e -> p dc e", p=128)
            w_in_b = self.dbuf["w_in"]
            blocks = [(c0, min(512, DIN - c0)) for c0 in range(0, DIN, 512)]

            def load_block(bi):
                c0, ncol = blocks[bi]
                ws, ws_b = wst.next()
                S.dma("sp", ws[:, 0:4, 0:ncol], w_in[:, 0:4, c0:c0 + ncol], reads=[w_in_b], writes=[ws_b])
                S.dma("sp", ws[:, 4:8, 0:ncol], w_in[:, 4:8, c0:c0 + ncol], reads=[w_in_b], writes=[ws_b])
                wb, wb_b = wbf.next()
                S.op("dve", lambda e: e.tensor_copy(out=wb[:, 0:4, 0:ncol], in_=ws[:, 0:4, 0:ncol]),
                     reads=[ws_b], writes=[wb_b])
                S.op("act", lambda e: e.activation(out=wb[:, 4:8, 0:ncol], in_=ws[:, 4:8, 0:ncol], func=AF.Copy),
                     reads=[ws_b], writes=[wb_b])
                return wb, wb_b

            pending = [load_block(0), load_block(1)]
            for tt in range(16):
                xt, xt_b = xr.next()
                S.dma("pool", xt[:], xin[tt * 128:(tt + 1) * 128, :],
                      reads=[xin_b], writes=[xt_b])
                ss, ss_b = ssr.next()
                S.op("act", lambda e: e.activation(out=junk[:], in_=xt[:], func=AF.Square,
                                                   accum_out=ss[:, 0:1]),
                     reads=[xt_b], writes=[junk_b, ss_b])
                S.op("dve", lambda e: e.tensor_scalar(out=ss[:, 1:2], in0=ss[:, 0:1], scalar1=1.0 / D,
                                                      scalar2=RMS_EPS, op0=ALU.mult, op1=ALU.add),
                     reads=[ss_b], writes=[ss_b])
                S.op("pool", lambda e: e.tensor_tensor(out=ss[:, 0:1], in0=ss[:, 1:2], in1=self.K["mhalf"][0][:, 0:1],
                                                       op=ALU.pow), reads=[ss_b, self.K["mhalf"][1]], writes=[ss_b])
                hf, hf_b = hr.next()
                S.op("dve", lambda e: e.scalar_tensor_tensor(out=hf[:], in0=xt[:], scalar=ss[:, 0:1],
                                                             in1=gpre[:], op0=ALU.mult, op1=ALU.mult),
                     reads=[xt_b, ss_b, gpre_b], writes=[hf_b])
                for half in range(2):
                    ps, ps_b = pst.next()
                    for j in range(4):
                        dc = half * 4 + j
                        S.op("pe", lambda e: e.transpose(ps[:, j * 128:(j + 1) * 128],
                                                         hf[:, dc * 128:(dc + 1) * 128], identf[:]),
                             reads=[hf_b, identf_b], writes=[ps_b])
                    eng = "act" if half == 0 else "dve"
                    dst = hT[:, half * 4:half * 4 + 4, tt * 128:(tt + 1) * 128]
                    src = ps[:, :].rearrange("p (a b) -> p a b", a=4)
                    if eng == "act":
                        S.op("act", lambda e: e.activation(out=dst, in_=src, func=AF.Copy),
                             reads=[ps_b], writes=[hT_b[tt]])
                    else:
                        S.op("dve", lambda e: e.tensor_copy(out=dst, in_=src),
                             reads=[ps_b], writes=[hT_b[tt]])
            stg = Ring(es, nc, "p1_stg", 12, [128, 512], F32)
            vst = Ring(es, nc, "p1_vst", 2, [128, 8, 65], BF16)
            for (vt, vb) in vst.items:
                S.op("pool", lambda e: e.memset(vt[:], 1.0), writes=[vb])
            nev = 0
            for bi, (c0, ncol) in enumerate(blocks):
                wb, wb_b = pending.pop(0)
                if bi + 2 < len(blocks):
                    pending.append(load_block(bi + 2))
                if c0 == C_ATT_V:
                    for tt in range(16):
                        ps, ps_b = psm.next()
                        for dc in range(8):
                            S.op("pe", lambda e: e.matmul(ps[:, :], lhsT=hT[:, dc, tt * 128:(tt + 1) * 128],
                                                          rhs=wb[:, dc, :], start=(dc == 0), stop=(dc == 7)),
                                 reads=[hT_b[tt], wb_b], writes=[ps_b])
                        vt, vt_b = vst.next()
                        src = ps[:, :].rearrange("p (h d) -> p h d", h=8)
                        S.op("dve", lambda e: e.tensor_copy(out=vt[:, :, 0:64], in_=src),
                             reads=[ps_b], writes=[vt_b])
                        S.dma("pool", vtm[tt * 128:(tt + 1) * 128, :], vt[:].rearrange("p h d -> p (h d)"),
                              reads=[vt_b], writes=[vtm_b])
                    continue
                for g in range(ncol // 128):
                    for tc in range(4):
                        ps, ps_b = psm.next()
                        for dc in range(8):
                            S.op("pe", lambda e: e.matmul(ps[:, :], lhsT=wb[:, dc, g * 128:(g + 1) * 128],
                                                          rhs=hT[:, dc, tc * 512:(tc + 1) * 512],
                                                          start=(dc == 0), stop=(dc == 7)),
                                 reads=[wb_b] + hT_b[tc * 4:tc * 4 + 4], writes=[ps_b])
                        st, st_b = stg.next()
                        if nev % 2 == 0:
                            S.op("act", lambda e: e.activation(out=st[:], in_=ps[:, :], func=AF.Copy),
                                 reads=[ps_b], writes=[st_b])
                        else:
                            S.op("dve", lambda e: e.tensor_copy(out=st[:], in_=ps[:, :]),
                                 reads=[ps_b], writes=[st_b])
                        nev += 1
                        r0 = c0 + g * 128
                        S.dma(("pool", "act", "sp")[nev % 3],
                              projT[r0:r0 + 128, tc * 512:(tc + 1) * 512], st[:],
                              reads=[st_b], writes=[projT_b])
            if l >= 1:
                wv, wv_b = sb(es, nc, "p1_wv", [128, 8, 32], F32)
                wvb, wvb_b = sb(es, nc, "p1_wvb", [128, 8, 32], BF16)
                S.dma("sp", wv[:], self.inp["rw_vmix_down"][l - 1].rearrange("(dc p) e -> p dc e", p=128),
                      writes=[wv_b])
                S.op("dve", lambda e: e.tensor_copy(out=wvb[:], in_=wv[:]), reads=[wv_b], writes=[wvb_b])
                for tc in range(4):
                    ps, ps_b = psm.next()
                    for dc in range(8):
                        S.op("pe", lambda e: e.matmul(ps[0:32, :], lhsT=wvb[:, dc, :],
                                                      rhs=hT[:, dc, tc * 512:(tc + 1) * 512],
                                                      start=(dc == 0), stop=(dc == 7)),
                             reads=[wvb_b] + hT_b[tc * 4:tc * 4 + 4], writes=[ps_b])
                    st, st_b = stg.next()
                    S.op("dve", lambda e: e.tensor_copy(out=st[0:32, :], in_=ps[0:32, :]),
                         reads=[ps_b], writes=[st_b])
                    S.dma("sp", projT[DIN:DIN + 32, tc * 512:(tc + 1) * 512], st[0:32, :],
                          reads=[st_b], writes=[projT_b])

    def phase2(self, l):
        nc, S = self.nc, self.S
        projT, projT_b = self.scr["projT"], self.dbuf["projT"]
        vtm, vtm_b = self.scr["vtm"], self.dbuf["vtm"]
        yagT, yagT_b = self.scr["yagT"], self.dbuf["yagT"]
        identb, identb_b = self.K["ident_b"]
        trim, trim_b = self.K["trimask"]
        gsum, gsum_b = self.K["gsum"]
        thr, thr_b = self.K["thr"]
        ones, ones_b = self.K["ones"]
        with ExitStack() as es:
            vext, vext_b = sb(es, nc, "p2_vext", [128, 16, 520], BF16)
            S.dma("pool", vext[:], vtm.rearrange("(t p) c -> p t c", p=128), reads=[vtm_b], writes=[vext_b])
            slots = []
            for i in range(2):
                qaug_, _ = sb(es, nc, "p2_qaug", [128, T], BF16)
                kaug_, _ = sb(es, nc, "p2_kaug", [128, T], BF16)
                sl = dict(qaug=qaug_, kaug=kaug_, qa_q=Buf("qa_q"), qa_n=[Buf("qa_n%d" % k) for k in range(4)],
                          qa_c=Buf("qa_c"), ka_k=Buf("ka_k"), ka_c=Buf("ka_c"))
                S.dma("sp", qaug_[72:76, :], self.inp["c_qconst"][:, :], writes=[sl["qa_c"]])
                slots.append(sl)
            qfr = Ring(es, nc, "p2_qf", 2, [64, T], F32)
            kfr = Ring(es, nc, "p2_kf", 2, [64, T], F32)
            azr = Ring(es, nc, "p2_az", 2, [64, T], F32)
            szr = Ring(es, nc, "p2_sz", 2, [64, T], F32)
            kmr = Ring(es, nc, "p2_kmean", 2, [64, 8], F32)
            kdr = Ring(es, nc, "p2_kdiff", 2, [64, 8, 8], F32)
            indr = Ring(es, nc, "p2_ind", 2, [64, 512], BF16)
            ptr = Ring(es, nc, "p2_pt", 3, [128, 512], BF16)
            rden, rden_b = sb(es, nc, "p2_rden", [128, 512], F32)
            bcs, bcs_b = sb(es, nc, "p2_bcs", [64, 512], F32)
            yac, yac_b = sb(es, nc, "p2_yac", [64, 512], F32)
            yagr = Ring(es, nc, "p2_yag", 2, [64, T], BF16)
            ps_s = Ring(es, nc, "p2_pss", 4, [128, 512], F32, psum=True)
            ps_o = Ring(es, nc, "p2_pso", 2, [128, 512], F32, psum=True)
            ps_m = Ring(es, nc, "p2_psm", 2, [128, 512], F32, psum=True)
            deferred = []

            def setup(h):
                sl = slots[h % 2]
                qaug, kaug = sl["qaug"], sl["kaug"]
                qf, qf_b = qfr.next()
                kf, kf_b = kfr.next()
                azf, azf_b = azr.next()
                S.dma("sp", qf[:], projT[C_ATT_Q + h * 64:C_ATT_Q + (h + 1) * 64, :], reads=[projT_b], writes=[qf_b])
                S.dma("pool", kf[:], projT[C_ATT_K + h * 64:C_ATT_K + (h + 1) * 64, :], reads=[projT_b], writes=[kf_b])
                S.dma("sp", azf[:], projT[C_ATT_Z + h * 64:C_ATT_Z + (h + 1) * 64, :], reads=[projT_b], writes=[azf_b])
                S.dma("pool", kaug[64:76, :], self.inp["c_kconst"][h], writes=[sl["ka_c"]])
                S.op("pool", lambda e: e.tensor_copy(out=kaug[0:64, :], in_=kf[:]), reads=[kf_b], writes=[sl["ka_k"]])
                S.op("act", lambda e: e.activation(out=qaug[0:64, :], in_=qf[:], func=AF.Copy, scale=0.125),
                     reads=[qf_b], writes=[sl["qa_q"]])
                sz, sz_b = szr.next()
                S.op("act", lambda e: e.activation(out=sz[:], in_=azf[:], func=AF.Tanh, scale=0.5), reads=[azf_b], writes=[sz_b])
                S.op("dve", lambda e: e.scalar_tensor_tensor(out=sz[:], in0=sz[:], scalar=1.0, in1=azf[:],
                                                             op0=ALU.add, op1=ALU.mult),
                     reads=[sz_b, azf_b], writes=[sz_b])
                kmean, kmean_b = kmr.next()
                kdiff, kdiff_b = kdr.next()
                S.op("dve", lambda e: e.reduce_sum(out=kmean[:], in_=kf[:].rearrange("p (n k) -> p n k", k=256),
                                                   axis=AX.X), reads=[kf_b], writes=[kmean_b])
                S.op("dve", lambda e: e.tensor_tensor(out=kdiff[:], in0=kmean[:, :].unsqueeze(1).to_broadcast([64, 8, 8]),
                                                      in1=kmean[:, :].unsqueeze(2).to_broadcast([64, 8, 8]),
                                                      op=ALU.subtract), reads=[kmean_b], writes=[kdiff_b])
                return dict(sl=sl, qf=qf, qf_b=qf_b, sz=sz, sz_b=sz_b, kdiff=kdiff, kdiff_b=kdiff_b)

            nxt_setup = setup(0)
            for h in range(8):
                st_ = nxt_setup
                sl = st_["sl"]
                qaug, kaug = sl["qaug"], sl["kaug"]
                qa_q, qa_n, qa_c, ka_k, ka_c = sl["qa_q"], sl["qa_n"], sl["qa_c"], sl["ka_k"], sl["ka_c"]
                qf, qf_b, sz, sz_b = st_["qf"], st_["qf_b"], st_["sz"], st_["sz_b"]
                kdiff, kdiff_b = st_["kdiff"], st_["kdiff_b"]
                yag, yag_b = yagr.next()
                for c in range(4):
                    if c == 2 and h + 1 < 8:
                        nxt_setup = setup(h + 1)
                    pg, pg_b = ps_m.next()
                    S.op("pe", lambda e: e.matmul(pg[0:64, :], lhsT=kdiff[:].rearrange("p n m -> p (n m)"),
                                                  rhs=qf[:, c * 512:(c + 1) * 512], start=True, stop=True),
                         reads=[kdiff_b, qf_b], writes=[pg_b])
                    ind, ind_b = indr.next()
                    S.op("dve", lambda e: e.tensor_single_scalar(out=ind[:], in_=pg[0:64, :], scalar=0.0, op=ALU.is_gt),
                         reads=[pg_b], writes=[ind_b])
                    pr, pr_b = ps_m.next()
                    for j in range(2):
                        qb = 2 * c + j
                        S.op("pe", lambda e: e.matmul(pr[0:72, j * 256:(j + 1) * 256], lhsT=gsum[:, qb, :],
                                                      rhs=ind[:, j * 256:(j + 1) * 256], start=True, stop=True),
                             reads=[gsum_b, ind_b], writes=[pr_b])
                    for j in range(2):
                        qb = 2 * c + j
                        S.op("dve", lambda e: e.tensor_scalar(out=qaug[64:72, qb * 256:(qb + 1) * 256],
                                                              in0=pr[64:72, j * 256:(j + 1) * 256],
                                                              scalar1=thr[64:72, qb:qb + 1], scalar2=None,
                                                              op0=ALU.is_ge),
                             reads=[pr_b, thr_b], writes=[qa_n[c]])
                    po, po_b = ps_o.next()
                    nkt = 4 * c + 4

                    def qk(kt, c=c, h=h):
                        j = kt - 4 * c
                        off = 0 if j < 0 else j * 128
                        n = 512 - off
                        q0 = c * 512 + off
                        pss, pss_b = ps_s.next()
                        S.op("pe", lambda e: e.matmul(pss[:, 0:n], lhsT=kaug[0:76, kt * 128:(kt + 1) * 128],
                                                      rhs=qaug[0:76, q0:q0 + n], start=True, stop=(j < 0)),
                             reads=[ka_k, ka_c, qa_q, qa_n[c], qa_c], writes=[pss_b])
                        if j >= 0:
                            S.op("pe", lambda e: e.matmul(pss[:, 0:128], lhsT=identb[:], rhs=trim[:],
                                                          start=False, stop=True),
                                 reads=[identb_b, trim_b], writes=[pss_b])
                        return pss, pss_b, off, n

                    def finalize(po=po, po_b=po_b, c=c, h=h, yag=yag, yag_b=yag_b, sz=sz, sz_b=sz_b, last=(c == 3)):
                        S.op("dve", lambda e: e.reciprocal(out=rden[64:65, :], in_=po[64:65, :]),
                             reads=[po_b], writes=[rden_b])
                        pb, pb_b = ps_m.next()
                        S.op("pe", lambda e: e.matmul(pb[0:64, :], lhsT=ones[64:65, 0:64], rhs=rden[64:65, :],
                                                      start=True, stop=True), reads=[ones_b, rden_b], writes=[pb_b])
                        S.op("act", lambda e: e.activation(out=bcs[:], in_=pb[0:64, :], func=AF.Copy),
                             reads=[pb_b], writes=[bcs_b])
                        S.op("dve", lambda e: e.scalar_tensor_tensor(out=yac[:], in0=po[0:64, :], scalar=0.5, in1=bcs[:],
                                                                     op0=ALU.mult, op1=ALU.mult),
                             reads=[po_b, bcs_b], writes=[yac_b])
                        S.op("pool", lambda e: e.tensor_tensor(out=yag[:, c * 512:(c + 1) * 512], in0=yac[:],
                                                               in1=sz[:, c * 512:(c + 1) * 512], op=ALU.mult),
                             reads=[yac_b, sz_b], writes=[yag_b])
                        if last:
                            S.dma("sp", yagT[h * 64:(h + 1) * 64, :], yag[:], reads=[yag_b], writes=[yagT_b])

                    pend = [qk(0), qk(1)]
                    for kt in range(nkt):
                        if kt + 2 < nkt:
                            pend.append(qk(kt + 2))
                        if kt == 1 and deferred:
                            deferred.pop()()
                        pss, pss_b, off, n = pend.pop(0)
                        pt, pt_b = ptr.next()
                        S.op("act", lambda e: e.activation(out=pt[:, 0:n], in_=pss[:, 0:n], func=AF.Exp),
                             reads=[pss_b], writes=[pt_b])
                        S.op("pe", lambda e: e.matmul(po[0:65, off:512], lhsT=vext[:, kt, h * 65:(h + 1) * 65],
                                                      rhs=pt[:, 0:n], start=(kt == 0), stop=(kt == nkt - 1)),
                             reads=[vext_b, pt_b], writes=[po_b])
                    deferred.append(finalize)
            while deferred:
                deferred.pop()()

    def phase3_gen(self, l, es, TH=256):
        nc, S = self.nc, self.S
        projT, projT_b = self.scr["projT"], self.dbuf["projT"]
        ybT, ybT_b = self.scr["ybT"], self.dbuf["ybT"]
        vfirst, vfirst_b = self.scr["vfirst"], self.dbuf["vfirst"]
        identf, identf_b = self.K["ident_f"]
        blk64, blk64_b = self.K["blk64"]
        mask2, mask2_b = self.K["mask2"]
        maskl, maskl_b = self.K["maskl"]
        pc, pc_b = self.K["pc"]
        pc1m, pc1m_b = self.K["pc1m"]
        NCH = TH // CH
        C0 = math.exp(-0.5)
        PE_PER_B = 6
        R32 = F32R
        CARVE = False

        def col(t, c):
            return t[:, l, c:c + 1]

        wau, wau_b = sb(es, nc, "p3_wau", [128, 512], F32)
        S.dma("sp", wau[0:64, :], self.inp["rw_w_up"][l], writes=[wau_b])
        S.dma("sp", wau[64:128, :], self.inp["rw_a_up"][l], writes=[wau_b])
        if l >= 1:
            vmu, vmu_b = sb(es, nc, "p3_vmu", [32, 512], F32)
            S.dma("sp", vmu[:], self.inp["rw_vmix_up"][l - 1], writes=[vmu_b])
        idr, idr_b = sb(es, nc, "p3_idr", [128, 128], R32)
        S.op("dve", lambda e: e.tensor_copy(out=idr[:], in_=identf[:]), reads=[identf_b], writes=[idr_b])
        rings = {}

        def R(name, n=1, shape=None, dt=F32):
            if name not in rings:
                rings[name] = Ring(es, nc, "p3_" + name, n, list(shape or (128, TH)), dt)
            return rings[name].next()

        zf, zf_b = sb(es, nc, "p3_zf", [128, 2], F32)
        S.op("pool", lambda e: e.memset(zf[:], 0.0), writes=[zf_b])
        ARr = Ring(es, nc, "p3_AR", 4, [128, NCH * 256], R32)
        BKr = Ring(es, nc, "p3_BK", 3, [128, NCH * 256], R32)
        BVr = Ring(es, nc, "p3_BV", 3, [128, NCH * 384], R32)
        Wn2 = [Ring(es, nc, "p3_W%d" % i, 2 * NCH, [128, 384], R32) for i in range(2)]
        for rg, pat, kw in ((ARr, "p (c a q t) -> p c a q t", dict(c=NCH, a=2, q=2)),
                            (BKr, "p (c a q t) -> p c a q t", dict(c=NCH, a=2, q=2)),
                            (BVr, "p (c a q t) -> p c a q t", dict(c=NCH, a=3, q=2)),
                            (Wn2[0], "p (a b) -> p a b", dict(a=3)),
                            (Wn2[1], "p (a b) -> p a b", dict(a=3))):
            new_items = []
            for (t_, b_) in rg.items:
                n_ = t_[:].shape[1]
                S.op("dve", lambda e: e.tensor_copy(out=t_[:, :], in_=zf[:, 0:1].to_broadcast([128, n_])),
                     reads=[zf_b], writes=[b_])
                new_items.append((t_[:, :].rearrange(pat, **kw), b_))
            rg.items = new_items
        PTr2 = [Ring(es, nc, "p3_PT%d" % i, 2 * NCH, [128, 128], R32) for i in range(2)]
        NM1r = Ring(es, nc, "p3_NM1", 3 * NCH, [128, 256], R32)
        NM2r = Ring(es, nc, "p3_NM2", 3 * NCH, [128, 256], R32)
        NbTr2 = [Ring(es, nc, "p3_NbT%d" % i, NCH, [128, 128], R32) for i in range(2)]
        Tfr = Ring(es, nc, "p3_Tf", 3 * NCH, [128, 128], R32)
        TM3r = Ring(es, nc, "p3_TM3", 3 * NCH, [128, 3, 128], R32)
        W1r = Ring(es, nc, "p3_W1", 2, [128, 128], R32)
        UTr = Ring(es, nc, "p3_UT", 2, [128, 128], R32)
        Sr = Ring(es, nc, "p3_S", 2, [128, 128], R32)
        if CARVE:
            psA = PsumRing(es, nc, "p3_psA", 4, 256)
            psB = PsumRing(es, nc, "p3_psB", 8, 128)
        else:
            psA = PsumRing(es, nc, "p3_psA", 2, 512)
            psB = PsumRing(es, nc, "p3_psB", 4, 512)
            psA.items = [(a[:, 0:256], b) for a, b in psA.items]
            psB.items = [(a[:, 0:128], b) for a, b in psB.items]
        psC = PsumRing(es, nc, "p3_psC", 2, 512)

        pch, pch_b = self.K["pch"]
        half, half_b = self.K["half"]
        mhalf, mhalf_b = self.K["mhalf"]

        def shift(dst, dst_b, X, X_b, mucol, npart=128, eng="pool"):
            tmp, tmp_b = R("shtmp", 2)
            S.op("act", lambda e: e.activation(out=dst[0:npart, :], in_=X[0:npart, 1:TH + 1], func=AF.Copy,
                                               scale=col(pc1m, mucol)[0:npart]),
                 reads=[X_b, pc1m_b], writes=[dst_b])
            S.op("act", lambda e: e.activation(out=tmp[0:npart, :], in_=X[0:npart, 0:TH], func=AF.Copy,
                                               scale=col(pc, mucol)[0:npart]),
                 reads=[X_b, pc_b], writes=[tmp_b])
            S.op("pool", lambda e: e.tensor_tensor(out=dst[0:npart, :], in0=dst[0:npart, :], in1=tmp[0:npart, :],
                                                   op=ALU.add), reads=[dst_b, tmp_b], writes=[dst_b])

        def sigm(dst, dst_b, src, src_b, bcol):
            S.op("act", lambda e: e.activation(out=dst[:], in_=src, func=AF.Tanh, scale=0.5, bias=col(pch, bcol)),
                 reads=[src_b, pch_b], writes=[dst_b])
            S.op("act", lambda e: e.activation(out=dst[:], in_=dst[:], func=AF.Identity, scale=0.5, bias=half[:, 0:1]),
                 reads=[dst_b, half_b], writes=[dst_b])

        def load_shifted(name, row0, nrows, t0, q):
            X, X_b = R("X" + name, 3, (128, TH + 1))
            if t0 == 0:
                S.op("pool", lambda e: e.memset(X[0:nrows, 0:1], 0.0), writes=[X_b])
                S.dma(q, X[0:nrows, 1:TH + 1], projT[row0:row0 + nrows, 0:TH], reads=[projT_b], writes=[X_b])
            else:
                S.dma(q, X[0:nrows, :], projT[row0:row0 + nrows, t0 - 1:t0 + TH], reads=[projT_b], writes=[X_b])
            return X, X_b

        blocks = [(j, tb) for j in range(4) for tb in range(T // TH)]
        preloaded = {}

        def issue_loads(bi):
            j, tb = blocks[bi]
            t0 = tb * TH
            d = {}
            d["r"] = load_shifted("r", C_RW_R + j * 128, 128, t0, "sp")
            d["k"] = load_shifted("k", C_RW_K + j * 128, 128, t0, "pool")
            d["v"] = load_shifted("v", C_RW_V + j * 128, 128, t0, "sp")
            d["w"] = load_shifted("w", C_RW_WD, 128, t0, "pool")
            Xz, Xz_b = R("Xz", 5)
            S.dma("sp", Xz[:], projT[C_RW_Z + j * 128:C_RW_Z + (j + 1) * 128, t0:t0 + TH],
                  reads=[projT_b], writes=[Xz_b])
            d["z"] = (Xz, Xz_b)
            if l >= 1:
                d["m"] = load_shifted("m", DIN, 32, t0, "sp")
                vf, vf_b = R("vf", 2)
                S.dma("pool", vf[:], vfirst[j * 128:(j + 1) * 128, t0:t0 + TH], reads=[vfirst_b], writes=[vf_b])
                d["vf"] = (vf, vf_b)
            preloaded[bi] = d

        def prepA(j, tb, par, bi):
            t0 = tb * TH
            ctx = {}
            Wn, PTr, NbTr = Wn2[par], PTr2[par], NbTr2[par]
            if bi not in preloaded:
                issue_loads(bi)
            if bi + 1 < len(blocks):
                issue_loads(bi + 1)
            ld = preloaded.pop(bi)
            (Xr, Xr_b), (Xk, Xk_b), (Xv, Xv_b), (Xw, Xw_b), (Xz, Xz_b) = ld["r"], ld["k"], ld["v"], ld["w"], ld["z"]
            yield
            rs, rs_b = R("rs")
            ks, ks_b = R("ks")
            vs, vs_b = R("vs", 2)
            was, was_b = R("was")
            shift(rs, rs_b, Xr, Xr_b, PC_MU + j)
            shift(ks, ks_b, Xk, Xk_b, PC_MU + 4 + j)
            yield
            shift(vs, vs_b, Xv, Xv_b, PC_MU + 8 + j, eng="pool")
            shift(was, was_b, Xw, Xw_b, PC_MU + 12, eng="pool")
            yield
            S.op("act", lambda e: e.activation(out=was[0:64, :], in_=was[0:64, :], func=AF.Tanh),
                 reads=[was_b], writes=[was_b])
            pz, pz_b = psC.next()
            S.op("pe", lambda e: e.matmul(pz[:, 0:TH], lhsT=wau[0:64, j * 128:(j + 1) * 128], rhs=was[0:64, :],
                                          start=True, stop=True), reads=[wau_b, was_b], writes=[pz_b])
            sg, sg_b = R("sg")
            sigm(sg, sg_b, pz[:, 0:TH], pz_b, PC_W0 + j)
            pa, pa_b = psC.next()
            S.op("pe", lambda e: e.matmul(pa[:, 0:TH], lhsT=wau[64:128, j * 128:(j + 1) * 128],
                                          rhs=was[64:128, :], start=True, stop=True),
                 reads=[wau_b, was_b], writes=[pa_b])
            aic, aic_b = R("aic")
            sigm(aic, aic_b, pa[:, 0:TH], pa_b, PC_A0 + j)
            yield
            if l == 0:
                vr, vr_b = vs, vs_b
                S.dma("pool", vfirst[j * 128:(j + 1) * 128, t0:t0 + TH], vs[:], reads=[vs_b], writes=[vfirst_b])
            else:
                Xm, Xm_b = ld["m"]
                vms, vms_b = R("vms")
                shift(vms, vms_b, Xm, Xm_b, PC_VMU, npart=32, eng="pool")
                pv, pv_b = psC.next()
                S.op("pe", lambda e: e.matmul(pv[:, 0:TH], lhsT=vmu[0:32, j * 128:(j + 1) * 128],
                                              rhs=vms[0:32, :], start=True, stop=True),
                     reads=[vmu_b, vms_b], writes=[pv_b])
                gt, gt_b = R("gt")
                sigm(gt, gt_b, pv[:, 0:TH], pv_b, PC_VM0 + j)
                vf, vf_b = ld["vf"]
                S.op("pool", lambda e: e.tensor_tensor(out=vf[:], in0=vf[:], in1=vs[:], op=ALU.subtract),
                     reads=[vf_b, vs_b], writes=[vf_b])
                S.op("pool", lambda e: e.tensor_tensor(out=vf[:], in0=vf[:], in1=gt[:], op=ALU.mult),
                     reads=[vf_b, gt_b], writes=[vf_b])
                vr, vr_b = R("vr", 2)
                S.op("pool", lambda e: e.tensor_tensor(out=vr[:], in0=vf[:], in1=vs[:], op=ALU.add),
                     reads=[vf_b, vs_b], writes=[vr_b])
            yield
            kk, kk_b = R("kk")
            S.op("act", lambda e: e.activation(out=kk[:], in_=ks[:], func=AF.Copy, scale=col(pc, PC_KK + j)),
                 reads=[ks_b, pc_b], writes=[kk_b])
            sq, sq_b = R("sq")
            S.op("pool", lambda e: e.tensor_tensor(out=sq[:], in0=kk[:], in1=kk[:], op=ALU.mult),
                 reads=[kk_b], writes=[sq_b])
            pq, pq_b = psC.next()
            S.op("pe", lambda e: e.matmul(pq[:, 0:TH], lhsT=blk64[:], rhs=sq[:], start=True, stop=True),
                 reads=[blk64_b, sq_b], writes=[pq_b])
            rn, rn_b = R("rn")
            S.op("dve", lambda e: e.tensor_scalar_max(out=rn[:], in0=pq[:, 0:TH], scalar1=1e-24),
                 reads=[pq_b], writes=[rn_b])
            S.op("dve", lambda e: e.reciprocal(out=rn[:], in_=rn[:]), reads=[rn_b], writes=[rn_b])
            bv, bv_b = R("bv")
            S.op("pool", lambda e: e.tensor_tensor(out=bv[:], in0=kk[:], in1=aic[:], op=ALU.mult),
                 reads=[kk_b, aic_b], writes=[bv_b])
            S.op("pool", lambda e: e.tensor_tensor(out=kk[:], in0=kk[:], in1=rn[:], op=ALU.mult),
                 reads=[kk_b, rn_b], writes=[kk_b])
            yield
            km, km_b = R("km")
            S.op("dve", lambda e: e.tensor_scalar(out=km[:], in0=aic[:], scalar1=col(pc, PC_KA + j),
                                                  scalar2=col(pc1m, PC_KA + j), op0=ALU.mult, op1=ALU.add),
                 reads=[aic_b, pc_b, pc1m_b], writes=[km_b])
            S.op("dve", lambda e: e.tensor_tensor(out=km[:], in0=km[:], in1=ks[:], op=ALU.mult),
                 reads=[km_b, ks_b], writes=[km_b])
            S.op("dve", lambda e: e.scalar_tensor_tensor(out=sq[:], in0=rs[:], scalar=col(pc, PC_RK + j),
                                                         in1=km[:], op0=ALU.mult, op1=ALU.mult),
                 reads=[rs_b, pc_b, km_b, sq_b], writes=[sq_b])
            pb, pb_b = psC.next()
            S.op("pe", lambda e: e.matmul(pb[:, 0:TH], lhsT=blk64[:], rhs=sq[:], start=True, stop=True),
                 reads=[blk64_b, sq_b], writes=[pb_b])
            bon, bon_b = R("bon", 4)
            S.op("dve", lambda e: e.tensor_tensor(out=bon[:], in0=pb[:, 0:TH], in1=vr[:], op=ALU.mult),
                 reads=[pb_b, vr_b], writes=[bon_b])
            yield
            G, G_b = R("G")
            S.op("dve", lambda e: e.tensor_tensor_scan(out=G[:], data0=sg[:], data1=sg[:], initial=0.0,
                                                       op0=ALU.add, op1=ALU.bypass), reads=[sg_b], writes=[G_b])
            Gs, Gs_b = R("Gs", 1, (128, NCH))
            S.op("pool", lambda e: e.memset(Gs[:, 0:1], 0.0), writes=[Gs_b])
            G3 = G[:, :].rearrange("p (c t) -> p c t", t=CH)
            S.op("dve", lambda e: e.tensor_copy(out=Gs[:, 1:NCH], in_=G3[:, 0:NCH - 1, CH - 1]),
                 reads=[G_b], writes=[Gs_b])
            csp, csp_b = R("csp")
            csp3 = csp[:, :].rearrange("p (c t) -> p c t", t=CH)
            S.op("dve", lambda e: e.tensor_tensor(out=csp3, in0=G3,
                                                  in1=Gs[:, :].unsqueeze(2).to_broadcast([128, NCH, CH]),
                                                  op=ALU.subtract), reads=[G_b, Gs_b], writes=[csp_b])
            Ep, Ep_b = R("Ep", 4)
            Em, Em_b = R("Em")
            Eq, Eq_b = R("Eq")
            S.op("act", lambda e: e.activation(out=Ep[:], in_=csp[:], func=AF.Exp, scale=-C0),
                 reads=[csp_b], writes=[Ep_b])
            S.op("act", lambda e: e.activation(out=Em[:], in_=csp[:], func=AF.Exp, scale=C0),
                 reads=[csp_b], writes=[Em_b])
            S.op("pool", lambda e: e.tensor_tensor(out=Eq[:], in0=csp[:], in1=sg[:], op=ALU.subtract),
                 reads=[csp_b, sg_b], writes=[Eq_b])
            S.op("act", lambda e: e.activation(out=Eq[:], in_=Eq[:], func=AF.Exp, scale=-C0),
                 reads=[Eq_b], writes=[Eq_b])
            yield
            Ep3 = Ep[:, :].rearrange("p (c t) -> p c t", t=CH)
            AR, AR_b = ARr.next()
            BK, BK_b = BKr.next()
            BV, BV_b = BVr.next()

            def v3(t_, p):
                return t_[p * 64:(p + 1) * 64, :].rearrange("p (c t) -> p c t", t=CH)

            for p in range(2):
                hs = slice(p * 64, (p + 1) * 64)
                S.op("dve", lambda e: e.scalar_tensor_tensor(out=AR[hs, :, 0, p, :], in0=v3(kk, p), scalar=-1.0,
                                                             in1=v3(Eq, p), op0=ALU.mult, op1=ALU.mult),
                     reads=[kk_b, Eq_b], writes=[AR_b])
                S.op("dve", lambda e: e.tensor_tensor(out=AR[hs, :, 1, p, :], in0=v3(rs, p), in1=v3(Ep, p),
                                                      op=ALU.mult), reads=[rs_b, Ep_b], writes=[AR_b])
                S.op("dve", lambda e: e.tensor_tensor(out=BK[hs, :, 0, p, :], in0=v3(bv, p), in1=v3(Em, p),
                                                      op=ALU.mult), reads=[bv_b, Em_b], writes=[BK_b])
                S.op("dve", lambda e: e.tensor_tensor(out=BK[hs, :, 1, p, :], in0=v3(km, p), in1=v3(Em, p),
                                                      op=ALU.mult), reads=[km_b, Em_b], writes=[BK_b])
                gcb = Ep3[hs, :, CH - 1:CH].to_broadcast([64, NCH, CH])
                S.op("dve", lambda e: e.tensor_tensor(out=BV[hs, :, 0, p, :], in0=BK[hs, :, 0, p, :], in1=gcb,
                                                      op=ALU.mult), reads=[BK_b, Ep_b], writes=[BV_b])
                S.op("dve", lambda e: e.tensor_tensor(out=BV[hs, :, 1, p, :], in0=BK[hs, :, 1, p, :], in1=gcb,
                                                      op=ALU.mult), reads=[BK_b, Ep_b], writes=[BV_b])
                S.op("dve", lambda e: e.tensor_copy(out=BV[hs, :, 2, p, :], in_=v3(vr, p)),
                     reads=[vr_b], writes=[BV_b])
                yield
            yield "A"
            ch = []
            for c in range(NCH):
                ARc = AR[:, c].rearrange("p a q t -> p (a q t)")
                d = {"ARc": ARc, "Abd": ARc[:, 0:128], "Rbd": ARc[:, 128:256],
                     "Bbd": BK[:, c, 0].rearrange("p q t -> p (q t)"),
                     "Kbd": BK[:, c, 1].rearrange("p q t -> p (q t)")}
                ch.append(d)
            for d in ch:
                p1, p1_b = psA.next()
                S.op("pe", lambda e: e.matmul(p1[:, 0:256], lhsT=d["Bbd"], rhs=d["ARc"], start=True, stop=True),
                     reads=[BK_b, AR_b], writes=[p1_b])
                d["NM1"], d["NM1_b"] = NM1r.next()
                S.op("dve", lambda e: e.tensor_tensor(out=d["NM1"][:], in0=p1[:, 0:256], in1=mask2[:], op=ALU.mult),
                     reads=[p1_b, mask2_b], writes=[d["NM1_b"]])
            yield
            for d in ch:
                p2, p2_b = psA.next()
                S.op("pe", lambda e: e.matmul(p2[:, 0:256], lhsT=d["Kbd"], rhs=d["ARc"], start=True, stop=True),
                     reads=[BK_b, AR_b], writes=[p2_b])
                d["NM2"], d["NM2_b"] = NM2r.next()
                S.op("dve", lambda e: e.tensor_tensor(out=d["NM2"][:], in0=p2[:, 0:256], in1=mask2[:], op=ALU.mult),
                     reads=[p2_b, mask2_b], writes=[d["NM2_b"]])
            yield
            for d in ch:
                p3, p3_b = psB.next()
                S.op("pe", lambda e: e.matmul(p3[:, :], lhsT=d["Abd"], rhs=d["Bbd"], start=True, stop=True),
                     reads=[AR_b, BK_b], writes=[p3_b])
                d["NbT"], d["NbT_b"] = NbTr.next()
                S.op("dve", lambda e: e.tensor_tensor(out=d["NbT"][:], in0=p3[:, :], in1=maskl[:], op=ALU.mult),
                     reads=[p3_b, maskl_b], writes=[d["NbT_b"]])
            yield
            for d in ch:
                Nba = d["NM1"][:, 0:128]
                d["W"], d["W_b"] = Wn.next()
                W = d["W"]
                S.op("pool", lambda e: e.tensor_tensor(out=W[:, 2, :], in0=Nba, in1=idr[:], op=ALU.add),
                     reads=[d["NM1_b"], idr_b], writes=[d["W_b"]])
                p4, p4_b = psB.next()
                S.op("pe", lambda e: e.matmul(p4[:, :], lhsT=d["NbT"][:], rhs=Nba, start=True, stop=True),
                     reads=[d["NbT_b"], d["NM1_b"]], writes=[p4_b])
                S.op("act", lambda e: e.activation(out=W[:, 0, :], in_=p4[:, :], func=AF.Copy),
                     reads=[p4_b], writes=[d["W_b"]])
                p5, p5_b = psB.next()
                S.op("pe", lambda e: e.matmul(p5[:, :], lhsT=Nba, rhs=d["NbT"][:], start=True, stop=True),
                     reads=[d["NbT_b"], d["NM1_b"]], writes=[p5_b])
                d["PT"], d["PT_b"] = PTr.next()
                PT = d["PT"]
                S.op("act", lambda e: e.activation(out=PT[:], in_=p5[:, :], func=AF.Copy),
                     reads=[p5_b], writes=[d["PT_b"]])
            yield
            for m in (2, 4, 8, 16, 32):
                for d in ch:
                    W, W_b, PT, PT_b = d["W"], d["W_b"], d["PT"], d["PT_b"]
                    pw, pw_b = psA.next()
                    S.op("pe", lambda e: e.matmul(pw[:, 0:256].rearrange("p (a b) -> p a b", a=2), lhsT=PT[:],
                                                  rhs=W[:, 0:3:2, :], start=True, stop=True),
                         reads=[PT_b, W_b], writes=[pw_b])
                    if m < 32:
                        W2, W2_b = Wn.next()
                        S.op("dve", lambda e: e.tensor_tensor(out=W2[:, 0:3:2, :],
                                                              in0=pw[:, 0:256].rearrange("p (a b) -> p a b", a=2),
                                                              in1=W[:, 1:3, :], op=ALU.add),
                             reads=[pw_b, W_b], writes=[W2_b])
                        pq2, pq2_b = psB.next()
                        S.op("pe", lambda e: e.matmul(pq2[:, :], lhsT=W[:, 0, :], rhs=PT[:], start=True, stop=True),
                             reads=[W_b, PT_b], writes=[pq2_b])
                        PT2, PT2_b = PTr.next()
                        S.op("act", lambda e: e.activation(out=PT2[:], in_=pq2[:, :], func=AF.Copy),
                             reads=[pq2_b], writes=[PT2_b])
                        d["W"], d["W_b"], d["PT"], d["PT_b"] = W2, W2_b, PT2, PT2_b
                    else:
                        d["Tf"], d["Tf_b"] = Tfr.next()
                        Tf = d["Tf"]
                        S.op("dve", lambda e: e.tensor_tensor(out=Tf[:], in0=pw[:, 128:256], in1=W[:, 2, :], op=ALU.add),
                             reads=[pw_b, W_b], writes=[d["Tf_b"]])
                yield
            for c, d in enumerate(ch):
                pt3, pt3_b = psC.next()
                for i in range(3):
                    S.op("pe", lambda e: e.matmul(pt3[:, i * 128:(i + 1) * 128],
                                                  lhsT=BV[:, c, i].rearrange("p q t -> p (q t)"), rhs=idr[:],
                                                  start=True, stop=True),
                         reads=[BV_b, idr_b], writes=[pt3_b])
                d["TM3"], d["TM3_b"] = TM3r.next()
                TM3 = d["TM3"]
                S.op("act", lambda e: e.activation(out=TM3[:].rearrange("p a b -> p (a b)"), in_=pt3[:, 0:384],
                                                   func=AF.Copy), reads=[pt3_b], writes=[d["TM3_b"]])
            yield
            ctx.update(ch=ch, AR_b=AR_b, Ep=Ep, Ep_b=Ep_b, bon=bon, bon_b=bon_b, Xz=Xz, Xz_b=Xz_b, j=j, t0=t0)
            return ctx

        def stageB(ctx, state):
            ch, AR_b, Ep, Ep_b = ctx["ch"], ctx["AR_b"], ctx["Ep"], ctx["Ep_b"]
            j, t0 = ctx["j"], ctx["t0"]
            ob, ob_b = R("ob", 2)
            for c, d in enumerate(ch):
                Scur, Scur_b = state["S"], state["S_b"]
                Nka, Mkr = d["NM2"][:, 0:128], d["NM2"][:, 128:256]
                Mbr = d["NM1"][:, 128:256]
                TM3, TM3_b, Tf, Tf_b = d["TM3"], d["TM3_b"], d["Tf"], d["Tf_b"]
                BpT, KpT, VT = TM3[:, 0, :], TM3[:, 1, :], TM3[:, 2, :]
                pw1, pw1_b = psB.next()
                S.op("pe", lambda e: e.matmul(pw1[:, :], lhsT=d["Abd"], rhs=Scur[:], start=True, stop=False),
                     reads=[AR_b, Scur_b], writes=[pw1_b])
                S.op("pe", lambda e: e.matmul(pw1[:, :], lhsT=Nka, rhs=VT, start=False, stop=True),
                     reads=[d["NM2_b"], TM3_b], writes=[pw1_b])
                W1, W1_b = W1r.next()
                S.op("act", lambda e: e.activation(out=W1[:], in_=pw1[:, :], func=AF.Copy),
                     reads=[pw1_b], writes=[W1_b])
                yield
                pu, pu_b = psB.next()
                S.op("pe", lambda e: e.matmul(pu[:, :], lhsT=Tf[:], rhs=W1[:], start=True, stop=True),
                     reads=[Tf_b, W1_b], writes=[pu_b])
                UT, UT_b = UTr.next()
                S.op("dve", lambda e: e.tensor_copy(out=UT[:], in_=pu[:, :]), reads=[pu_b], writes=[UT_b])
                yield
                ps2, ps2_b = psB.next()
                S.op("pe", lambda e: e.matmul(ps2[:, :], lhsT=BpT, rhs=UT[:], start=True, stop=False),
                     reads=[TM3_b, UT_b], writes=[ps2_b])
                S.op("pe", lambda e: e.matmul(ps2[:, :], lhsT=KpT, rhs=VT, start=False, stop=True),
                     reads=[TM3_b], writes=[ps2_b])
                Snx, Snx_b = Sr.next()
                gc = Ep[:, c * CH + CH - 1:c * CH + CH]
                S.op("dve", lambda e: e.scalar_tensor_tensor(out=Snx[:], in0=Scur[:], scalar=gc, in1=ps2[:, :],
                                                             op0=ALU.mult, op1=ALU.add),
                     reads=[Scur_b, Ep_b, ps2_b], writes=[Snx_b])
                po, po_b = psB.next()
                S.op("pe", lambda e: e.matmul(po[:, :], lhsT=Scur[:], rhs=d["Rbd"], start=True, stop=False),
                     reads=[Scur_b, AR_b], writes=[po_b])
                S.op("pe", lambda e: e.matmul(po[:, :], lhsT=UT[:], rhs=Mbr, start=False, stop=False),
                     reads=[UT_b, d["NM1_b"]], writes=[po_b])
                S.op("pe", lambda e: e.matmul(po[:, :], lhsT=VT, rhs=Mkr, start=False, stop=True),
                     reads=[TM3_b, d["NM2_b"]], writes=[po_b])
                for p in range(2):
                    hs = slice(p * 64, (p + 1) * 64)
                    S.op("act", lambda e: e.activation(out=ob[hs, c * CH:(c + 1) * CH],
                                                       in_=po[hs, p * 64:(p + 1) * 64], func=AF.Copy),
                         reads=[po_b], writes=[ob_b])
                state["S"], state["S_b"] = Snx, Snx_b
                yield
            bon, bon_b, Xz, Xz_b = ctx["bon"], ctx["bon_b"], ctx["Xz"], ctx["Xz_b"]
            pm, pm_b = psC.next()
            S.op("pe", lambda e: e.matmul(pm[:, 0:TH], lhsT=blk64[:], rhs=ob[:], start=True, stop=True),
                 reads=[blk64_b, ob_b], writes=[pm_b])
            dd, dd_b = R("dd")
            S.op("dve", lambda e: e.scalar_tensor_tensor(out=dd[:], in0=pm[:, 0:TH], scalar=-1.0 / 64, in1=ob[:],
                                                         op0=ALU.mult, op1=ALU.add),
                 reads=[pm_b, ob_b], writes=[dd_b])
            sq2, sq2_b = R("sq2")
            S.op("pool", lambda e: e.tensor_tensor(out=sq2[:], in0=dd[:], in1=dd[:], op=ALU.mult),
                 reads=[dd_b], writes=[sq2_b])
            pvv, pvv_b = psC.next()
            S.op("pe", lambda e: e.matmul(pvv[:, 0:TH], lhsT=blk64[:], rhs=sq2[:], start=True, stop=True),
                 reads=[blk64_b, sq2_b], writes=[pvv_b])
            rn2, rn2_b = R("rn2")
            S.op("dve", lambda e: e.tensor_scalar(out=rn2[:], in0=pvv[:, 0:TH], scalar1=1.0 / 64, scalar2=GN_EPS,
                                                  op0=ALU.mult, op1=ALU.add), reads=[pvv_b], writes=[rn2_b])
            S.op("act", lambda e: e.activation(out=rn2[:], in_=rn2[:], func=AF.Sqrt), reads=[rn2_b], writes=[rn2_b])
            S.op("dve", lambda e: e.reciprocal(out=rn2[:], in_=rn2[:]), reads=[rn2_b], writes=[rn2_b])
            yield
            S.op("pool", lambda e: e.tensor_tensor(out=dd[:], in0=dd[:], in1=rn2[:], op=ALU.mult),
                 reads=[dd_b, rn2_b], writes=[dd_b])
            S.op("dve", lambda e: e.tensor_scalar(out=dd[:], in0=dd[:], scalar1=col(pc, PC_LNG + j),
                                                  scalar2=col(pc, PC_LNB + j), op0=ALU.mult, op1=ALU.add),
                 reads=[dd_b, pc_b], writes=[dd_b])
            S.op("pool", lambda e: e.tensor_tensor(out=dd[:], in0=dd[:], in1=bon[:], op=ALU.add),
                 reads=[dd_b, bon_b], writes=[dd_b])
            th, th_b = R("th")
            S.op("act", lambda e: e.activation(out=th[:], in_=Xz[:], func=AF.Tanh, scale=0.5), reads=[Xz_b], writes=[th_b])
            S.op("dve", lambda e: e.scalar_tensor_tensor(out=th[:], in0=th[:], scalar=1.0, in1=Xz[:],
                                                         op0=ALU.add, op1=ALU.mult),
                 reads=[th_b, Xz_b], writes=[th_b])
            yb, yb_b = R("yb", 2, (128, TH), BF16)
            S.op("dve", lambda e: e.scalar_tensor_tensor(out=yb[:], in0=dd[:], scalar=0.5, in1=th[:],
                                                         op0=ALU.mult, op1=ALU.mult),
                 reads=[dd_b, th_b], writes=[yb_b])
            S.dma("sp", ybT[j * 128:(j + 1) * 128, t0:t0 + TH], yb[:], reads=[yb_b], writes=[ybT_b])
            yield

        state = {}
        nxt = 0
        gP = gB = None
        gAs = []
        gP_waiting = False
        bq = []
        while nxt < len(blocks) or gP is not None or gAs or gB is not None or bq:
            if gP is None and nxt < len(blocks):
                gP = prepA(blocks[nxt][0], blocks[nxt][1], nxt % 2, nxt)
                nxt += 1
                gP_waiting = False
            if gB is None and bq:
                ctx = bq.pop(0)
                if ctx["t0"] == 0:
                    S0, S0_b = Sr.next()
                    S.op("dve", lambda e: e.tensor_copy(out=S0[:], in_=zf[:, 0:1].to_broadcast([128, 128])),
                         reads=[zf_b], writes=[S0_b])
                    state["S"], state["S_b"] = S0, S0_b
                gB = stageB(ctx, state)
            if gB is not None:
                try:
                    next(gB)
                except StopIteration:
                    gB = None
            for g in list(gAs):
                try:
                    next(g)
                except StopIteration as st:
                    assert g is gAs[0]
                    bq.append(st.value)
                    gAs.remove(g)
            if gP is not None:
                if not gP_waiting:
                    if next(gP) == "A":
                        gP_waiting = True
                if gP_waiting and len(gAs) < 2 and len(gAs) + len(bq) + (1 if gB is not None else 0) <= 2:
                    gAs.append(gP)
                    gP, gP_waiting = None, False
            yield

    def phase3(self, l):
        with ExitStack() as es:
            for _ in self.phase3_gen(l, es):
                pass

    def phase4(self, l, xin, xin_b, xo, xo_b):
        nc, S = self.nc, self.S
        projT, projT_b = self.scr["projT"], self.dbuf["projT"]
        yagT, yagT_b = self.scr["yagT"], self.dbuf["yagT"]
        ybT, ybT_b = self.scr["ybT"], self.dbuf["ybT"]
        with ExitStack() as es:
            gpost, gpost_b = sb(es, nc, "p4_gpost", [128, D], F32)
            S.dma("pool", gpost[:], self.inp["norm_post"][l].partition_broadcast(128), writes=[gpost_b])
            ya, ya_b = sb(es, nc, "p4_ya", [128, 4, T], BF16)
            yb, yb_b = sb(es, nc, "p4_yb", [128, 4, T], BF16)
            S.dma("pool", ya[:], yagT.rearrange("(c p) t -> p c t", p=128), reads=[yagT_b], writes=[ya_b])
            S.dma("pool", yb[:], ybT.rearrange("(c p) t -> p c t", p=128), reads=[ybT_b], writes=[yb_b])
            wst = Ring(es, nc, "p4_wst", 2, [128, 4, D], F32)
            wua, wua_b = sb(es, nc, "p4_wua", [128, 4, D], BF16)
            wur, wur_b = sb(es, nc, "p4_wur", [128, 4, D], BF16)
            wo, wo_b = sb(es, nc, "p4_wo", [128, 8, D], BF16)
            srcs = [(self.inp["w_up_att"][l].rearrange("(c p) e -> p c e", p=128), wua[:, :, :], wua_b),
                    (self.inp["w_up_rw"][l].rearrange("(c p) e -> p c e", p=128), wur[:, :, :], wur_b),
                    (self.inp["w_out"][l].rearrange("(c p) e -> p c e", p=128)[:, 0:4, :], wo[:, 0:4, :], wo_b),
                    (self.inp["w_out"][l].rearrange("(c p) e -> p c e", p=128)[:, 4:8, :], wo[:, 4:8, :], wo_b)]
            for i, (src, dst, dst_b) in enumerate(srcs):
                ws, ws_b = wst.next()
                S.dma("sp", ws[:], src, writes=[ws_b])
                S.op("pool" if i % 2 == 0 else "dve", lambda e: e.tensor_copy(out=dst, in_=ws[:]),
                     reads=[ws_b], writes=[dst_b])
            uTr = Ring(es, nc, "p4_uT", 2, [128, 8, 512], BF16)
            gAr = Ring(es, nc, "p4_gA", 6, [128, 512], F32)
            gRr = Ring(es, nc, "p4_gR", 6, [128, 512], F32)
            t1r = Ring(es, nc, "p4_t1", 2, [128, 512], F32)
            t2r = Ring(es, nc, "p4_t2", 2, [128, 512], F32)
            junk, junk_b = sb(es, nc, "p4_junk", [128, 512], F32)
            ssr = Ring(es, nc, "p4_ss", 4, [128, 4], F32)
            xtr = Ring(es, nc, "p4_xt", 4, [128, D], F32)
            otr = Ring(es, nc, "p4_ot", 3, [128, D], F32)
            psa = Ring(es, nc, "p4_psa", 2, [128, 512], F32, psum=True)
            psr = Ring(es, nc, "p4_psr", 2, [128, 512], F32, psum=True)
            psy = Ring(es, nc, "p4_psy", 4, [128, 512], F32, psum=True)
            half, half_b = self.K["half"]
            mhalf, mhalf_b = self.K["mhalf"]
            items = [(tc, eg) for tc in range(4) for eg in range(8)]

            def issue_g(i):
                tc, eg = items[i]
                ts_ = slice(tc * 512, (tc + 1) * 512)
                gA, gA_b = gAr.next()
                gR, gR_b = gRr.next()
                S.dma("sp", gA[:], projT[C_G_ATT + eg * 128:C_G_ATT + (eg + 1) * 128, ts_], reads=[projT_b], writes=[gA_b])
                S.dma("pool", gR[:], projT[C_G_RW + eg * 128:C_G_RW + (eg + 1) * 128, ts_], reads=[projT_b], writes=[gR_b])
                return gA, gA_b, gR, gR_b

            def ytile(tc, tt, uT, uT_b, xt, xt_b):
                tok0 = tc * 512 + tt * 128
                ss, ss_b = ssr.next()
                pys = []
                for hf in range(2):
                    py, py_b = psy.next()
                    for eg in range(8):
                        S.op("pe", lambda e: e.matmul(py[:, :], lhsT=uT[:, eg, tt * 128:(tt + 1) * 128],
                                                      rhs=wo[:, eg, hf * 512:(hf + 1) * 512],
                                                      start=(eg == 0), stop=(eg == 7)), reads=[uT_b, wo_b], writes=[py_b])
                    S.op("act", lambda e: e.activation(out=junk[:], in_=py[:, :], func=AF.Square,
                                                       accum_out=ss[:, hf:hf + 1]),
                         reads=[py_b], writes=[junk_b, ss_b])
                    pys.append((py, py_b))
                S.op("dve", lambda e: e.tensor_tensor(out=ss[:, 2:3], in0=ss[:, 0:1], in1=ss[:, 1:2], op=ALU.add),
                     reads=[ss_b], writes=[ss_b])
                S.op("dve", lambda e: e.tensor_scalar(out=ss[:, 3:4], in0=ss[:, 2:3], scalar1=1.0 / D, scalar2=RMS_EPS,
                                                      op0=ALU.mult, op1=ALU.add), reads=[ss_b], writes=[ss_b])
                S.op("pool", lambda e: e.tensor_tensor(out=ss[:, 2:3], in0=ss[:, 3:4], in1=mhalf[:, 0:1],
                                                       op=ALU.pow), reads=[ss_b, mhalf_b], writes=[ss_b])
                ot, ot_b = otr.next()
                for hf in range(2):
                    py, py_b = pys[hf]
                    S.op("dve", lambda e: e.scalar_tensor_tensor(out=ot[:, hf * 512:(hf + 1) * 512], in0=py[:, :],
                                                                 scalar=ss[:, 2:3], in1=gpost[:, hf * 512:(hf + 1) * 512],
                                                                 op0=ALU.mult, op1=ALU.mult),
                         reads=[py_b, ss_b, gpost_b], writes=[ot_b])
                S.op("pool", lambda e: e.tensor_tensor(out=ot[:], in0=ot[:], in1=xt[:], op=ALU.add),
                     reads=[ot_b, xt_b], writes=[ot_b])
                S.dma("act" if tt % 2 == 0 else "sp", xo[tok0:tok0 + 128, :], ot[:], reads=[ot_b], writes=[xo_b])

            LOOK = 4
            pendg = [issue_g(i) for i in range(LOOK)]
            ytodo = []
            for tc in range(4):
                ts_ = slice(tc * 512, (tc + 1) * 512)
                uT, uT_b = uTr.next()
                for eg in range(8):
                    i = tc * 8 + eg
                    gA, gA_b, gR, gR_b = pendg.pop(0)
                    if i + LOOK < len(items):
                        pendg.append(issue_g(i + LOOK))
                    for (g_, g_b) in ((gA, gA_b), (gR, gR_b)):
                        S.op("act", lambda e: e.activation(out=g_[:], in_=g_[:], func=AF.Tanh, scale=0.5),
                             reads=[g_b], writes=[g_b])
                        S.op("act", lambda e: e.activation(out=g_[:], in_=g_[:], func=AF.Identity, scale=0.5,
                                                           bias=half[:, 0:1]),
                             reads=[g_b, half_b], writes=[g_b])
                    pa, pa_b = psa.next()
                    pr, pr_b = psr.next()
                    for c in range(4):
                        S.op("pe", lambda e: e.matmul(pa[:, :], lhsT=wua[:, c, eg * 128:(eg + 1) * 128], rhs=ya[:, c, ts_],
                                                      start=(c == 0), stop=(c == 3)), reads=[wua_b, ya_b], writes=[pa_b])
                    for c in range(4):
                        S.op("pe", lambda e: e.matmul(pr[:, :], lhsT=wur[:, c, eg * 128:(eg + 1) * 128], rhs=yb[:, c, ts_],
                                                      start=(c == 0), stop=(c == 3)), reads=[wur_b, yb_b], writes=[pr_b])
                    t1, t1_b = t1r.next()
                    t2, t2_b = t2r.next()
                    S.op("dve", lambda e: e.tensor_tensor(out=t1[:], in0=pa[:, :], in1=gA[:], op=ALU.mult),
                         reads=[pa_b, gA_b], writes=[t1_b])
                    S.op("dve", lambda e: e.tensor_tensor(out=t2[:], in0=pr[:, :], in1=gR[:], op=ALU.mult),
                         reads=[pr_b, gR_b], writes=[t2_b])
                    S.op("pool", lambda e: e.tensor_tensor(out=uT[:, eg, :], in0=t1[:], in1=t2[:], op=ALU.add),
                         reads=[t1_b, t2_b], writes=[uT_b])
                    if eg % 2 == 1 and ytodo:
                        ytile(*ytodo.pop(0))
                while ytodo:
                    ytile(*ytodo.pop(0))
                for tt in range(4):
                    tok0 = tc * 512 + tt * 128
                    xt, xt_b = xtr.next()
                    S.dma("pool", xt[:], xin[tok0:tok0 + 128, :], reads=[xin_b], writes=[xt_b])
                    ytodo.append((tc, tt, uT, uT_b, xt, xt_b))
            while ytodo:
                ytile(*ytodo.pop(0))


def make_in_maps(inputs, hc=None):
    hc = hc or host_consts()
    pc = pack_params(inputs)
    shared = {}
    for k in ("norm_pre", "norm_post", "w_in", "rw_w_up", "rw_a_up", "rw_vmix_down", "rw_vmix_up",
              "w_up_att", "w_up_rw", "w_out"):
        shared[k] = np.ascontiguousarray(np.asarray(inputs[k], dtype=np.float32))
    shared["pc"] = pc
    for k, v in hc.items():
        shared["c_" + k] = v
    x = np.asarray(inputs["x"], dtype=np.float32)
    maps = []
    for c in range(NCORES):
        m = dict(shared)
        m["x"] = np.ascontiguousarray(x[c])
        maps.append(m)
    return maps


def kernel(**inputs):
    prog = Prog()
    nc = prog.build()
    maps = make_in_maps(inputs)
    res = run_bass_kernel_spmd(nc, maps, core_ids=list(range(NCORES)))
    return np.stack([np.asarray(r["out"], dtype=np.float32) for r in res.results], axis=0)
```

```python
import math
from contextlib import ExitStack
import numpy as np
import ml_dtypes
import concourse.bass as bass
import concourse.mybir as mybir
from concourse.bass_utils import run_bass_kernel_spmd

F32 = mybir.dt.float32
F32R = mybir.dt.float32r
BF16 = mybir.dt.bfloat16
ALU = mybir.AluOpType
AF = mybir.ActivationFunctionType
AX = mybir.AxisListType

D = 1024
T = 2048
DIN = 6272
NL = 2
NCORES = 8
RMS_EPS = 1e-6
GN_EPS = 64e-5
C_ATT_Q, C_ATT_K, C_ATT_V, C_ATT_Z = 0, 512, 1024, 1536
C_RW_R, C_RW_K, C_RW_V, C_RW_WD, C_RW_AD, C_RW_Z = 2048, 2560, 3072, 3584, 3648, 3712
C_G_ATT, C_G_RW = 4224, 5248
PROJ_ROWS = DIN + 32
NEGM = -30000.0
CH = 64
PC_MU, PC_W0, PC_A0, PC_KK, PC_KA, PC_RK, PC_LNG, PC_LNB, PC_VM0, PC_VMU = 0, 13, 17, 21, 25, 29, 33, 37, 41, 45
NPC = 46


class Buf:
    __slots__ = ("name", "w", "rs")

    def __init__(self, name=""):
        self.name = name
        self.w = None
        self.rs = []


class Sched:
    def __init__(self, nc, ndma=10, same_engine_waits=True):
        self.nc = nc
        self.eng = {"pe": nc.tensor, "act": nc.scalar, "dve": nc.vector,
                    "pool": nc.gpsimd, "sp": nc.sync}
        self.stack = []
        self.sem = {}
        self.cnt = {}
        for e in self.eng:
            cm = nc.semaphore("s_" + e)
            self.sem[e] = cm.__enter__()
            self.stack.append(cm)
            self.cnt[e] = 0
        self.dma_sems = {}
        self.dma_rr = {}
        for q in ("sp", "pool", "act"):
            lst = []
            for i in range(ndma):
                key = "d_%s%d" % (q, i)
                cm = nc.semaphore(key)
                self.sem[key] = cm.__enter__()
                self.stack.append(cm)
                self.cnt[key] = 0
                lst.append(key)
            self.dma_sems[q] = lst
            self.dma_rr[q] = 0
        self.seen = {e: {} for e in self.eng}
        self.same = same_engine_waits
        self.n_wait = 0
        self.n_ins = 0

    def _need(self, e, needs, ev):
        if ev is None:
            return
        k, v = ev
        if k == e and (e == "pe" or not self.same):
            return
        if self.seen[e].get(k, 0) >= v:
            return
        if needs.get(k, 0) < v:
            needs[k] = v

    def _collect(self, e, reads, writes):
        needs = {}
        for b in reads:
            self._need(e, needs, b.w)
        for b in writes:
            self._need(e, needs, b.w)
            for r in b.rs:
                self._need(e, needs, r)
        return needs

    def _emit_waits(self, e, needs):
        eng = self.eng[e]
        for k, v in needs.items():
            eng.wait_ge(self.sem[k], v)
            self.seen[e][k] = v
            self.n_wait += 1

    def _commit(self, ev, reads, writes):
        for b in reads:
            b.rs.append(ev)
        for b in writes:
            b.w = ev
            b.rs = []

    def op(self, e, fn, reads=(), writes=()):
        needs = self._collect(e, reads, writes)
        self._emit_waits(e, needs)
        ins = fn(self.eng[e])
        self.cnt[e] += 1
        ins.then_inc(self.sem[e], 1)
        ev = (e, self.cnt[e])
        if e != "pe" and self.same:
            pass
        self._commit(ev, reads, writes)
        self.n_ins += 1
        return ev

    def dma(self, q, out, in_, reads=(), writes=(), **kw):
        lst = self.dma_sems[q]
        key = lst[self.dma_rr[q] % len(lst)]
        self.dma_rr[q] += 1
        needs = self._collect(q, reads, writes)
        if self.cnt[key] > 0:
            self._need(q, needs, (key, self.cnt[key]))
        self._emit_waits(q, needs)
        ins = self.eng[q].dma_start(out=out, in_=in_, **kw)
        self.cnt[key] += 16
        ins.then_inc(self.sem[key], 16)
        ev = (key, self.cnt[key])
        self._commit(ev, reads, writes)
        self.n_ins += 1
        return ev

    def wait_all(self, e, bufs):
        needs = {}
        for b in bufs:
            self._need(e, needs, b.w)
        self._emit_waits(e, needs)

    def barrier(self):
        snap = {k: v for k, v in self.cnt.items() if v > 0}
        for e in self.eng:
            needs = {}
            for k, v in snap.items():
                if k == e:
                    continue
                if self.seen[e].get(k, 0) < v:
                    needs[k] = v
            self._emit_waits(e, needs)

    def close(self):
        for cm in reversed(self.stack):
            cm.__exit__(None, None, None)


_UID = [0]


def _uname(name):
    _UID[0] += 1
    return "%s_%d" % (name, _UID[0])


class Ring:
    def __init__(self, es, nc, name, n, shape, dtype, psum=False):
        self.items = []
        name = _uname(name)
        for i in range(n):
            if psum:
                t = es.enter_context(nc.psum_tensor("%s%d" % (name, i), shape, dtype))
            else:
                t = es.enter_context(nc.sbuf_tensor("%s%d" % (name, i), shape, dtype))
            self.items.append((t, Buf(name + str(i))))
        self.i = 0

    def next(self):
        it = self.items[self.i % len(self.items)]
        self.i += 1
        return it


class PsumRing:
    def __init__(self, es, nc, name, n, width):
        per = 512 // width
        nb = (n + per - 1) // per
        name = _uname(name)
        self.items = []
        for b in range(nb):
            t = es.enter_context(nc.psum_tensor("%s_%d" % (name, b), [128, 512], F32))
            for k in range(per):
                if len(self.items) < n:
                    self.items.append((t[:, k * width:(k + 1) * width], Buf("%s_%d_%d" % (name, b, k))))
        self.i = 0

    def next(self):
        it = self.items[self.i % len(self.items)]
        self.i += 1
        return it


def sb(es, nc, name, shape, dtype):
    name = _uname(name)
    return es.enter_context(nc.sbuf_tensor(name, shape, dtype)), Buf(name)


def host_consts():
    bf = ml_dtypes.bfloat16
    c = {}
    c["ident_f"] = np.eye(128, dtype=np.float32)
    c["ident_b"] = np.eye(128).astype(bf)
    kk, qq = np.meshgrid(np.arange(128), np.arange(128), indexing="ij")
    c["trimask"] = np.where(kk > qq, NEGM, 0.0).astype(bf)
    half = np.arange(128) // 64
    same = (half[:, None] == half[None, :])
    c["blk64"] = same.astype(np.float32)
    loc = np.arange(128) % 64
    su = same & (loc[:, None] < loc[None, :])
    iu = same & (loc[:, None] <= loc[None, :])
    c["mask2"] = np.concatenate([su, iu], axis=1).astype(np.float32)
    c["maskl"] = (same & (loc[:, None] > loc[None, :])).astype(np.float32)
    gs = np.zeros((64, 8, 72), np.float32)
    for n in range(8):
        for m in range(8):
            for qb in range(8):
                if m < qb:
                    gs[n * 8 + m, qb, 64 + n] = 1.0
    c["gsum"] = gs.astype(bf)
    thr = np.zeros((128, 8), np.float32)
    for n in range(8):
        for qb in range(8):
            thr[64 + n, qb] = 2.5 if n < qb else (1e9 if n == qb else -1.0)
    c["thr"] = thr
    pos = np.arange(T)
    hi, lo = pos // 128, pos % 128
    c["qconst"] = np.stack([-128.0 * hi, -1.0 * lo, np.ones(T), np.ones(T)]).astype(bf)
    kc = np.zeros((8, 12, T), np.float32)
    for h in range(8):
        sl = 2.0 ** (-(h + 1))
        for n in range(8):
            kc[h, n] = np.where(pos // 256 == n, NEGM, 0.0)
        kc[h, 8] = sl
        kc[h, 9] = sl
        kc[h, 10] = sl * 128.0 * hi
        kc[h, 11] = sl * lo
    c["kconst"] = kc.astype(bf)
    return c


def pack_params(inp):
    out = np.zeros((NL, 128, NPC), np.float32)

    def put(l, col, vec):
        n = vec.shape[0]
        if n % 128 == 0:
            out[l, :, col:col + n // 128] = vec.reshape(n // 128, 128).T
        else:
            out[l, :n, col] = vec

    for l in range(NL):
        put(l, PC_MU, inp["rw_mu"][l])
        put(l, PC_W0, inp["rw_w0"][l])
        put(l, PC_A0, inp["rw_a0"][l])
        put(l, PC_KK, inp["rw_k_k"][l])
        put(l, PC_KA, inp["rw_k_a"][l])
        put(l, PC_RK, inp["rw_r_k"][l])
        put(l, PC_LNG, inp["rw_ln_g"][l])
        put(l, PC_LNB, inp["rw_ln_b"][l])
        if l >= 1:
            put(l, PC_VM0, inp["rw_vmix0"][l - 1])
            put(l, PC_VMU, inp["rw_vmix_mu"][l - 1])
    return out


class Prog:
    def __init__(self, dbg=(), nlayers=NL, stop_after=None):
        self.dbg = set(dbg)
        self.nlayers = nlayers
        self.stop_after = stop_after
        nc = bass.Bass("TRN2", target_bir_lowering=False)
        self.nc = nc
        self.S = Sched(nc)
        self.inp = {}
        self.scr = {}
        self.dbuf = {}

    def din(self, name, shape, dtype=F32):
        ap = self.nc.dram_tensor(name, list(shape), dtype, kind="ExternalInput").ap()
        self.inp[name] = ap
        self.dbuf[name] = Buf(name)
        return ap

    def dscr(self, name, shape, dtype=F32):
        kind = "ExternalOutput" if name in self.dbg else "Internal"
        ap = self.nc.dram_tensor(name, list(shape), dtype, kind=kind).ap()
        self.scr[name] = ap
        self.dbuf[name] = Buf(name)
        return ap

    def build(self):
        nc, S = self.nc, self.S
        x = self.din("x", [T, D])
        self.din("norm_pre", [NL, D])
        self.din("norm_post", [NL, D])
        self.din("w_in", [NL, D, DIN])
        self.din("rw_w_up", [NL, 64, 512])
        self.din("rw_a_up", [NL, 64, 512])
        self.din("rw_vmix_down", [1, D, 32])
        self.din("rw_vmix_up", [1, 32, 512])
        self.din("w_up_att", [NL, 512, D])
        self.din("w_up_rw", [NL, 512, D])
        self.din("w_out", [NL, D, D])
        self.din("pc", [NL, 128, NPC])
        hc = host_consts()
        for k, v in hc.items():
            self.din("c_" + k, v.shape, BF16 if v.dtype == ml_dtypes.bfloat16 else F32)
        out = self.nc.dram_tensor("out", [T, D], F32, kind="ExternalOutput").ap()
        self.dbuf["out"] = Buf("out")
        self.dscr("projT", [PROJ_ROWS, T])
        self.dscr("vtm", [T, 520], BF16)
        self.dscr("yagT", [512, T], BF16)
        self.dscr("ybT", [512, T], BF16)
        self.dscr("vfirst", [512, T])
        self.dscr("x1", [T, D])

        with ExitStack() as es:
            self.load_consts(es)
            xin, xin_b = x, self.dbuf["x"]
            for l in range(self.nlayers):
                last = (l == self.nlayers - 1)
                xo, xo_b = (out, self.dbuf["out"]) if last else (self.scr["x1"], self.dbuf["x1"])
                self.phase1(l, xin, xin_b)
                if self.stop_after == (l, 1):
                    break
                S.barrier()
                self.phase2(l)
                if self.stop_after == (l, 2):
                    break
                S.barrier()
                self.phase3(l)
                if self.stop_after == (l, 3):
                    break
                S.barrier()
                self.phase4(l, xin, xin_b, xo, xo_b)
                S.barrier()
                xin, xin_b = xo, xo_b
            S.wait_all("sp", list(self.dbuf.values()))
            S.barrier()
        S.close()
        return nc

    def load_consts(self, es):
        nc, S = self.nc, self.S
        self.K = {}
        for name, shape, dt in (("ident_f", [128, 128], F32), ("ident_b", [128, 128], BF16),
                                ("trimask", [128, 128], BF16), ("blk64", [128, 128], F32),
                                ("mask2", [128, 256], F32), ("maskl", [128, 128], F32),
                                ("thr", [128, 8], F32)):
            t, b = sb(es, nc, "k_" + name, shape, dt)
            S.dma("sp", t[:], self.inp["c_" + name][:, :], writes=[b])
            self.K[name] = (t, b)
        t, b = sb(es, nc, "k_gsum", [64, 8, 72], BF16)
        S.dma("sp", t[:], self.inp["c_gsum"][:, :, :], writes=[b])
        self.K["gsum"] = (t, b)
        t, b = sb(es, nc, "k_ones", [128, 64], F32)
        S.op("pool", lambda e: e.memset(t[:], 1.0), writes=[b])
        self.K["ones"] = (t, b)
        t2, b2 = sb(es, nc, "k_pc", [128, NL, NPC], F32)
        S.dma("sp", t2[:], self.inp["pc"].rearrange("l p c -> p l c"), writes=[b2])
        self.K["pc"] = (t2, b2)
        t3, b3 = sb(es, nc, "k_pc1m", [128, NL, NPC], F32)
        S.op("dve", lambda e: e.tensor_scalar(out=t3[:], in0=t2[:], scalar1=-1.0, scalar2=1.0,
                                              op0=ALU.mult, op1=ALU.add), reads=[b2], writes=[b3])
        self.K["pc1m"] = (t3, b3)
        t4, b4 = sb(es, nc, "k_pch", [128, NL, NPC], F32)
        S.op("dve", lambda e: e.tensor_scalar(out=t4[:], in0=t2[:], scalar1=0.5, scalar2=None, op0=ALU.mult),
             reads=[b2], writes=[b4])
        self.K["pch"] = (t4, b4)
        t5, b5 = sb(es, nc, "k_half", [128, 2], F32)
        S.op("pool", lambda e: e.memset(t5[:], 0.5), writes=[b5])
        self.K["half"] = (t5, b5)
        t6, b6 = sb(es, nc, "k_mhalf", [128, 512], F32)
        S.op("pool", lambda e: e.memset(t6[:], -0.5), writes=[b6])
        self.K["mhalf"] = (t6, b6)

    def phase1(self, l, xin, xin_b):
        nc, S = self.nc, self.S
        projT, projT_b = self.scr["projT"], self.dbuf["projT"]
        vtm, vtm_b = self.scr["vtm"], self.dbuf["vtm"]
        identf, identf_b = self.K["ident_f"]
        with ExitStack() as es:
            gpre, gpre_b = sb(es, nc, "p1_gpre", [128, D], F32)
            S.dma("sp", gpre[:], self.inp["norm_pre"][l].partition_broadcast(128), writes=[gpre_b])
            hT, _ = sb(es, nc, "p1_hT", [128, 8, T], BF16)
            hT_b = [Buf("hT%d" % i) for i in range(16)]
            xr = Ring(es, nc, "p1_x", 2, [128, D], F32)
            hr = Ring(es, nc, "p1_h", 2, [128, D], F32)
            junk, junk_b = sb(es, nc, "p1_junk", [128, D], F32)
            ssr = Ring(es, nc, "p1_ss", 4, [128, 2], F32)
            pst = Ring(es, nc, "p1_pst", 2, [128, 512], F32, psum=True)
            psm = Ring(es, nc, "p1_psm", 4, [128, 512], F32, psum=True)
            wst = Ring(es, nc, "p1_wst", 3, [128, 8, 512], F32)
            wbf = Ring(es, nc, "p1_wbf", 3, [128, 8, 512], BF16)
            w_in = self.inp["w_in"][l].rearrange("(dc p) e -> p dc e", p=128)
            w_in_b = self.dbuf["w_in"]
            blocks = [(c0, min(512, DIN - c0)) for c0 in range(0, DIN, 512)]

            def load_block(bi):
                c0, ncol = blocks[bi]
                ws, ws_b = wst.next()
                S.dma("sp", ws[:, 0:4, 0:ncol], w_in[:, 0:4, c0:c0 + ncol], reads=[w_in_b], writes=[ws_b])
                S.dma("sp", ws[:, 4:8, 0:ncol], w_in[:, 4:8, c0:c0 + ncol], reads=[w_in_b], writes=[ws_b])
                wb, wb_b = wbf.next()
                S.op("dve", lambda e: e.tensor_copy(out=wb[:, 0:4, 0:ncol], in_=ws[:, 0:4, 0:ncol]),
                     reads=[ws_b], writes=[wb_b])
                S.op("act", lambda e: e.activation(out=wb[:, 4:8, 0:ncol], in_=ws[:, 4:8, 0:ncol], func=AF.Copy),
                     reads=[ws_b], writes=[wb_b])
                return wb, wb_b

            pending = [load_block(0), load_block(1)]
            for tt in range(16):
                xt, xt_b = xr.next()
                S.dma("pool", xt[:], xin[tt * 128:(tt + 1) * 128, :],
                      reads=[xin_b], writes=[xt_b])
                ss, ss_b = ssr.next()
                S.op("act", lambda e: e.activation(out=junk[:], in_=xt[:], func=AF.Square,
                                                   accum_out=ss[:, 0:1]),
                     reads=[xt_b], writes=[junk_b, ss_b])
                S.op("dve", lambda e: e.tensor_scalar(out=ss[:, 1:2], in0=ss[:, 0:1], scalar1=1.0 / D,
                                                      scalar2=RMS_EPS, op0=ALU.mult, op1=ALU.add),
                     reads=[ss_b], writes=[ss_b])
                S.op("act", lambda e: e.activation(out=ss[:, 1:2], in_=ss[:, 1:2], func=AF.Sqrt),
                     reads=[ss_b], writes=[ss_b])
                S.op("dve", lambda e: e.reciprocal(out=ss[:, 0:1], in_=ss[:, 1:2]),
                     reads=[ss_b], writes=[ss_b])
                hf, hf_b = hr.next()
                S.op("dve", lambda e: e.scalar_tensor_tensor(out=hf[:], in0=xt[:], scalar=ss[:, 0:1],
                                                             in1=gpre[:], op0=ALU.mult, op1=ALU.mult),
                     reads=[xt_b, ss_b, gpre_b], writes=[hf_b])
                for half in range(2):
                    ps, ps_b = pst.next()
                    for j in range(4):
                        dc = half * 4 + j
                        S.op("pe", lambda e: e.transpose(ps[:, j * 128:(j + 1) * 128],
                                                         hf[:, dc * 128:(dc + 1) * 128], identf[:]),
                             reads=[hf_b, identf_b], writes=[ps_b])
                    eng = "act" if half == 0 else "dve"
                    dst = hT[:, half * 4:half * 4 + 4, tt * 128:(tt + 1) * 128]
                    src = ps[:, :].rearrange("p (a b) -> p a b", a=4)
                    if eng == "act":
                        S.op("act", lambda e: e.activation(out=dst, in_=src, func=AF.Copy),
                             reads=[ps_b], writes=[hT_b[tt]])
                    else:
                        S.op("dve", lambda e: e.tensor_copy(out=dst, in_=src),
                             reads=[ps_b], writes=[hT_b[tt]])
            stg = Ring(es, nc, "p1_stg", 12, [128, 512], F32)
            vst = Ring(es, nc, "p1_vst", 2, [128, 8, 65], BF16)
            for (vt, vb) in vst.items:
                S.op("pool", lambda e: e.memset(vt[:], 1.0), writes=[vb])
            nev = 0
            for bi, (c0, ncol) in enumerate(blocks):
                wb, wb_b = pending.pop(0)
                if bi + 2 < len(blocks):
                    pending.append(load_block(bi + 2))
                if c0 == C_ATT_V:
                    for tt in range(16):
                        ps, ps_b = psm.next()
                        for dc in range(8):
                            S.op("pe", lambda e: e.matmul(ps[:, :], lhsT=hT[:, dc, tt * 128:(tt + 1) * 128],
                                                          rhs=wb[:, dc, :], start=(dc == 0), stop=(dc == 7)),
                                 reads=[hT_b[tt], wb_b], writes=[ps_b])
                        vt, vt_b = vst.next()
                        src = ps[:, :].rearrange("p (h d) -> p h d", h=8)
                        S.op("dve", lambda e: e.tensor_copy(out=vt[:, :, 0:64], in_=src),
                             reads=[ps_b], writes=[vt_b])
                        S.dma("pool", vtm[tt * 128:(tt + 1) * 128, :], vt[:].rearrange("p h d -> p (h d)"),
                              reads=[vt_b], writes=[vtm_b])
                    continue
                for g in range(ncol // 128):
                    for tc in range(4):
                        ps, ps_b = psm.next()
                        for dc in range(8):
                            S.op("pe", lambda e: e.matmul(ps[:, :], lhsT=wb[:, dc, g * 128:(g + 1) * 128],
                                                          rhs=hT[:, dc, tc * 512:(tc + 1) * 512],
                                                          start=(dc == 0), stop=(dc == 7)),
                                 reads=[wb_b] + hT_b[tc * 4:tc * 4 + 4], writes=[ps_b])
                        st, st_b = stg.next()
                        if nev % 2 == 0:
                            S.op("act", lambda e: e.activation(out=st[:], in_=ps[:, :], func=AF.Copy),
                                 reads=[ps_b], writes=[st_b])
                        else:
                            S.op("dve", lambda e: e.tensor_copy(out=st[:], in_=ps[:, :]),
                                 reads=[ps_b], writes=[st_b])
                        nev += 1
                        r0 = c0 + g * 128
                        S.dma(("pool", "sp")[nev % 2],
                              projT[r0:r0 + 128, tc * 512:(tc + 1) * 512], st[:],
                              reads=[st_b], writes=[projT_b])
            if l >= 1:
                wv, wv_b = sb(es, nc, "p1_wv", [128, 8, 32], F32)
                wvb, wvb_b = sb(es, nc, "p1_wvb", [128, 8, 32], BF16)
                S.dma("sp", wv[:], self.inp["rw_vmix_down"][l - 1].rearrange("(dc p) e -> p dc e", p=128),
                      writes=[wv_b])
                S.op("dve", lambda e: e.tensor_copy(out=wvb[:], in_=wv[:]), reads=[wv_b], writes=[wvb_b])
                for tc in range(4):
                    ps, ps_b = psm.next()
                    for dc in range(8):
                        S.op("pe", lambda e: e.matmul(ps[0:32, :], lhsT=wvb[:, dc, :],
                                                      rhs=hT[:, dc, tc * 512:(tc + 1) * 512],
                                                      start=(dc == 0), stop=(dc == 7)),
                             reads=[wvb_b] + hT_b[tc * 4:tc * 4 + 4], writes=[ps_b])
                    st, st_b = stg.next()
                    S.op("dve", lambda e: e.tensor_copy(out=st[0:32, :], in_=ps[0:32, :]),
                         reads=[ps_b], writes=[st_b])
                    S.dma("sp", projT[DIN:DIN + 32, tc * 512:(tc + 1) * 512], st[0:32, :],
                          reads=[st_b], writes=[projT_b])

    def phase2(self, l):
        nc, S = self.nc, self.S
        projT, projT_b = self.scr["projT"], self.dbuf["projT"]
        vtm, vtm_b = self.scr["vtm"], self.dbuf["vtm"]
        yagT, yagT_b = self.scr["yagT"], self.dbuf["yagT"]
        identb, identb_b = self.K["ident_b"]
        trim, trim_b = self.K["trimask"]
        gsum, gsum_b = self.K["gsum"]
        thr, thr_b = self.K["thr"]
        ones, ones_b = self.K["ones"]
        with ExitStack() as es:
            vext, vext_b = sb(es, nc, "p2_vext", [128, 16, 520], BF16)
            S.dma("pool", vext[:], vtm.rearrange("(t p) c -> p t c", p=128), reads=[vtm_b], writes=[vext_b])
            slots = []
            for i in range(2):
                qaug_, _ = sb(es, nc, "p2_qaug", [128, T], BF16)
                kaug_, _ = sb(es, nc, "p2_kaug", [128, T], BF16)
                sl = dict(qaug=qaug_, kaug=kaug_, qa_q=Buf("qa_q"), qa_n=[Buf("qa_n%d" % k) for k in range(4)],
                          qa_c=Buf("qa_c"), ka_k=Buf("ka_k"), ka_c=Buf("ka_c"))
                S.dma("sp", qaug_[72:76, :], self.inp["c_qconst"][:, :], writes=[sl["qa_c"]])
                slots.append(sl)
            qfr = Ring(es, nc, "p2_qf", 2, [64, T], F32)
            kfr = Ring(es, nc, "p2_kf", 2, [64, T], F32)
            azr = Ring(es, nc, "p2_az", 2, [64, T], F32)
            szr = Ring(es, nc, "p2_sz", 2, [64, T], F32)
            kmr = Ring(es, nc, "p2_kmean", 2, [64, 8], F32)
            kdr = Ring(es, nc, "p2_kdiff", 2, [64, 8, 8], F32)
            indr = Ring(es, nc, "p2_ind", 2, [64, 512], BF16)
            ptr = Ring(es, nc, "p2_pt", 3, [128, 512], BF16)
            rden, rden_b = sb(es, nc, "p2_rden", [128, 512], F32)
            bcs, bcs_b = sb(es, nc, "p2_bcs", [64, 512], F32)
            yac, yac_b = sb(es, nc, "p2_yac", [64, 512], F32)
            yagr = Ring(es, nc, "p2_yag", 2, [64, T], BF16)
            ps_s = Ring(es, nc, "p2_pss", 4, [128, 512], F32, psum=True)
            ps_o = Ring(es, nc, "p2_pso", 2, [128, 512], F32, psum=True)
            ps_m = Ring(es, nc, "p2_psm", 2, [128, 512], F32, psum=True)
            deferred = []

            def setup(h):
                sl = slots[h % 2]
                qaug, kaug = sl["qaug"], sl["kaug"]
                qf, qf_b = qfr.next()
                kf, kf_b = kfr.next()
                azf, azf_b = azr.next()
                S.dma("sp", qf[:], projT[C_ATT_Q + h * 64:C_ATT_Q + (h + 1) * 64, :], reads=[projT_b], writes=[qf_b])
                S.dma("pool", kf[:], projT[C_ATT_K + h * 64:C_ATT_K + (h + 1) * 64, :], reads=[projT_b], writes=[kf_b])
                S.dma("sp", azf[:], projT[C_ATT_Z + h * 64:C_ATT_Z + (h + 1) * 64, :], reads=[projT_b], writes=[azf_b])
                S.dma("pool", kaug[64:76, :], self.inp["c_kconst"][h], writes=[sl["ka_c"]])
                S.op("pool", lambda e: e.tensor_copy(out=kaug[0:64, :], in_=kf[:]), reads=[kf_b], writes=[sl["ka_k"]])
                S.op("act", lambda e: e.activation(out=qaug[0:64, :], in_=qf[:], func=AF.Copy, scale=0.125),
                     reads=[qf_b], writes=[sl["qa_q"]])
                sz, sz_b = szr.next()
                S.op("act", lambda e: e.activation(out=sz[:], in_=azf[:], func=AF.Tanh, scale=0.5), reads=[azf_b], writes=[sz_b])
                S.op("dve", lambda e: e.scalar_tensor_tensor(out=sz[:], in0=sz[:], scalar=1.0, in1=azf[:],
                                                             op0=ALU.add, op1=ALU.mult),
                     reads=[sz_b, azf_b], writes=[sz_b])
                kmean, kmean_b = kmr.next()
                kdiff, kdiff_b = kdr.next()
                S.op("dve", lambda e: e.reduce_sum(out=kmean[:], in_=kf[:].rearrange("p (n k) -> p n k", k=256),
                                                   axis=AX.X), reads=[kf_b], writes=[kmean_b])
                S.op("dve", lambda e: e.tensor_tensor(out=kdiff[:], in0=kmean[:, :].unsqueeze(1).to_broadcast([64, 8, 8]),
                                                      in1=kmean[:, :].unsqueeze(2).to_broadcast([64, 8, 8]),
                                                      op=ALU.subtract), reads=[kmean_b], writes=[kdiff_b])
                return dict(sl=sl, qf=qf, qf_b=qf_b, sz=sz, sz_b=sz_b, kdiff=kdiff, kdiff_b=kdiff_b)

            nxt_setup = setup(0)
            for h in range(8):
                st_ = nxt_setup
                sl = st_["sl"]
                qaug, kaug = sl["qaug"], sl["kaug"]
                qa_q, qa_n, qa_c, ka_k, ka_c = sl["qa_q"], sl["qa_n"], sl["qa_c"], sl["ka_k"], sl["ka_c"]
                qf, qf_b, sz, sz_b = st_["qf"], st_["qf_b"], st_["sz"], st_["sz_b"]
                kdiff, kdiff_b = st_["kdiff"], st_["kdiff_b"]
                yag, yag_b = yagr.next()
                for c in range(4):
                    if c == 2 and h + 1 < 8:
                        nxt_setup = setup(h + 1)
                    pg, pg_b = ps_m.next()
                    S.op("pe", lambda e: e.matmul(pg[0:64, :], lhsT=kdiff[:].rearrange("p n m -> p (n m)"),
                                                  rhs=qf[:, c * 512:(c + 1) * 512], start=True, stop=True),
                         reads=[kdiff_b, qf_b], writes=[pg_b])
                    ind, ind_b = indr.next()
                    S.op("dve", lambda e: e.tensor_single_scalar(out=ind[:], in_=pg[0:64, :], scalar=0.0, op=ALU.is_gt),
                         reads=[pg_b], writes=[ind_b])
                    pr, pr_b = ps_m.next()
                    for j in range(2):
                        qb = 2 * c + j
                        S.op("pe", lambda e: e.matmul(pr[0:72, j * 256:(j + 1) * 256], lhsT=gsum[:, qb, :],
                                                      rhs=ind[:, j * 256:(j + 1) * 256], start=True, stop=True),
                             reads=[gsum_b, ind_b], writes=[pr_b])
                    for j in range(2):
                        qb = 2 * c + j
                        S.op("dve", lambda e: e.tensor_scalar(out=qaug[64:72, qb * 256:(qb + 1) * 256],
                                                              in0=pr[64:72, j * 256:(j + 1) * 256],
                                                              scalar1=thr[64:72, qb:qb + 1], scalar2=None,
                                                              op0=ALU.is_ge),
                             reads=[pr_b, thr_b], writes=[qa_n[c]])
                    po, po_b = ps_o.next()
                    nkt = 4 * c + 4

                    def qk(kt, c=c, h=h):
                        j = kt - 4 * c
                        off = 0 if j < 0 else j * 128
                        n = 512 - off
                        q0 = c * 512 + off
                        pss, pss_b = ps_s.next()
                        S.op("pe", lambda e: e.matmul(pss[:, 0:n], lhsT=kaug[0:76, kt * 128:(kt + 1) * 128],
                                                      rhs=qaug[0:76, q0:q0 + n], start=True, stop=(j < 0)),
                             reads=[ka_k, ka_c, qa_q, qa_n[c], qa_c], writes=[pss_b])
                        if j >= 0:
                            S.op("pe", lambda e: e.matmul(pss[:, 0:128], lhsT=identb[:], rhs=trim[:],
                                                          start=False, stop=True),
                                 reads=[identb_b, trim_b], writes=[pss_b])
                        return pss, pss_b, off, n

                    def finalize1(po=po, po_b=po_b):
                        S.op("dve", lambda e: e.reciprocal(out=rden[64:65, :], in_=po[64:65, :]),
                             reads=[po_b], writes=[rden_b])

                    def finalize(po=po, po_b=po_b, c=c, h=h, yag=yag, yag_b=yag_b, sz=sz, sz_b=sz_b, last=(c == 3)):
                        pb, pb_b = ps_m.next()
                        S.op("pe", lambda e: e.matmul(pb[0:64, :], lhsT=ones[64:65, 0:64], rhs=rden[64:65, :],
                                                      start=True, stop=True), reads=[ones_b, rden_b], writes=[pb_b])
                        S.op("act", lambda e: e.activation(out=bcs[:], in_=pb[0:64, :], func=AF.Copy),
                             reads=[pb_b], writes=[bcs_b])
                        S.op("dve", lambda e: e.scalar_tensor_tensor(out=yac[:], in0=po[0:64, :], scalar=0.5, in1=bcs[:],
                                                                     op0=ALU.mult, op1=ALU.mult),
                             reads=[po_b, bcs_b], writes=[yac_b])
                        S.op("pool", lambda e: e.tensor_tensor(out=yag[:, c * 512:(c + 1) * 512], in0=yac[:],
                                                               in1=sz[:, c * 512:(c + 1) * 512], op=ALU.mult),
                             reads=[yac_b, sz_b], writes=[yag_b])
                        if last:
                            S.dma("sp", yagT[h * 64:(h + 1) * 64, :], yag[:], reads=[yag_b], writes=[yagT_b])

                    pend = [qk(0), qk(1)]
                    for kt in range(nkt):
                        if kt + 2 < nkt:
                            pend.append(qk(kt + 2))
                        if kt == 1 and deferred:
                            deferred[0][0]()
                        if kt == 3 and deferred:
                            deferred.pop(0)[1]()
                        pss, pss_b, off, n = pend.pop(0)
                        pt, pt_b = ptr.next()
                        S.op("act", lambda e: e.activation(out=pt[:, 0:n], in_=pss[:, 0:n], func=AF.Exp),
                             reads=[pss_b], writes=[pt_b])
                        S.op("pe", lambda e: e.matmul(po[0:65, off:512], lhsT=vext[:, kt, h * 65:(h + 1) * 65],
                                                      rhs=pt[:, 0:n], start=(kt == 0), stop=(kt == nkt - 1)),
                             reads=[vext_b, pt_b], writes=[po_b])
                    deferred.append((finalize1, finalize))
            while deferred:
                f1, f2 = deferred.pop(0)
                f1()
                f2()

    def phase3_gen(self, l, es, TH=256):
        nc, S = self.nc, self.S
        projT, projT_b = self.scr["projT"], self.dbuf["projT"]
        ybT, ybT_b = self.scr["ybT"], self.dbuf["ybT"]
        vfirst, vfirst_b = self.scr["vfirst"], self.dbuf["vfirst"]
        identf, identf_b = self.K["ident_f"]
        blk64, blk64_b = self.K["blk64"]
        mask2, mask2_b = self.K["mask2"]
        maskl, maskl_b = self.K["maskl"]
        pc, pc_b = self.K["pc"]
        pc1m, pc1m_b = self.K["pc1m"]
        NCH = TH // CH
        C0 = math.exp(-0.5)
        PE_PER_B = 6
        R32 = F32R
        CARVE = False

        def col(t, c):
            return t[:, l, c:c + 1]

        wau, wau_b = sb(es, nc, "p3_wau", [128, 512], F32)
        S.dma("sp", wau[0:64, :], self.inp["rw_w_up"][l], writes=[wau_b])
        S.dma("sp", wau[64:128, :], self.inp["rw_a_up"][l], writes=[wau_b])
        if l >= 1:
            vmu, vmu_b = sb(es, nc, "p3_vmu", [32, 512], F32)
            S.dma("sp", vmu[:], self.inp["rw_vmix_up"][l - 1], writes=[vmu_b])
        idr, idr_b = sb(es, nc, "p3_idr", [128, 128], R32)
        S.op("dve", lambda e: e.tensor_copy(out=idr[:], in_=identf[:]), reads=[identf_b], writes=[idr_b])
        rings = {}

        def R(name, n=1, shape=None, dt=F32):
            if name not in rings:
                rings[name] = Ring(es, nc, "p3_" + name, n, list(shape or (128, TH)), dt)
            return rings[name].next()

        zf, zf_b = sb(es, nc, "p3_zf", [128, 2], F32)
        S.op("pool", lambda e: e.memset(zf[:], 0.0), writes=[zf_b])
        ARr = Ring(es, nc, "p3_AR", 4, [128, NCH * 256], R32)
        BKr = Ring(es, nc, "p3_BK", 3, [128, NCH * 256], R32)
        BVr = Ring(es, nc, "p3_BV", 3, [128, NCH * 384], R32)
        Wn2 = [Ring(es, nc, "p3_W%d" % i, 2 * NCH, [128, 384], R32) for i in range(2)]
        for rg, pat, kw in ((ARr, "p (c a q t) -> p c a q t", dict(c=NCH, a=2, q=2)),
                            (BKr, "p (c a q t) -> p c a q t", dict(c=NCH, a=2, q=2)),
                            (BVr, "p (c a q t) -> p c a q t", dict(c=NCH, a=3, q=2)),
                            (Wn2[0], "p (a b) -> p a b", dict(a=3)),
                            (Wn2[1], "p (a b) -> p a b", dict(a=3))):
            new_items = []
            for (t_, b_) in rg.items:
                n_ = t_[:].shape[1]
                S.op("dve", lambda e: e.tensor_copy(out=t_[:, :], in_=zf[:, 0:1].to_broadcast([128, n_])),
                     reads=[zf_b], writes=[b_])
                new_items.append((t_[:, :].rearrange(pat, **kw), b_))
            rg.items = new_items
        PTr2 = [Ring(es, nc, "p3_PT%d" % i, 2 * NCH, [128, 128], R32) for i in range(2)]
        NM1r = Ring(es, nc, "p3_NM1", 3 * NCH, [128, 256], R32)
        NM2r = Ring(es, nc, "p3_NM2", 3 * NCH, [128, 256], R32)
        NbTr2 = [Ring(es, nc, "p3_NbT%d" % i, NCH, [128, 128], R32) for i in range(2)]
        Tfr = Ring(es, nc, "p3_Tf", 3 * NCH, [128, 128], R32)
        TM3r = Ring(es, nc, "p3_TM3", 3 * NCH, [128, 3, 128], R32)
        W1r = Ring(es, nc, "p3_W1", 2, [128, 128], R32)
        UTr = Ring(es, nc, "p3_UT", 2, [128, 128], R32)
        Sr = Ring(es, nc, "p3_S", 2, [128, 128], R32)
        if CARVE:
            psA = PsumRing(es, nc, "p3_psA", 4, 256)
            psB = PsumRing(es, nc, "p3_psB", 8, 128)
        else:
            psA = PsumRing(es, nc, "p3_psA", 2, 512)
            psB = PsumRing(es, nc, "p3_psB", 4, 512)
            psA.items = [(a[:, 0:256], b) for a, b in psA.items]
            psB.items = [(a[:, 0:128], b) for a, b in psB.items]
        psC = PsumRing(es, nc, "p3_psC", 2, 512)

        pch, pch_b = self.K["pch"]
        half, half_b = self.K["half"]
        mhalf, mhalf_b = self.K["mhalf"]

        def shift(dst, dst_b, X, X_b, mucol, npart=128, eng="pool"):
            tmp, tmp_b = R("shtmp", 2)
            S.op("act", lambda e: e.activation(out=dst[0:npart, :], in_=X[0:npart, 1:TH + 1], func=AF.Copy,
                                               scale=col(pc1m, mucol)[0:npart]),
                 reads=[X_b, pc1m_b], writes=[dst_b])
            S.op("act", lambda e: e.activation(out=tmp[0:npart, :], in_=X[0:npart, 0:TH], func=AF.Copy,
                                               scale=col(pc, mucol)[0:npart]),
                 reads=[X_b, pc_b], writes=[tmp_b])
            S.op("pool", lambda e: e.tensor_tensor(out=dst[0:npart, :], in0=dst[0:npart, :], in1=tmp[0:npart, :],
                                                   op=ALU.add), reads=[dst_b, tmp_b], writes=[dst_b])

        def sigm(dst, dst_b, src, src_b, bcol):
            S.op("act", lambda e: e.activation(out=dst[:], in_=src, func=AF.Tanh, scale=0.5, bias=col(pch, bcol)),
                 reads=[src_b, pch_b], writes=[dst_b])
            S.op("act", lambda e: e.activation(out=dst[:], in_=dst[:], func=AF.Identity, scale=0.5, bias=half[:, 0:1]),
                 reads=[dst_b, half_b], writes=[dst_b])

        def load_shifted(name, row0, nrows, t0, q):
            X, X_b = R("X" + name, 3, (128, TH + 1))
            if t0 == 0:
                S.op("pool", lambda e: e.memset(X[0:nrows, 0:1], 0.0), writes=[X_b])
                S.dma(q, X[0:nrows, 1:TH + 1], projT[row0:row0 + nrows, 0:TH], reads=[projT_b], writes=[X_b])
            else:
                S.dma(q, X[0:nrows, :], projT[row0:row0 + nrows, t0 - 1:t0 + TH], reads=[projT_b], writes=[X_b])
            return X, X_b

        blocks = [(j, tb) for j in range(4) for tb in range(T // TH)]
        preloaded = {}

        def issue_loads(bi):
            j, tb = blocks[bi]
            t0 = tb * TH
            d = {}
            d["r"] = load_shifted("r", C_RW_R + j * 128, 128, t0, "sp")
            d["k"] = load_shifted("k", C_RW_K + j * 128, 128, t0, "pool")
            d["v"] = load_shifted("v", C_RW_V + j * 128, 128, t0, "sp")
            d["w"] = load_shifted("w", C_RW_WD, 128, t0, "pool")
            Xz, Xz_b = R("Xz", 5)
            S.dma("sp", Xz[:], projT[C_RW_Z + j * 128:C_RW_Z + (j + 1) * 128, t0:t0 + TH],
                  reads=[projT_b], writes=[Xz_b])
            d["z"] = (Xz, Xz_b)
            if l >= 1:
                d["m"] = load_shifted("m", DIN, 32, t0, "sp")
                vf, vf_b = R("vf", 2)
                S.dma("pool", vf[:], vfirst[j * 128:(j + 1) * 128, t0:t0 + TH], reads=[vfirst_b], writes=[vf_b])
                d["vf"] = (vf, vf_b)
            preloaded[bi] = d

        def prepA(j, tb, par, bi):
            t0 = tb * TH
            ctx = {}
            Wn, PTr, NbTr = Wn2[par], PTr2[par], NbTr2[par]
            if bi not in preloaded:
                issue_loads(bi)
            if bi + 1 < len(blocks):
                issue_loads(bi + 1)
            ld = preloaded.pop(bi)
            (Xr, Xr_b), (Xk, Xk_b), (Xv, Xv_b), (Xw, Xw_b), (Xz, Xz_b) = ld["r"], ld["k"], ld["v"], ld["w"], ld["z"]
            yield
            rs, rs_b = R("rs")
            ks, ks_b = R("ks")
            vs, vs_b = R("vs", 2)
            was, was_b = R("was")
            shift(rs, rs_b, Xr, Xr_b, PC_MU + j)
            shift(ks, ks_b, Xk, Xk_b, PC_MU + 4 + j)
            yield
            shift(vs, vs_b, Xv, Xv_b, PC_MU + 8 + j, eng="pool")
            shift(was, was_b, Xw, Xw_b, PC_MU + 12, eng="pool")
            yield
            S.op("act", lambda e: e.activation(out=was[0:64, :], in_=was[0:64, :], func=AF.Tanh),
                 reads=[was_b], writes=[was_b])
            pz, pz_b = psC.next()
            S.op("pe", lambda e: e.matmul(pz[:, 0:TH], lhsT=wau[0:64, j * 128:(j + 1) * 128], rhs=was[0:64, :],
                                          start=True, stop=True), reads=[wau_b, was_b], writes=[pz_b])
            sg, sg_b = R("sg")
            sigm(sg, sg_b, pz[:, 0:TH], pz_b, PC_W0 + j)
            pa, pa_b = psC.next()
            S.op("pe", lambda e: e.matmul(pa[:, 0:TH], lhsT=wau[64:128, j * 128:(j + 1) * 128],
                                          rhs=was[64:128, :], start=True, stop=True),
                 reads=[wau_b, was_b], writes=[pa_b])
            aic, aic_b = R("aic")
            sigm(aic, aic_b, pa[:, 0:TH], pa_b, PC_A0 + j)
            yield
            if l == 0:
                vr, vr_b = vs, vs_b
                S.dma("pool", vfirst[j * 128:(j + 1) * 128, t0:t0 + TH], vs[:], reads=[vs_b], writes=[vfirst_b])
            else:
                Xm, Xm_b = ld["m"]
                vms, vms_b = R("vms")
                shift(vms, vms_b, Xm, Xm_b, PC_VMU, npart=32, eng="pool")
                pv, pv_b = psC.next()
                S.op("pe", lambda e: e.matmul(pv[:, 0:TH], lhsT=vmu[0:32, j * 128:(j + 1) * 128],
                                              rhs=vms[0:32, :], start=True, stop=True),
                     reads=[vmu_b, vms_b], writes=[pv_b])
                gt, gt_b = R("gt")
                sigm(gt, gt_b, pv[:, 0:TH], pv_b, PC_VM0 + j)
                vf, vf_b = ld["vf"]
                S.op("pool", lambda e: e.tensor_tensor(out=vf[:], in0=vf[:], in1=vs[:], op=ALU.subtract),
                     reads=[vf_b, vs_b], writes=[vf_b])
                S.op("pool", lambda e: e.tensor_tensor(out=vf[:], in0=vf[:], in1=gt[:], op=ALU.mult),
                     reads=[vf_b, gt_b], writes=[vf_b])
                vr, vr_b = R("vr", 2)
                S.op("pool", lambda e: e.tensor_tensor(out=vr[:], in0=vf[:], in1=vs[:], op=ALU.add),
                     reads=[vf_b, vs_b], writes=[vr_b])
            yield
            kk, kk_b = R("kk")
            S.op("act", lambda e: e.activation(out=kk[:], in_=ks[:], func=AF.Copy, scale=col(pc, PC_KK + j)),
                 reads=[ks_b, pc_b], writes=[kk_b])
            sq, sq_b = R("sq")
            S.op("pool", lambda e: e.tensor_tensor(out=sq[:], in0=kk[:], in1=kk[:], op=ALU.mult),
                 reads=[kk_b], writes=[sq_b])
            pq, pq_b = psC.next()
            S.op("pe", lambda e: e.matmul(pq[:, 0:TH], lhsT=blk64[:], rhs=sq[:], start=True, stop=True),
                 reads=[blk64_b, sq_b], writes=[pq_b])
            rn, rn_b = R("rn")
            S.op("dve", lambda e: e.tensor_scalar_max(out=rn[:], in0=pq[:, 0:TH], scalar1=1e-24),
                 reads=[pq_b], writes=[rn_b])
            S.op("dve", lambda e: e.reciprocal(out=rn[:], in_=rn[:]), reads=[rn_b], writes=[rn_b])
            bv, bv_b = R("bv")
            S.op("pool", lambda e: e.tensor_tensor(out=bv[:], in0=kk[:], in1=aic[:], op=ALU.mult),
                 reads=[kk_b, aic_b], writes=[bv_b])
            S.op("pool", lambda e: e.tensor_tensor(out=kk[:], in0=kk[:], in1=rn[:], op=ALU.mult),
                 reads=[kk_b, rn_b], writes=[kk_b])
            yield
            km, km_b = R("km")
            S.op("dve", lambda e: e.tensor_scalar(out=km[:], in0=aic[:], scalar1=col(pc, PC_KA + j),
                                                  scalar2=col(pc1m, PC_KA + j), op0=ALU.mult, op1=ALU.add),
                 reads=[aic_b, pc_b, pc1m_b], writes=[km_b])
            S.op("dve", lambda e: e.tensor_tensor(out=km[:], in0=km[:], in1=ks[:], op=ALU.mult),
                 reads=[km_b, ks_b], writes=[km_b])
            S.op("dve", lambda e: e.scalar_tensor_tensor(out=sq[:], in0=rs[:], scalar=col(pc, PC_RK + j),
                                                         in1=km[:], op0=ALU.mult, op1=ALU.mult),
                 reads=[rs_b, pc_b, km_b, sq_b], writes=[sq_b])
            pb, pb_b = psC.next()
            S.op("pe", lambda e: e.matmul(pb[:, 0:TH], lhsT=blk64[:], rhs=sq[:], start=True, stop=True),
                 reads=[blk64_b, sq_b], writes=[pb_b])
            bon, bon_b = R("bon", 4)
            S.op("dve", lambda e: e.tensor_tensor(out=bon[:], in0=pb[:, 0:TH], in1=vr[:], op=ALU.mult),
                 reads=[pb_b, vr_b], writes=[bon_b])
            yield
            G, G_b = R("G")
            S.op("dve", lambda e: e.tensor_tensor_scan(out=G[:], data0=sg[:], data1=sg[:], initial=0.0,
                                                       op0=ALU.add, op1=ALU.bypass), reads=[sg_b], writes=[G_b])
            Gs, Gs_b = R("Gs", 1, (128, NCH))
            S.op("pool", lambda e: e.memset(Gs[:, 0:1], 0.0), writes=[Gs_b])
            G3 = G[:, :].rearrange("p (c t) -> p c t", t=CH)
            S.op("dve", lambda e: e.tensor_copy(out=Gs[:, 1:NCH], in_=G3[:, 0:NCH - 1, CH - 1]),
                 reads=[G_b], writes=[Gs_b])
            csp, csp_b = R("csp")
            csp3 = csp[:, :].rearrange("p (c t) -> p c t", t=CH)
            S.op("dve", lambda e: e.tensor_tensor(out=csp3, in0=G3,
                                                  in1=Gs[:, :].unsqueeze(2).to_broadcast([128, NCH, CH]),
                                                  op=ALU.subtract), reads=[G_b, Gs_b], writes=[csp_b])
            Ep, Ep_b = R("Ep", 4)
            Em, Em_b = R("Em")
            Eq, Eq_b = R("Eq")
            S.op("act", lambda e: e.activation(out=Ep[:], in_=csp[:], func=AF.Exp, scale=-C0),
                 reads=[csp_b], writes=[Ep_b])
            S.op("act", lambda e: e.activation(out=Em[:], in_=csp[:], func=AF.Exp, scale=C0),
                 reads=[csp_b], writes=[Em_b])
            S.op("pool", lambda e: e.tensor_tensor(out=Eq[:], in0=csp[:], in1=sg[:], op=ALU.subtract),
                 reads=[csp_b, sg_b], writes=[Eq_b])
            S.op("act", lambda e: e.activation(out=Eq[:], in_=Eq[:], func=AF.Exp, scale=-C0),
                 reads=[Eq_b], writes=[Eq_b])
            yield
            Ep3 = Ep[:, :].rearrange("p (c t) -> p c t", t=CH)
            AR, AR_b = ARr.next()
            BK, BK_b = BKr.next()
            BV, BV_b = BVr.next()

            def v3(t_, p):
                return t_[p * 64:(p + 1) * 64, :].rearrange("p (c t) -> p c t", t=CH)

            for p in range(2):
                hs = slice(p * 64, (p + 1) * 64)
                S.op("dve", lambda e: e.scalar_tensor_tensor(out=AR[hs, :, 0, p, :], in0=v3(kk, p), scalar=-1.0,
                                                             in1=v3(Eq, p), op0=ALU.mult, op1=ALU.mult),
                     reads=[kk_b, Eq_b], writes=[AR_b])
                S.op("dve", lambda e: e.tensor_tensor(out=AR[hs, :, 1, p, :], in0=v3(rs, p), in1=v3(Ep, p),
                                                      op=ALU.mult), reads=[rs_b, Ep_b], writes=[AR_b])
                S.op("dve", lambda e: e.tensor_tensor(out=BK[hs, :, 0, p, :], in0=v3(bv, p), in1=v3(Em, p),
                                                      op=ALU.mult), reads=[bv_b, Em_b], writes=[BK_b])
                S.op("dve", lambda e: e.tensor_tensor(out=BK[hs, :, 1, p, :], in0=v3(km, p), in1=v3(Em, p),
                                                      op=ALU.mult), reads=[km_b, Em_b], writes=[BK_b])
                gcb = Ep3[hs, :, CH - 1:CH].to_broadcast([64, NCH, CH])
                S.op("dve", lambda e: e.tensor_tensor(out=BV[hs, :, 0, p, :], in0=BK[hs, :, 0, p, :], in1=gcb,
                                                      op=ALU.mult), reads=[BK_b, Ep_b], writes=[BV_b])
                S.op("dve", lambda e: e.tensor_tensor(out=BV[hs, :, 1, p, :], in0=BK[hs, :, 1, p, :], in1=gcb,
                                                      op=ALU.mult), reads=[BK_b, Ep_b], writes=[BV_b])
                S.op("dve", lambda e: e.tensor_copy(out=BV[hs, :, 2, p, :], in_=v3(vr, p)),
                     reads=[vr_b], writes=[BV_b])
                yield
            yield "A"
            ch = []
            for c in range(NCH):
                ARc = AR[:, c].rearrange("p a q t -> p (a q t)")
                d = {"ARc": ARc, "Abd": ARc[:, 0:128], "Rbd": ARc[:, 128:256],
                     "Bbd": BK[:, c, 0].rearrange("p q t -> p (q t)"),
                     "Kbd": BK[:, c, 1].rearrange("p q t -> p (q t)")}
                ch.append(d)
            for d in ch:
                p1, p1_b = psA.next()
                S.op("pe", lambda e: e.matmul(p1[:, 0:256], lhsT=d["Bbd"], rhs=d["ARc"], start=True, stop=True),
                     reads=[BK_b, AR_b], writes=[p1_b])
                d["NM1"], d["NM1_b"] = NM1r.next()
                S.op("dve", lambda e: e.tensor_tensor(out=d["NM1"][:], in0=p1[:, 0:256], in1=mask2[:], op=ALU.mult),
                     reads=[p1_b, mask2_b], writes=[d["NM1_b"]])
            yield
            for d in ch:
                p2, p2_b = psA.next()
                S.op("pe", lambda e: e.matmul(p2[:, 0:256], lhsT=d["Kbd"], rhs=d["ARc"], start=True, stop=True),
                     reads=[BK_b, AR_b], writes=[p2_b])
                d["NM2"], d["NM2_b"] = NM2r.next()
                S.op("dve", lambda e: e.tensor_tensor(out=d["NM2"][:], in0=p2[:, 0:256], in1=mask2[:], op=ALU.mult),
                     reads=[p2_b, mask2_b], writes=[d["NM2_b"]])
            yield
            for d in ch:
                p3, p3_b = psB.next()
                S.op("pe", lambda e: e.matmul(p3[:, :], lhsT=d["Abd"], rhs=d["Bbd"], start=True, stop=True),
                     reads=[AR_b, BK_b], writes=[p3_b])
                d["NbT"], d["NbT_b"] = NbTr.next()
                S.op("dve", lambda e: e.tensor_tensor(out=d["NbT"][:], in0=p3[:, :], in1=maskl[:], op=ALU.mult),
                     reads=[p3_b, maskl_b], writes=[d["NbT_b"]])
            yield
            for d in ch:
                Nba = d["NM1"][:, 0:128]
                d["W"], d["W_b"] = Wn.next()
                W = d["W"]
                S.op("pool", lambda e: e.tensor_tensor(out=W[:, 2, :], in0=Nba, in1=idr[:], op=ALU.add),
                     reads=[d["NM1_b"], idr_b], writes=[d["W_b"]])
                p4, p4_b = psB.next()
                S.op("pe", lambda e: e.matmul(p4[:, :], lhsT=d["NbT"][:], rhs=Nba, start=True, stop=True),
                     reads=[d["NbT_b"], d["NM1_b"]], writes=[p4_b])
                S.op("act", lambda e: e.activation(out=W[:, 0, :], in_=p4[:, :], func=AF.Copy),
                     reads=[p4_b], writes=[d["W_b"]])
                p5, p5_b = psB.next()
                S.op("pe", lambda e: e.matmul(p5[:, :], lhsT=Nba, rhs=d["NbT"][:], start=True, stop=True),
                     reads=[d["NbT_b"], d["NM1_b"]], writes=[p5_b])
                d["PT"], d["PT_b"] = PTr.next()
                PT = d["PT"]
                S.op("act", lambda e: e.activation(out=PT[:], in_=p5[:, :], func=AF.Copy),
                     reads=[p5_b], writes=[d["PT_b"]])
            yield
            for m in (2, 4, 8, 16, 32):
                for d in ch:
                    W, W_b, PT, PT_b = d["W"], d["W_b"], d["PT"], d["PT_b"]
                    pw, pw_b = psA.next()
                    S.op("pe", lambda e: e.matmul(pw[:, 0:256].rearrange("p (a b) -> p a b", a=2), lhsT=PT[:],
                                                  rhs=W[:, 0:3:2, :], start=True, stop=True),
                         reads=[PT_b, W_b], writes=[pw_b])
                    if m < 32:
                        W2, W2_b = Wn.next()
                        S.op("dve", lambda e: e.tensor_tensor(out=W2[:, 0:3:2, :],
                                                              in0=pw[:, 0:256].rearrange("p (a b) -> p a b", a=2),
                                                              in1=W[:, 1:3, :], op=ALU.add),
                             reads=[pw_b, W_b], writes=[W2_b])
                        pq2, pq2_b = psB.next()
                        S.op("pe", lambda e: e.matmul(pq2[:, :], lhsT=W[:, 0, :], rhs=PT[:], start=True, stop=True),
                             reads=[W_b, PT_b], writes=[pq2_b])
                        PT2, PT2_b = PTr.next()
                        S.op("act", lambda e: e.activation(out=PT2[:], in_=pq2[:, :], func=AF.Copy),
                             reads=[pq2_b], writes=[PT2_b])
                        d["W"], d["W_b"], d["PT"], d["PT_b"] = W2, W2_b, PT2, PT2_b
                    else:
                        d["Tf"], d["Tf_b"] = Tfr.next()
                        Tf = d["Tf"]
                        S.op("dve", lambda e: e.tensor_tensor(out=Tf[:], in0=pw[:, 128:256], in1=W[:, 2, :], op=ALU.add),
                             reads=[pw_b, W_b], writes=[d["Tf_b"]])
                yield
            for c, d in enumerate(ch):
                pt3, pt3_b = psC.next()
                for i in range(3):
                    S.op("pe", lambda e: e.matmul(pt3[:, i * 128:(i + 1) * 128],
                                                  lhsT=BV[:, c, i].rearrange("p q t -> p (q t)"), rhs=idr[:],
                                                  start=True, stop=True),
                         reads=[BV_b, idr_b], writes=[pt3_b])
                d["TM3"], d["TM3_b"] = TM3r.next()
                TM3 = d["TM3"]
                S.op("act", lambda e: e.activation(out=TM3[:].rearrange("p a b -> p (a b)"), in_=pt3[:, 0:384],
                                                   func=AF.Copy), reads=[pt3_b], writes=[d["TM3_b"]])
            yield
            ctx.update(ch=ch, AR_b=AR_b, Ep=Ep, Ep_b=Ep_b, bon=bon, bon_b=bon_b, Xz=Xz, Xz_b=Xz_b, j=j, t0=t0)
            return ctx

        def stageB(ctx, state):
            ch, AR_b, Ep, Ep_b = ctx["ch"], ctx["AR_b"], ctx["Ep"], ctx["Ep_b"]
            j, t0 = ctx["j"], ctx["t0"]
            ob, ob_b = R("ob", 2)
            for c, d in enumerate(ch):
                Scur, Scur_b = state["S"], state["S_b"]
                Nka, Mkr = d["NM2"][:, 0:128], d["NM2"][:, 128:256]
                Mbr = d["NM1"][:, 128:256]
                TM3, TM3_b, Tf, Tf_b = d["TM3"], d["TM3_b"], d["Tf"], d["Tf_b"]
                BpT, KpT, VT = TM3[:, 0, :], TM3[:, 1, :], TM3[:, 2, :]
                pw1, pw1_b = psB.next()
                S.op("pe", lambda e: e.matmul(pw1[:, :], lhsT=d["Abd"], rhs=Scur[:], start=True, stop=False),
                     reads=[AR_b, Scur_b], writes=[pw1_b])
                S.op("pe", lambda e: e.matmul(pw1[:, :], lhsT=Nka, rhs=VT, start=False, stop=True),
                     reads=[d["NM2_b"], TM3_b], writes=[pw1_b])
                W1, W1_b = W1r.next()
                S.op("act", lambda e: e.activation(out=W1[:], in_=pw1[:, :], func=AF.Copy),
                     reads=[pw1_b], writes=[W1_b])
                yield
                pu, pu_b = psB.next()
                S.op("pe", lambda e: e.matmul(pu[:, :], lhsT=Tf[:], rhs=W1[:], start=True, stop=True),
                     reads=[Tf_b, W1_b], writes=[pu_b])
                UT, UT_b = UTr.next()
                S.op("dve", lambda e: e.tensor_copy(out=UT[:], in_=pu[:, :]), reads=[pu_b], writes=[UT_b])
                yield
                ps2, ps2_b = psB.next()
                S.op("pe", lambda e: e.matmul(ps2[:, :], lhsT=BpT, rhs=UT[:], start=True, stop=False),
                     reads=[TM3_b, UT_b], writes=[ps2_b])
                S.op("pe", lambda e: e.matmul(ps2[:, :], lhsT=KpT, rhs=VT, start=False, stop=True),
                     reads=[TM3_b], writes=[ps2_b])
                Snx, Snx_b = Sr.next()
                gc = Ep[:, c * CH + CH - 1:c * CH + CH]
                S.op("dve", lambda e: e.scalar_tensor_tensor(out=Snx[:], in0=Scur[:], scalar=gc, in1=ps2[:, :],
                                                             op0=ALU.mult, op1=ALU.add),
                     reads=[Scur_b, Ep_b, ps2_b], writes=[Snx_b])
                po, po_b = psB.next()
                S.op("pe", lambda e: e.matmul(po[:, :], lhsT=Scur[:], rhs=d["Rbd"], start=True, stop=False),
                     reads=[Scur_b, AR_b], writes=[po_b])
                S.op("pe", lambda e: e.matmul(po[:, :], lhsT=UT[:], rhs=Mbr, start=False, stop=False),
                     reads=[UT_b, d["NM1_b"]], writes=[po_b])
                S.op("pe", lambda e: e.matmul(po[:, :], lhsT=VT, rhs=Mkr, start=False, stop=True),
                     reads=[TM3_b, d["NM2_b"]], writes=[po_b])
                for p in range(2):
                    hs = slice(p * 64, (p + 1) * 64)
                    S.op("act", lambda e: e.activation(out=ob[hs, c * CH:(c + 1) * CH],
                                                       in_=po[hs, p * 64:(p + 1) * 64], func=AF.Copy),
                         reads=[po_b], writes=[ob_b])
                state["S"], state["S_b"] = Snx, Snx_b
                yield
            bon, bon_b, Xz, Xz_b = ctx["bon"], ctx["bon_b"], ctx["Xz"], ctx["Xz_b"]
            pm, pm_b = psC.next()
            S.op("pe", lambda e: e.matmul(pm[:, 0:TH], lhsT=blk64[:], rhs=ob[:], start=True, stop=True),
                 reads=[blk64_b, ob_b], writes=[pm_b])
            dd, dd_b = R("dd")
            S.op("dve", lambda e: e.scalar_tensor_tensor(out=dd[:], in0=pm[:, 0:TH], scalar=-1.0 / 64, in1=ob[:],
                                                         op0=ALU.mult, op1=ALU.add),
                 reads=[pm_b, ob_b], writes=[dd_b])
            sq2, sq2_b = R("sq2")
            S.op("pool", lambda e: e.tensor_tensor(out=sq2[:], in0=dd[:], in1=dd[:], op=ALU.mult),
                 reads=[dd_b], writes=[sq2_b])
            pvv, pvv_b = psC.next()
            S.op("pe", lambda e: e.matmul(pvv[:, 0:TH], lhsT=blk64[:], rhs=sq2[:], start=True, stop=True),
                 reads=[blk64_b, sq2_b], writes=[pvv_b])
            rn2, rn2_b = R("rn2")
            S.op("dve", lambda e: e.tensor_scalar(out=rn2[:], in0=pvv[:, 0:TH], scalar1=1.0 / 64, scalar2=GN_EPS,
                                                  op0=ALU.mult, op1=ALU.add), reads=[pvv_b], writes=[rn2_b])
            S.op("act", lambda e: e.activation(out=rn2[:], in_=rn2[:], func=AF.Sqrt), reads=[rn2_b], writes=[rn2_b])
            S.op("dve", lambda e: e.reciprocal(out=rn2[:], in_=rn2[:]), reads=[rn2_b], writes=[rn2_b])
            yield
            S.op("pool", lambda e: e.tensor_tensor(out=dd[:], in0=dd[:], in1=rn2[:], op=ALU.mult),
                 reads=[dd_b, rn2_b], writes=[dd_b])
            S.op("dve", lambda e: e.tensor_scalar(out=dd[:], in0=dd[:], scalar1=col(pc, PC_LNG + j),
                                                  scalar2=col(pc, PC_LNB + j), op0=ALU.mult, op1=ALU.add),
                 reads=[dd_b, pc_b], writes=[dd_b])
            S.op("pool", lambda e: e.tensor_tensor(out=dd[:], in0=dd[:], in1=bon[:], op=ALU.add),
                 reads=[dd_b, bon_b], writes=[dd_b])
            th, th_b = R("th")
            S.op("act", lambda e: e.activation(out=th[:], in_=Xz[:], func=AF.Tanh, scale=0.5), reads=[Xz_b], writes=[th_b])
            S.op("dve", lambda e: e.scalar_tensor_tensor(out=th[:], in0=th[:], scalar=1.0, in1=Xz[:],
                                                         op0=ALU.add, op1=ALU.mult),
                 reads=[th_b, Xz_b], writes=[th_b])
            yb, yb_b = R("yb", 2, (128, TH), BF16)
            S.op("dve", lambda e: e.scalar_tensor_tensor(out=yb[:], in0=dd[:], scalar=0.5, in1=th[:],
                                                         op0=ALU.mult, op1=ALU.mult),
                 reads=[dd_b, th_b], writes=[yb_b])
            S.dma("sp", ybT[j * 128:(j + 1) * 128, t0:t0 + TH], yb[:], reads=[yb_b], writes=[ybT_b])
            yield

        state = {}
        nxt = 0
        gP = gB = None
        gAs = []
        gP_waiting = False
        bq = []
        while nxt < len(blocks) or gP is not None or gAs or gB is not None or bq:
            if gP is None and nxt < len(blocks):
                gP = prepA(blocks[nxt][0], blocks[nxt][1], nxt % 2, nxt)
                nxt += 1
                gP_waiting = False
            if gB is None and bq:
                ctx = bq.pop(0)
                if ctx["t0"] == 0:
                    S0, S0_b = Sr.next()
                    S.op("dve", lambda e: e.tensor_copy(out=S0[:], in_=zf[:, 0:1].to_broadcast([128, 128])),
                         reads=[zf_b], writes=[S0_b])
                    state["S"], state["S_b"] = S0, S0_b
                gB = stageB(ctx, state)
            if gB is not None:
                try:
                    next(gB)
                except StopIteration:
                    gB = None
            for g in list(gAs):
                try:
                    next(g)
                except StopIteration as st:
                    assert g is gAs[0]
                    bq.append(st.value)
                    gAs.remove(g)
            if gP is not None:
                if not gP_waiting:
                    if next(gP) == "A":
                        gP_waiting = True
                if gP_waiting and len(gAs) < 2 and len(gAs) + len(bq) + (1 if gB is not None else 0) <= 2:
                    gAs.append(gP)
                    gP, gP_waiting = None, False
            yield

    def phase3(self, l):
        with ExitStack() as es:
            for _ in self.phase3_gen(l, es):
                pass

    def phase4(self, l, xin, xin_b, xo, xo_b):
        nc, S = self.nc, self.S
        projT, projT_b = self.scr["projT"], self.dbuf["projT"]
        yagT, yagT_b = self.scr["yagT"], self.dbuf["yagT"]
        ybT, ybT_b = self.scr["ybT"], self.dbuf["ybT"]
        with ExitStack() as es:
            gpost, gpost_b = sb(es, nc, "p4_gpost", [128, D], F32)
            S.dma("pool", gpost[:], self.inp["norm_post"][l].partition_broadcast(128), writes=[gpost_b])
            ya, ya_b = sb(es, nc, "p4_ya", [128, 4, T], BF16)
            yb, yb_b = sb(es, nc, "p4_yb", [128, 4, T], BF16)
            S.dma("pool", ya[:], yagT.rearrange("(c p) t -> p c t", p=128), reads=[yagT_b], writes=[ya_b])
            S.dma("pool", yb[:], ybT.rearrange("(c p) t -> p c t", p=128), reads=[ybT_b], writes=[yb_b])
            wst = Ring(es, nc, "p4_wst", 2, [128, 4, D], F32)
            wua, wua_b = sb(es, nc, "p4_wua", [128, 4, D], BF16)
            wur, wur_b = sb(es, nc, "p4_wur", [128, 4, D], BF16)
            wo, wo_b = sb(es, nc, "p4_wo", [128, 8, D], BF16)
            srcs = [(self.inp["w_up_att"][l].rearrange("(c p) e -> p c e", p=128), wua[:, :, :], wua_b),
                    (self.inp["w_up_rw"][l].rearrange("(c p) e -> p c e", p=128), wur[:, :, :], wur_b),
                    (self.inp["w_out"][l].rearrange("(c p) e -> p c e", p=128)[:, 0:4, :], wo[:, 0:4, :], wo_b),
                    (self.inp["w_out"][l].rearrange("(c p) e -> p c e", p=128)[:, 4:8, :], wo[:, 4:8, :], wo_b)]
            for i, (src, dst, dst_b) in enumerate(srcs):
                ws, ws_b = wst.next()
                S.dma("sp", ws[:], src, writes=[ws_b])
                S.op("pool" if i % 2 == 0 else "dve", lambda e: e.tensor_copy(out=dst, in_=ws[:]),
                     reads=[ws_b], writes=[dst_b])
            uTr = Ring(es, nc, "p4_uT", 2, [128, 8, 512], BF16)
            gAr = Ring(es, nc, "p4_gA", 6, [128, 512], F32)
            gRr = Ring(es, nc, "p4_gR", 6, [128, 512], F32)
            t1r = Ring(es, nc, "p4_t1", 2, [128, 512], F32)
            t2r = Ring(es, nc, "p4_t2", 2, [128, 512], F32)
            junk, junk_b = sb(es, nc, "p4_junk", [128, 512], F32)
            ssr = Ring(es, nc, "p4_ss", 4, [128, 4], F32)
            xtr = Ring(es, nc, "p4_xt", 4, [128, D], F32)
            otr = Ring(es, nc, "p4_ot", 3, [128, D], F32)
            psa = Ring(es, nc, "p4_psa", 2, [128, 512], F32, psum=True)
            psr = Ring(es, nc, "p4_psr", 2, [128, 512], F32, psum=True)
            psy = Ring(es, nc, "p4_psy", 4, [128, 512], F32, psum=True)
            half, half_b = self.K["half"]
            mhalf, mhalf_b = self.K["mhalf"]
            items = [(tc, eg) for tc in range(4) for eg in range(8)]

            def issue_g(i):
                tc, eg = items[i]
                ts_ = slice(tc * 512, (tc + 1) * 512)
                gA, gA_b = gAr.next()
                gR, gR_b = gRr.next()
                S.dma("sp", gA[:], projT[C_G_ATT + eg * 128:C_G_ATT + (eg + 1) * 128, ts_], reads=[projT_b], writes=[gA_b])
                S.dma("pool", gR[:], projT[C_G_RW + eg * 128:C_G_RW + (eg + 1) * 128, ts_], reads=[projT_b], writes=[gR_b])
                return gA, gA_b, gR, gR_b

            def ytile(tc, tt, uT, uT_b, xt, xt_b):
                tok0 = tc * 512 + tt * 128
                ss, ss_b = ssr.next()
                pys = []
                for hf in range(2):
                    py, py_b = psy.next()
                    for eg in range(8):
                        S.op("pe", lambda e: e.matmul(py[:, :], lhsT=uT[:, eg, tt * 128:(tt + 1) * 128],
                                                      rhs=wo[:, eg, hf * 512:(hf + 1) * 512],
                                                      start=(eg == 0), stop=(eg == 7)), reads=[uT_b, wo_b], writes=[py_b])
                    S.op("act", lambda e: e.activation(out=junk[:], in_=py[:, :], func=AF.Square,
                                                       accum_out=ss[:, hf:hf + 1]),
                         reads=[py_b], writes=[junk_b, ss_b])
                    pys.append((py, py_b))
                S.op("dve", lambda e: e.tensor_tensor(out=ss[:, 2:3], in0=ss[:, 0:1], in1=ss[:, 1:2], op=ALU.add),
                     reads=[ss_b], writes=[ss_b])
                S.op("dve", lambda e: e.tensor_scalar(out=ss[:, 3:4], in0=ss[:, 2:3], scalar1=1.0 / D, scalar2=RMS_EPS,
                                                      op0=ALU.mult, op1=ALU.add), reads=[ss_b], writes=[ss_b])
                S.op("act", lambda e: e.activation(out=ss[:, 3:4], in_=ss[:, 3:4], func=AF.Sqrt),
                     reads=[ss_b], writes=[ss_b])
                S.op("dve", lambda e: e.reciprocal(out=ss[:, 2:3], in_=ss[:, 3:4]),
                     reads=[ss_b], writes=[ss_b])
                ot, ot_b = otr.next()
                for hf in range(2):
                    py, py_b = pys[hf]
                    S.op("dve", lambda e: e.scalar_tensor_tensor(out=ot[:, hf * 512:(hf + 1) * 512], in0=py[:, :],
                                                                 scalar=ss[:, 2:3], in1=gpost[:, hf * 512:(hf + 1) * 512],
                                                                 op0=ALU.mult, op1=ALU.mult),
                         reads=[py_b, ss_b, gpost_b], writes=[ot_b])
                S.op("pool", lambda e: e.tensor_tensor(out=ot[:], in0=ot[:], in1=xt[:], op=ALU.add),
                     reads=[ot_b, xt_b], writes=[ot_b])
                S.dma("sp", xo[tok0:tok0 + 128, :], ot[:], reads=[ot_b], writes=[xo_b])

            LOOK = 4
            pendg = [issue_g(i) for i in range(LOOK)]
            ytodo = []
            for tc in range(4):
                ts_ = slice(tc * 512, (tc + 1) * 512)
                uT, uT_b = uTr.next()
                for eg in range(8):
                    i = tc * 8 + eg
                    gA, gA_b, gR, gR_b = pendg.pop(0)
                    if i + LOOK < len(items):
                        pendg.append(issue_g(i + LOOK))
                    for (g_, g_b) in ((gA, gA_b), (gR, gR_b)):
                        S.op("act", lambda e: e.activation(out=g_[:], in_=g_[:], func=AF.Tanh, scale=0.5),
                             reads=[g_b], writes=[g_b])
                        S.op("act", lambda e: e.activation(out=g_[:], in_=g_[:], func=AF.Identity, scale=0.5,
                                                           bias=half[:, 0:1]),
                             reads=[g_b, half_b], writes=[g_b])
                    pa, pa_b = psa.next()
                    pr, pr_b = psr.next()
                    for c in range(4):
                        S.op("pe", lambda e: e.matmul(pa[:, :], lhsT=wua[:, c, eg * 128:(eg + 1) * 128], rhs=ya[:, c, ts_],
                                                      start=(c == 0), stop=(c == 3)), reads=[wua_b, ya_b], writes=[pa_b])
                    for c in range(4):
                        S.op("pe", lambda e: e.matmul(pr[:, :], lhsT=wur[:, c, eg * 128:(eg + 1) * 128], rhs=yb[:, c, ts_],
                                                      start=(c == 0), stop=(c == 3)), reads=[wur_b, yb_b], writes=[pr_b])
                    t1, t1_b = t1r.next()
                    t2, t2_b = t2r.next()
                    S.op("dve", lambda e: e.tensor_tensor(out=t1[:], in0=pa[:, :], in1=gA[:], op=ALU.mult),
                         reads=[pa_b, gA_b], writes=[t1_b])
                    S.op("dve", lambda e: e.tensor_tensor(out=t2[:], in0=pr[:, :], in1=gR[:], op=ALU.mult),
                         reads=[pr_b, gR_b], writes=[t2_b])
                    S.op("pool", lambda e: e.tensor_tensor(out=uT[:, eg, :], in0=t1[:], in1=t2[:], op=ALU.add),
                         reads=[t1_b, t2_b], writes=[uT_b])
                    if eg % 2 == 1 and ytodo:
                        ytile(*ytodo.pop(0))
                while ytodo:
                    ytile(*ytodo.pop(0))
                for tt in range(4):
                    tok0 = tc * 512 + tt * 128
                    xt, xt_b = xtr.next()
                    S.dma("pool", xt[:], xin[tok0:tok0 + 128, :], reads=[xin_b], writes=[xt_b])
                    ytodo.append((tc, tt, uT, uT_b, xt, xt_b))
            while ytodo:
                ytile(*ytodo.pop(0))


def make_in_maps(inputs, hc=None):
    hc = hc or host_consts()
    pc = pack_params(inputs)
    shared = {}
    for k in ("norm_pre", "norm_post", "w_in", "rw_w_up", "rw_a_up", "rw_vmix_down", "rw_vmix_up",
              "w_up_att", "w_up_rw", "w_out"):
        shared[k] = np.ascontiguousarray(np.asarray(inputs[k], dtype=np.float32))
    shared["pc"] = pc
    for k, v in hc.items():
        shared["c_" + k] = v
    x = np.asarray(inputs["x"], dtype=np.float32)
    maps = []
    for c in range(NCORES):
        m = dict(shared)
        m["x"] = np.ascontiguousarray(x[c])
        maps.append(m)
    return maps


def kernel(**inputs):
    prog = Prog()
    nc = prog.build()
    maps = make_in_maps(inputs)
    res = run_bass_kernel_spmd(nc, maps, core_ids=list(range(NCORES)))
    return np.stack([np.asarray(r["out"], dtype=np.float32) for r in res.results], axis=0)
```
